# Optimizing a Trainium2 kernel written in Bass

```python
import jax, jax.numpy as jnp
from jax import lax
import numpy as np

D_MODEL = 2048
BATCH = 4
SEQ = 4096
DEPTH = 1

CHUNK = 64
EPS = 1e-6
PLE_DIM = 256
POOL_WINDOWS = (2, 4, 8, 16)
POOL_WIDTH = D_MODEL // 2
POOL_GROUP_DIM = POOL_WIDTH // len(POOL_WINDOWS)
SGU_BLOCK = 128
SGU_GROUPS = 8
SGU_WIDTH = D_MODEL // 2
SGU_GROUP_DIM = SGU_WIDTH // SGU_GROUPS
N_IN = POOL_WIDTH + 2 * SGU_WIDTH
N_EXPERT_GROUPS = 4
EXPERTS_PER_GROUP = 8
N_EXPERTS = N_EXPERT_GROUPS * EXPERTS_PER_GROUP
TOP_K_EXPERT = 2
D_EXPERT = D_MODEL // 4
MOE_BLOCK_ROWS = 256

kernel_name = "hybrid_pool_sgu_hmoe_block"


def _rmsnorm(x, g):
    xf = x.astype(jnp.float32)
    y = xf * lax.rsqrt(jnp.mean(xf * xf, axis=-1, keepdims=True) + EPS)
    return (y * g.astype(jnp.float32)).astype(x.dtype)


def _layernorm(x, g, b):
    xf = x.astype(jnp.float32)
    mu = jnp.mean(xf, axis=-1, keepdims=True)
    xc = xf - mu
    var = jnp.mean(xc * xc, axis=-1, keepdims=True)
    y = xc * lax.rsqrt(var + EPS) * g.astype(jnp.float32) + b.astype(jnp.float32)
    return y.astype(x.dtype)


def _pool_mixer(a, w_pool, pool_scale):
    bsz, s, _ = a.shape
    af = a.astype(jnp.float32)
    cs = jnp.cumsum(af, axis=1)
    t = jnp.arange(1, s + 1, dtype=jnp.float32)
    outs = []
    for gi, w in enumerate(POOL_WINDOWS):
        lo, hi = gi * POOL_GROUP_DIM, (gi + 1) * POOL_GROUP_DIM
        cs_g = cs[:, :, lo:hi]
        prev = jnp.pad(cs_g[:, : s - w], ((0, 0), (w, 0), (0, 0)))
        cnt = jnp.minimum(t, w)[None, :, None]
        outs.append((cs_g - prev) / cnt - af[:, :, lo:hi])
    z = jnp.stack(outs, axis=2).astype(a.dtype)
    y = jnp.einsum('bsgc,gcd->bsgd', z, w_pool).reshape(bsz, s, POOL_WIDTH)
    return y * pool_scale


def _spatial_gating(u, v, ln_g, ln_b, w_spatial, b_spatial):
    bsz, s, _ = u.shape
    u = jax.nn.gelu(u)
    v = _layernorm(jax.nn.gelu(v), ln_g, ln_b)
    nblk = s // SGU_BLOCK
    vb = v.reshape(bsz, nblk, SGU_BLOCK, SGU_GROUPS, SGU_GROUP_DIM)
    pos_chunk = jnp.arange(SGU_BLOCK) // CHUNK
    mask = pos_chunk[None, :] <= pos_chunk[:, None]
    ws = jnp.where(mask[None], w_spatial, 0.0)
    vm = jnp.einsum('gts,bnsgc->bntgc', ws, vb) + b_spatial.T[None, None, :, :, None]
    return u * vm.reshape(bsz, s, SGU_WIDTH)


def _hier_moe(h, w_rg, b_rg, w_re, b_re, w_g, w_u, w_d):
    bsz, s, d = h.shape
    n_tok = bsz * s
    hf = h.reshape(n_tok, d)
    grp_logits = (hf @ w_rg).astype(jnp.float32) + b_rg.astype(jnp.float32)
    grp_prob = jax.nn.softmax(grp_logits, axis=-1)
    grp_p, grp_idx = lax.top_k(grp_prob, 1)
    exp_logits = jnp.einsum('td,gde->tge', hf, w_re).astype(jnp.float32) + b_re.astype(jnp.float32)
    sel = jnp.take_along_axis(exp_logits, grp_idx[:, :, None], axis=1)[:, 0]
    top_logit, top_loc = lax.top_k(sel, TOP_K_EXPERT)
    weights = grp_p * jax.nn.softmax(top_logit, axis=-1)
    expert_id = grp_idx * EXPERTS_PER_GROUP + top_loc

    n_assign = n_tok * TOP_K_EXPERT
    n_blocks = -(-n_assign // MOE_BLOCK_ROWS) + N_EXPERTS
    n_rows = n_blocks * MOE_BLOCK_ROWS
    flat_e = expert_id.reshape(-1).astype(jnp.int32)
    flat_tok = jnp.repeat(jnp.arange(n_tok, dtype=jnp.int32), TOP_K_EXPERT)
    flat_w = weights.reshape(-1)
    order = jnp.argsort(flat_e)
    se, st, sw = flat_e[order], flat_tok[order], flat_w[order]
    counts = jnp.bincount(flat_e, length=N_EXPERTS)
    padded = (counts + MOE_BLOCK_ROWS - 1) // MOE_BLOCK_ROWS * MOE_BLOCK_ROWS
    pad_end = jnp.cumsum(padded)
    pad_start = pad_end - padded
    start = jnp.cumsum(counts) - counts
    dest = pad_start[se] + jnp.arange(n_assign, dtype=jnp.int32) - start[se]
    row_tok = jnp.full((n_rows,), n_tok, jnp.int32).at[dest].set(st)
    row_w = jnp.zeros((n_rows,), jnp.float32).at[dest].set(sw)
    block_start = jnp.arange(n_blocks, dtype=jnp.int32) * MOE_BLOCK_ROWS
    block_e = jnp.minimum(jnp.searchsorted(pad_end, block_start, side='right'), N_EXPERTS - 1)
    h_pad = jnp.concatenate([hf, jnp.zeros((1, d), hf.dtype)], axis=0)

    def expert_rows(args):
        e, tok, wt = args
        xb = h_pad[tok]
        hid = jax.nn.silu(xb @ w_g[e]) * (xb @ w_u[e])
        return (hid @ w_d[e]) * wt[:, None].astype(xb.dtype)

    ys = lax.map(expert_rows, (block_e, row_tok.reshape(n_blocks, MOE_BLOCK_ROWS),
                               row_w.reshape(n_blocks, MOE_BLOCK_ROWS)))
    out = jax.ops.segment_sum(ys.reshape(n_rows, d), row_tok, num_segments=n_tok + 1)[:n_tok]
    return out.reshape(bsz, s, d).astype(h.dtype)


def setup_inputs(seed: int = 0) -> dict:
    key = jax.random.key(seed)
    ks = jax.random.split(key, 32)
    L, D = DEPTH, D_MODEL

    def nrm(k, shape, fan_in):
        return jax.random.normal(k, shape, jnp.float32) * (fan_in ** -0.5)

    def gain(k, shape):
        return 1.0 + 0.02 * jax.random.normal(k, shape, jnp.float32)

    def bias(k, shape, scale=0.02):
        return scale * jax.random.normal(k, shape, jnp.float32)

    return {
        "x": jax.random.normal(ks[0], (BATCH, SEQ, D), jnp.float32),
        "p": jax.random.normal(ks[1], (DEPTH, BATCH, SEQ, PLE_DIM), jnp.float32),
        "g_mix": gain(ks[2], (L, D)),
        "w_in": nrm(ks[3], (L, D, N_IN), D),
        "w_pool": nrm(ks[4], (L, len(POOL_WINDOWS), POOL_GROUP_DIM, POOL_GROUP_DIM), POOL_GROUP_DIM),
        "pool_scale": gain(ks[5], (L, POOL_WIDTH)),
        "w_branch_a": nrm(ks[6], (L, POOL_WIDTH, D), POOL_WIDTH),
        "sgu_ln_g": gain(ks[7], (L, SGU_WIDTH)),
        "sgu_ln_b": bias(ks[8], (L, SGU_WIDTH)),
        "w_spatial": nrm(ks[9], (L, SGU_GROUPS, SGU_BLOCK, SGU_BLOCK), SGU_BLOCK),
        "b_spatial": gain(ks[10], (L, SGU_GROUPS, SGU_BLOCK)),
        "w_branch_b": nrm(ks[11], (L, SGU_WIDTH, D), SGU_WIDTH),
        "w_merge_gate": nrm(ks[12], (L, D, 2 * D), D),
        "b_merge_gate": bias(ks[13], (L, 2 * D)),
        "w_out": nrm(ks[14], (L, D, D), D),
        "g_ffn": gain(ks[15], (L, D)),
        "w_router_group": nrm(ks[16], (L, D, N_EXPERT_GROUPS), D),
        "b_router_group": bias(ks[17], (L, N_EXPERT_GROUPS), 0.01),
        "w_router_expert": nrm(ks[18], (L, N_EXPERT_GROUPS, D, EXPERTS_PER_GROUP), D),
        "b_router_expert": bias(ks[19], (L, N_EXPERT_GROUPS, EXPERTS_PER_GROUP), 0.01),
        "w_exp_gate": nrm(ks[20], (L, N_EXPERTS, D, D_EXPERT), D),
        "w_exp_up": nrm(ks[21], (L, N_EXPERTS, D, D_EXPERT), D),
        "w_exp_down": nrm(ks[22], (L, N_EXPERTS, D_EXPERT, D), D_EXPERT),
        "g_ple": gain(ks[23], (L, D)),
        "w_ple_gate": nrm(ks[24], (L, D, D), D),
        "b_ple_gate": bias(ks[25], (L, D)),
        "w_ple_up": nrm(ks[26], (L, PLE_DIM, D), PLE_DIM),
        "g_final": gain(ks[27], (D,)),
    }


def reference(x, p, g_mix, w_in, w_pool, pool_scale, w_branch_a, sgu_ln_g, sgu_ln_b, w_spatial,
              b_spatial, w_branch_b, w_merge_gate, b_merge_gate, w_out, g_ffn, w_router_group,
              b_router_group, w_router_expert, b_router_expert, w_exp_gate, w_exp_up, w_exp_down,
              g_ple, w_ple_gate, b_ple_gate, w_ple_up, g_final):
    for i in range(DEPTH):
        h = _rmsnorm(x, g_mix[i])
        z = h @ w_in[i]
        a_in = z[..., :POOL_WIDTH]
        u = z[..., POOL_WIDTH:POOL_WIDTH + SGU_WIDTH]
        v = z[..., POOL_WIDTH + SGU_WIDTH:]
        y_a = _pool_mixer(a_in, w_pool[i], pool_scale[i]) @ w_branch_a[i]
        y_b = _spatial_gating(u, v, sgu_ln_g[i], sgu_ln_b[i], w_spatial[i], b_spatial[i]) @ w_branch_b[i]
        gates = jax.nn.sigmoid(h @ w_merge_gate[i] + b_merge_gate[i])
        merged = gates[..., :D_MODEL] * y_a + gates[..., D_MODEL:] * y_b
        x = x + merged @ w_out[i]
        x = x + _hier_moe(_rmsnorm(x, g_ffn[i]), w_router_group[i], b_router_group[i],
                          w_router_expert[i], b_router_expert[i], w_exp_gate[i], w_exp_up[i], w_exp_down[i])
        gate_p = jax.nn.sigmoid(_rmsnorm(x, g_ple[i]) @ w_ple_gate[i] + b_ple_gate[i])
        x = x + gate_p * (p[i] @ w_ple_up[i])
    return _rmsnorm(x, g_final)
```

```python
import numpy as np
import concourse.bass as bass
import concourse.mybir as mybir
from concourse.bass_utils import run_bass_kernel_spmd

F32 = mybir.dt.float32
BF16 = mybir.dt.bfloat16
I32 = mybir.dt.int32
AF = mybir.ActivationFunctionType
ALU = mybir.AluOpType
AX = mybir.AxisListType

NCORES = 8
T = 2048
D = 2048
NT = T // 128
TB = 512
NB = T // TB
JT = TB // 128
HALO = 16
CAP = 256
NE = 32
EPS = 1e-6
BIG = float(1 << 20)
EPOCH = 6000
SAME_ENGINE_SYNC = True
DEBUG_X1 = False

CV_GMIX, CV_GFFN, CV_GPLE, CV_PSC, CV_BMG, CV_N = 0, 16, 32, 48, 56, 88
R_LNG, R_LNB, R_BSP, R_RB, R_GFIN, R_BPG, R_POS, R_N = 0, 1024, 2048, 3072, 3108, 5156, 7204, 7220


class Tok:
    __slots__ = ("w", "r", "name", "dkey", "dval")

    def __init__(self, name, init=None):
        self.name = name
        self.w = dict(init) if init else {}
        self.r = {}


class Ctx:
    def __init__(self):
        self.nc = bass.Bass("TRN2", target_bir_lowering=False)
        nc = self.nc
        self.eng = dict(pe=nc.tensor, act=nc.scalar, dve=nc.vector, pool=nc.gpsimd, sp=nc.sync)
        self.semh = {}
        self.cur = {}
        self.cnt = {}
        self.known = {e: {} for e in self.eng}
        self.nsem = 0
        for e in self.eng:
            self._new_epoch(e, 0)
        self.dma_sems = []

    def _sem(self, name):
        self.nsem += 1
        return self.nc.semaphore(name).__enter__()

    def _new_epoch(self, e, ep):
        key = (e, ep)
        self.semh[key] = self._sem(f"s_{e}_{ep}")
        self.cur[e] = key
        self.cnt[e] = 0

    def tok(self, name, init=None):
        return Tok(name, init)

    def dma_tok(self, name, init=None):
        t = Tok(name, init)
        key = ("dma", name)
        self.semh[key] = self._sem(f"d_{name}")
        t.dkey = key
        t.dval = 0
        return t

    def snapshot(self):
        d = {}
        for e in self.eng:
            if self.cnt[e] > 0:
                d[self.cur[e]] = self.cnt[e]
            elif self.cur[e][1] > 0:
                d[(e, self.cur[e][1] - 1)] = EPOCH
        for t in self.dma_sems:
            if t.dval > 0:
                d[t.dkey] = t.dval
        return d

    def _wait(self, e, deps):
        eng = self.eng[e]
        kn = self.known[e]
        for key, val in deps.items():
            if kn.get(key, 0) >= val:
                continue
            if key[0] == e and not SAME_ENGINE_SYNC:
                continue
            if key[0] == e and e in ("pe",):
                continue
            eng.wait_ge(self.semh[key], val)
            kn[key] = val

    @staticmethod
    def _merge(d, src):
        for k, v in src.items():
            if d.get(k, 0) < v:
                d[k] = v

    def op(self, e, fn, reads=(), writes=()):
        deps = {}
        for t in reads:
            self._merge(deps, t.w)
        for t in writes:
            self._merge(deps, t.w)
            self._merge(deps, t.r)
        self._wait(e, deps)
        ins = fn(self.eng[e])
        if self.cnt[e] >= EPOCH:
            self._new_epoch(e, self.cur[e][1] + 1)
        self.cnt[e] += 1
        key, val = self.cur[e], self.cnt[e]
        ins.then_inc(self.semh[key], 1)
        for t in reads:
            t.r[key] = val
        for t in writes:
            t.w = {key: val}
            t.r = {}
        return ins

    def dma(self, e, out, in_, reads=(), writes=(), dtok=None, **kw):
        deps = {}
        for t in reads:
            self._merge(deps, t.w)
        for t in writes:
            self._merge(deps, t.w)
            self._merge(deps, t.r)
        self._wait(e, deps)
        if dtok is None:
            dtok = writes[0]
        if dtok not in self.dma_sems:
            self.dma_sems.append(dtok)
        eng = self.eng[e]
        if "indirect" in kw:
            kw = dict(kw)
            kw.pop("indirect")
            ins = eng.indirect_dma_start(out=out, in_=in_, **kw)
        else:
            ins = eng.dma_start(out=out, in_=in_, **kw)
        dtok.dval += 16
        ins.then_inc(self.semh[dtok.dkey], 16)
        key, val = dtok.dkey, dtok.dval
        for t in reads:
            t.r[key] = val
        for t in writes:
            if t is dtok:
                t.w = {key: val}
                t.r = {}
            else:
                t.w = {key: val}
                t.r = {}
        return ins


def build(stage=2):
    cx = Ctx()
    nc = cx.nc
    op, dma = cx.op, cx.dma

    def din(name, shape, dt=F32):
        return nc.dram_tensor(name, list(shape), dt, kind="ExternalInput").ap()

    x_d = din("x", [T, D])
    xh_d = din("xh", [HALO, D])
    p_d = din("p", [T, 256])
    cvec_d = din("cvec", [128, CV_N])
    wr_d = din("wr", [128, 16 * 36])
    rows_d = din("rows", [1, R_N])
    w_in_d = din("w_in", [D, 3072])
    w_pool_d = din("w_pool", [4, 256, 256])
    w_ba_d = din("w_ba", [1024, D])
    w_bb_d = din("w_bb", [1024, D])
    w_sp_d = din("w_sp", [8, 128, 128])
    w_mg_d = din("w_mg", [D, 2 * D])
    w_out_d = din("w_out", [D, D])
    w_eg_d = din("w_eg", [NE, D, 512])
    w_eu_d = din("w_eu", [NE, D, 512])
    w_ed_d = din("w_ed", [NE, 512, D])
    w_pg_d = din("w_pg", [D, D])
    w_pu_d = din("w_pu", [256, D])
    y_d = nc.dram_tensor("y", [T, D], F32, kind="ExternalOutput").ap()
    xs_d = nc.dram_tensor("xs_scr", [NE * CAP, D], BF16).ap()
    ys_d = nc.dram_tensor("ys_scr", [NE * CAP, D], F32).ap()
    x1_d = nc.dram_tensor("x1_scr", [T, D], F32, kind=("ExternalOutput" if DEBUG_X1 else "Internal")).ap()

    def sb(name, shape, dt):
        return nc.alloc_sbuf_tensor("sb_" + name, list(shape), dt)

    bc_reg = nc.gpsimd.to_reg(NE * CAP - 1)
    NPAGE = 9
    pages = [sb(f"pg{i}", [128, 8192], BF16) for i in range(NPAGE)]
    aT = sb("aT", [128, 8, HALO + TB], BF16)
    xn = [sb(f"xn{i}", [128, D], BF16) for i in range(2)]
    scr = sb("scr", [128, 4 * 1088], F32)
    ident_b = sb("ident_b", [128, 128], BF16)
    ident_f = sb("ident_f", [128, 128], F32)
    ltri_f = sb("ltri_f", [128, 128], F32)
    ones_f = sb("ones_f", [128, 128], F32)
    ones_b = sb("ones_b", [1, 128], BF16)
    cvec = sb("cvec", [128, CV_N], F32)
    wr_sb = sb("wr_sb", [128, 16, 36], F32)
    wsT = sb("wsT", [128, 8, 128], BF16)
    bspB = sb("bspB", [128, 8, 128], F32)
    wpool = sb("wpool", [128, 4, 2, 256], BF16)
    lnG = sb("lnG", [128, 1024], F32)
    lnB = sb("lnB", [128, 1024], F32)
    rbB = sb("rbB", [128, 36], F32)
    corr = sb("corr", [128, 4, HALO], F32)
    offs = sb("offs", [128, NE], F32)
    offs_i = sb("offs_i", [128, NE], I32)
    msum = sb("msum", [128, NE], F32)
    wt_all = sb("wt_all", [128, NT, 2], F32)
    idx_all = sb("idx_all", [128, NT, 2], I32)
    sm = sb("sm", [128, 336], F32)
    posb = sb("posb", [128, HALO], F32)
    epsb = sb("epsb", [128, 1], F32)

    psb = [nc.alloc_psum_tensor(f"ps{i}", [128, 512], F32) for i in range(8)]
    ps_tok = [cx.tok(f"ps{i}") for i in range(8)]
    ps_i = [0]

    def bank():
        i = ps_i[0] % 8
        ps_i[0] += 1
        return psb[i], ps_tok[i]

    t_const = cx.dma_tok("const")
    t_cst2 = cx.tok("cst2")

    dma("sp", cvec[:, :], cvec_d[:, :], writes=[t_const])
    dma("sp", wr_sb[:, :, :], wr_d[:, :].rearrange("p (k n) -> p k n", k=16), writes=[t_const])
    dma("sp", lnG[:, :], rows_d[0:1, R_LNG:R_LNG + 1024].partition_broadcast(128), writes=[t_const])
    dma("sp", lnB[:, :], rows_d[0:1, R_LNB:R_LNB + 1024].partition_broadcast(128), writes=[t_const])
    dma("sp", bspB[:, :, :], rows_d[0:1, R_BSP:R_BSP + 1024].partition_broadcast(128), writes=[t_const])
    dma("sp", rbB[:, :], rows_d[0:1, R_RB:R_RB + 36].partition_broadcast(128), writes=[t_const])
    dma("sp", posb[:, :], rows_d[0:1, R_POS:R_POS + HALO].partition_broadcast(128), writes=[t_const])
    dma("pool", wpool[:, :, :, :], w_pool_d.rearrange("g (k p) n -> p g k n", p=128), writes=[t_const])

    op("pool", lambda g: g.memset(ones_f[:, :], 1.0), writes=[t_cst2])
    op("pool", lambda g: g.memset(ident_f[:, :], 1.0), writes=[t_cst2])
    op("pool", lambda g: g.affine_select(out=ident_f[:, :], in_=ident_f[:, :], pattern=[[1, 128]],
                                         compare_op=ALU.is_equal, fill=0.0, base=0, channel_multiplier=-1),
       writes=[t_cst2])
    op("pool", lambda g: g.memset(ltri_f[:, :], 1.0), writes=[t_cst2])
    op("pool", lambda g: g.affine_select(out=ltri_f[:, :], in_=ltri_f[:, :], pattern=[[1, 128]],
                                         compare_op=ALU.is_ge, fill=0.0, base=0, channel_multiplier=-1),
       writes=[t_cst2])
    op("pool", lambda g: g.iota(offs_i[:, :], pattern=[[CAP, NE]], base=-1, channel_multiplier=0), writes=[t_cst2])
    op("dve", lambda v: v.tensor_copy(out=ident_b[:, :], in_=ident_f[:, :]), reads=[t_cst2], writes=[t_cst2])
    op("dve", lambda v: v.tensor_copy(out=offs[:, :], in_=offs_i[:, :]), reads=[t_cst2], writes=[t_cst2])
    op("dve", lambda v: v.memset(ones_b[:, :], 1.0), writes=[t_cst2])
    op("dve", lambda v: v.memset(msum[:, :], 0.0), writes=[t_cst2])
    op("dve", lambda v: v.memset(epsb[:, :], EPS), writes=[t_cst2])
    for gi, w in enumerate((2, 4, 8, 16)):
        op("dve", lambda v, gi=gi, w=w: v.tensor_scalar(out=corr[:, gi, :], in0=posb[:, :], scalar1=float(w),
                                                        scalar2=None, op0=ALU.min),
           reads=[t_const], writes=[t_cst2])
        op("dve", lambda v, gi=gi: v.reciprocal(out=corr[:, gi, :], in_=corr[:, gi, :]), reads=[t_cst2], writes=[t_cst2])
        op("dve", lambda v, gi=gi, w=w: v.tensor_scalar(out=corr[:, gi, :], in0=corr[:, gi, :], scalar1=float(w),
                                                        scalar2=None, op0=ALU.mult),
           reads=[t_cst2], writes=[t_cst2])
    t_wsl = cx.dma_tok("wsl")
    for g8 in range(8):
        stg = scr[:, 0:128]
        dma("sp", stg, w_sp_d[g8], writes=[t_wsl])
        pb, pt = bank()
        op("pe", lambda pe, pb=pb: pe.transpose(out=pb[:, 0:128], in_=scr[:, 0:128], identity=ident_f[:, :]),
           reads=[t_wsl, t_cst2], writes=[pt])
        op("dve", lambda v, pb=pb, g8=g8: v.tensor_copy(out=wsT[:, g8, :], in_=pb[:, 0:128]), reads=[pt], writes=[t_cst2])
        t_wsl.r.update(pt.w)
    op("dve", lambda v: v.memset(wsT[64:128, :, 0:64], 0.0), reads=[t_cst2], writes=[t_cst2])
    CONST = [t_const, t_cst2]

    NSLOT_MIX = 3
    slot_pages = [0, 1, 2]
    slot_tok = {i: cx.dma_tok(f"wslot{i}") for i in range(NPAGE)}

    class WStream:
        def __init__(self):
            self.units = []
            self.issued = 0
            self.slots = list(slot_pages)

        def add(self, parts):
            self.units.append(parts)
            return len(self.units) - 1

        def slot_of(self, i):
            return self.units[i][0]

        def ensure(self, upto):
            while self.issued <= min(upto, len(self.units) - 1):
                pg_i, parts = self.units[self.issued]
                for dst, src in parts:
                    dma("pool", dst, src, writes=[slot_tok[pg_i]])
                self.issued += 1

    ws = WStream()

    def wview(pg_i, kc, n):
        return pages[pg_i][:, 0:kc * n].rearrange("p (k n) -> p k n", k=kc)

    def wsrc(w_ap, r0, nrows, c0, ncols):
        return w_ap[r0:r0 + nrows, c0:c0 + ncols].rearrange("(k p) n -> p k n", p=128)

    unit_seq = []
    slot_rr = [0]

    def next_slot(slots):
        s = slots[slot_rr[0] % len(slots)]
        slot_rr[0] += 1
        return s

    mix_slots = [0, 1, 2]

    def sched_unit(parts_fn, slots):
        s = next_slot(slots)
        return ws.add((s, parts_fn(s)))

    Xv = [pages[3 + (j // 2)][:, (j % 2) * 4096:(j % 2 + 1) * 4096].bitcast(F32) for j in range(4)]
    X_tok = [cx.dma_tok(f"X{j}") for j in range(4)]
    hT = pages[5][:, :].rearrange("p (k t) -> p k t", k=16)
    hT_tok = cx.tok("hT")
    mT = pages[6][:, :].rearrange("p (k t) -> p k t", k=16)
    mT_tok = cx.tok("mT")
    uT = pages[7][:, 0:4096].rearrange("p (k t) -> p k t", k=8)
    uT_tok = cx.tok("uT")
    vT = pages[7][:, 4096:8192].rearrange("p (j c) -> p j c", j=JT)
    vT_tok = cx.tok("vT")
    pzT = pages[8][:, 0:4096].rearrange("p (k t) -> p k t", k=8)
    pz_tok = cx.tok("pzT")
    ypT = pages[8][:, 4096:8192].rearrange("p (k t) -> p k t", k=8)
    yp_tok = cx.tok("ypT")
    aT_tok = cx.tok("aT")
    xn_tok = [cx.dma_tok(f"xn{i}") for i in range(2)]
    scr_tok = [cx.tok(f"scr{i}") for i in range(4)]
    sm_tok = cx.tok("sm")
    rt_tok = cx.tok("route")
    xs_tok = cx.dma_tok("xs")
    x1s_tok = [cx.dma_tok(f"x1s{i}") for i in range(NT)]
    hh_tok = cx.tok("hhalo")
    hhalo = sb("hhalo", [128, 16, HALO], BF16)

    def scrv(i, n=1088):
        return scr[:, i * 1088:i * 1088 + n]

    plan = []
    for b in range(NB):
        blk = {}
        blk["win"] = [sched_unit(lambda s, cb=cb: [(wview(s, 16, 512), wsrc(w_in_d, 0, D, cb * 512, 512))], mix_slots)
                      for cb in range(6)]
        blk["mrg"] = []
        for cb in range(4):
            u_g1 = sched_unit(lambda s, cb=cb: [(wview(s, 16, 512), wsrc(w_mg_d, 0, D, cb * 512, 512))], mix_slots)
            u_g2 = sched_unit(lambda s, cb=cb: [(wview(s, 16, 512), wsrc(w_mg_d, 0, D, D + cb * 512, 512))], mix_slots)
            u_ab = sched_unit(lambda s, cb=cb: [
                (pages[s][:, 0:4096].rearrange("p (k n) -> p k n", k=8), wsrc(w_ba_d, 0, 1024, cb * 512, 512)),
                (pages[s][:, 4096:8192].rearrange("p (k n) -> p k n", k=8), wsrc(w_bb_d, 0, 1024, cb * 512, 512))],
                mix_slots)
            blk["mrg"].append((u_ab, u_g1, u_g2))
        blk["wout"] = [sched_unit(lambda s, cb=cb: [(wview(s, 16, 512), wsrc(w_out_d, 0, D, cb * 512, 512))], mix_slots)
                       for cb in range(4)]
        plan.append(blk)
    n_mix_units = len(ws.units)

    def wget(u):
        ws.ensure(u)
        s = ws.units[u][0]
        return s, slot_tok[s]

    def wdone(u, nslots=3):
        ws.ensure(u + nslots)

    def rms_rstd(e_src, src_ap, src_toks, junk_ap, junk_toks, col, npart=128):
        sc = sm[0:npart, col:col + 1]
        op("act", lambda a: a.activation(out=junk_ap, in_=src_ap, func=AF.Square, accum_out=sc),
           reads=src_toks, writes=junk_toks + [sm_tok])
        op("act", lambda a: a.activation(out=sc, in_=sc, func=AF.Sqrt, scale=1.0 / D, bias=epsb[0:npart, :]),
           reads=[sm_tok] + CONST, writes=[sm_tok])
        op("dve", lambda v: v.reciprocal(out=sc, in_=sc), reads=[sm_tok], writes=[sm_tok])

    def transposes_bf(src_rows_ap, src_toks, dst_fn, dst_toks, gcol, evac_rr):
        for q in range(4):
            pb, pt = bank()
            pbb = pb[:, :].bitcast(BF16)
            for kk in range(4):
                k = q * 4 + kk
                op("pe", lambda pe, k=k, kk=kk, pbb=pbb: pe.transpose(out=pbb[:, kk * 128:(kk + 1) * 128],
                                                                       in_=src_rows_ap[:, k * 128:(k + 1) * 128],
                                                                       identity=ident_b[:, :]),
                   reads=src_toks + CONST, writes=[pt])
            for kk in range(4):
                k = q * 4 + kk
                if (evac_rr[0] % 2) == 0:
                    op("act", lambda a, k=k, kk=kk, pbb=pbb: a.activation(out=dst_fn(k), in_=pbb[:, kk * 128:(kk + 1) * 128],
                                                                          func=AF.Copy, scale=cvec[:, gcol + k:gcol + k + 1]),
                       reads=[pt] + CONST, writes=dst_toks)
                else:
                    op("dve", lambda v, k=k, kk=kk, pbb=pbb: v.tensor_scalar(out=dst_fn(k), in0=pbb[:, kk * 128:(kk + 1) * 128],
                                                                             scalar1=cvec[:, gcol + k:gcol + k + 1], scalar2=None,
                                                                             op0=ALU.mult),
                       reads=[pt] + CONST, writes=dst_toks)
                evac_rr[0] += 1

    evac_rr = [0]

    for b in range(NB):
        blk = plan[b]
        t0 = b * TB
        if b == 0:
            dma("sp", Xv[0][0:HALO, :], xh_d[:, :], writes=[X_tok[0]])
            rms_rstd("act", Xv[0][0:HALO, :], [X_tok[0]], xn[0][0:HALO, :], [xn_tok[0]], 0, npart=HALO)
            op("act", lambda a: a.activation(out=xn[0][0:HALO, :], in_=Xv[0][0:HALO, :], func=AF.Copy, scale=sm[0:HALO, 0:1]),
               reads=[X_tok[0], sm_tok], writes=[xn_tok[0]])
            for q in range(4):
                pb, pt = bank()
                pbb = pb[:, :].bitcast(BF16)
                for kk in range(4):
                    k = q * 4 + kk
                    op("pe", lambda pe, k=k, kk=kk, pbb=pbb: pe.transpose(out=pbb[:, kk * HALO:(kk + 1) * HALO],
                                                                           in_=xn[0][0:HALO, k * 128:(k + 1) * 128],
                                                                           identity=ident_b[0:HALO, 0:HALO]),
                       reads=[xn_tok[0]] + CONST, writes=[pt])
                for kk in range(4):
                    k = q * 4 + kk
                    op("dve", lambda v, k=k, kk=kk, pbb=pbb: v.tensor_scalar(out=hhalo[:, k, :], in0=pbb[:, kk * HALO:(kk + 1) * HALO],
                                                                             scalar1=cvec[:, CV_GMIX + k:CV_GMIX + k + 1],
                                                                             scalar2=None, op0=ALU.mult),
                       reads=[pt] + CONST, writes=[hh_tok])
        for j in range(JT):
            dma("sp", Xv[j], x_d[t0 + j * 128:t0 + (j + 1) * 128, :], writes=[X_tok[j]])
        for j in range(JT):
            xb = xn[j % 2]
            xt = xn_tok[j % 2]
            rms_rstd("act", Xv[j], [X_tok[j]], xb[:, :], [xt], j)
            op("act", lambda a, j=j, xb=xb: a.activation(out=xb[:, :], in_=Xv[j], func=AF.Copy, scale=sm[:, j:j + 1]),
               reads=[X_tok[j], sm_tok], writes=[xt])
            transposes_bf(xb, [xt], lambda k, j=j: hT[:, k, j * 128:(j + 1) * 128], [hT_tok], CV_GMIX, evac_rr)

        if b > 0:
            op("dve", lambda v: v.tensor_copy(out=aT[:, :, 0:HALO], in_=aT[:, :, TB:TB + HALO]), reads=[aT_tok], writes=[aT_tok])
        for cb in range(6):
            if cb > 0:
                wdone(blk["win"][cb - 1])
            s, st = wget(blk["win"][cb])
            wv = wview(s, 16, 512)
            if cb < 4:
                for mm in range(4):
                    m = (cb % 2) * 4 + mm
                    pb, pt = bank()
                    for k in range(16):
                        op("pe", lambda pe, k=k, mm=mm, pb=pb, wv=wv: pe.matmul(pb[:, :], lhsT=wv[:, k, mm * 128:(mm + 1) * 128],
                                                                               rhs=hT[:, k, :], start=(k == 0), stop=(k == 15)),
                           reads=[st, hT_tok], writes=[pt])
                    if cb < 2:
                        op("dve", lambda v, m=m, pb=pb: v.tensor_copy(out=aT[:, m, HALO:HALO + TB], in_=pb[:, :]),
                           reads=[pt], writes=[aT_tok])
                        if b == 0:
                            pb2, pt2 = bank()
                            for k in range(16):
                                op("pe", lambda pe, k=k, mm=mm, pb2=pb2, wv=wv: pe.matmul(pb2[:, 0:HALO], lhsT=wv[:, k, mm * 128:(mm + 1) * 128],
                                                                                         rhs=hhalo[:, k, :], start=(k == 0), stop=(k == 15)),
                                   reads=[st, hh_tok], writes=[pt2])
                            op("dve", lambda v, m=m, pb2=pb2: v.tensor_copy(out=aT[:, m, 0:HALO], in_=pb2[:, 0:HALO]),
                               reads=[pt2], writes=[aT_tok])
                    else:
                        op("act", lambda a, m=m, pb=pb: a.activation(out=uT[:, m, :], in_=pb[:, :], func=AF.Gelu_apprx_tanh),
                           reads=[pt], writes=[uT_tok])
            else:
                half = cb - 4
                for j in range(JT):
                    pb, pt = bank()
                    for k in range(16):
                        op("pe", lambda pe, k=k, j=j, pb=pb, wv=wv: pe.matmul(pb[:, :], lhsT=hT[:, k, j * 128:(j + 1) * 128],
                                                                             rhs=wv[:, k, :], start=(k == 0), stop=(k == 15)),
                           reads=[st, hT_tok], writes=[pt])
                    op("act", lambda a, j=j, half=half, pb=pb: a.activation(out=scrv(j)[:, half * 512:(half + 1) * 512], in_=pb[:, :],
                                                                            func=AF.Gelu_apprx_tanh),
                       reads=[pt], writes=[scr_tok[j]])
        wdone(blk["win"][5])
        for j in range(JT):
            vg = scrv(j, 1024)
            st6 = sm[:, 16:28]
            op("dve", lambda v, vg=vg: v.bn_stats(out=sm[:, 16:22], in_=vg[:, 0:512]), reads=[scr_tok[j]], writes=[sm_tok])
            op("dve", lambda v, vg=vg: v.bn_stats(out=sm[:, 22:28], in_=vg[:, 512:1024]), reads=[scr_tok[j], sm_tok], writes=[sm_tok])
            op("dve", lambda v: v.bn_aggr(out=sm[:, 28:30], in_=sm[:, 16:28]), reads=[sm_tok], writes=[sm_tok])
            op("act", lambda a: a.activation(out=sm[:, 29:30], in_=sm[:, 29:30], func=AF.Sqrt, scale=1.0, bias=epsb[:, :]),
               reads=[sm_tok] + CONST, writes=[sm_tok])
            op("dve", lambda v: v.reciprocal(out=sm[:, 29:30], in_=sm[:, 29:30]), reads=[sm_tok], writes=[sm_tok])
            op("dve", lambda v, vg=vg: v.scalar_tensor_tensor(out=vg, in0=vg, scalar=sm[:, 28:29], in1=lnG[:, :],
                                                              op0=ALU.subtract, op1=ALU.mult),
               reads=[scr_tok[j], sm_tok] + CONST, writes=[scr_tok[j]])
            op("dve", lambda v, vg=vg, j=j: v.scalar_tensor_tensor(out=vT[:, j, :], in0=vg, scalar=sm[:, 29:30], in1=lnB[:, :],
                                                                   op0=ALU.mult, op1=ALU.add),
               reads=[scr_tok[j], sm_tok] + CONST, writes=[vT_tok])

        W = HALO + TB
        for gi, w in enumerate((2, 4, 8, 16)):
            ca = aT[:, 2 * gi:2 * gi + 2, :]
            bufA = scrv(0, 2 * W).rearrange("p (k t) -> p k t", k=2)
            bufB = scrv(2, 2 * W).rearrange("p (k t) -> p k t", k=2)
            cur, cur_t = ca, aT_tok
            sh = 1
            lvl = 0
            while sh < w:
                dst = bufA if lvl % 2 == 0 else bufB
                dst_t = scr_tok[0] if lvl % 2 == 0 else scr_tok[2]
                lo = 2 * sh - 1
                op("dve", lambda v, dst=dst, cur=cur, lo=lo, sh=sh: v.tensor_tensor(out=dst[:, :, lo:W], in0=cur[:, :, lo:W],
                                                                                     in1=cur[:, :, lo - sh:W - sh], op=ALU.add),
                   reads=[cur_t], writes=[dst_t])
                cur, cur_t = dst, dst_t
                sh *= 2
                lvl += 1
            if b == 0:
                op("dve", lambda v, cur=cur, gi=gi: v.tensor_tensor(out=cur[:, 0, HALO:2 * HALO], in0=cur[:, 0, HALO:2 * HALO],
                                                                   in1=corr[:, gi, :], op=ALU.mult),
                   reads=[cur_t] + CONST, writes=[cur_t])
                op("dve", lambda v, cur=cur, gi=gi: v.tensor_tensor(out=cur[:, 1, HALO:2 * HALO], in0=cur[:, 1, HALO:2 * HALO],
                                                                   in1=corr[:, gi, :], op=ALU.mult),
                   reads=[cur_t] + CONST, writes=[cur_t])
            op("dve", lambda v, cur=cur, ca=ca, gi=gi, w=w: v.scalar_tensor_tensor(out=pzT[:, 2 * gi:2 * gi + 2, :], in0=cur[:, :, HALO:W],
                                                                                   scalar=1.0 / w, in1=ca[:, :, HALO:W],
                                                                                   op0=ALU.mult, op1=ALU.subtract),
               reads=[cur_t, aT_tok], writes=[pz_tok])
            for mo in range(2):
                pb, pt = bank()
                for ki in range(2):
                    op("pe", lambda pe, gi=gi, mo=mo, ki=ki, pb=pb: pe.matmul(pb[:, :], lhsT=wpool[:, gi, ki, mo * 128:(mo + 1) * 128],
                                                                             rhs=pzT[:, 2 * gi + ki, :], start=(ki == 0), stop=(ki == 1)),
                       reads=[pz_tok] + CONST, writes=[pt])
                c = 2 * gi + mo
                op("act", lambda a, c=c, pb=pb: a.activation(out=ypT[:, c, :], in_=pb[:, :], func=AF.Copy,
                                                             scale=cvec[:, CV_PSC + c:CV_PSC + c + 1]),
                   reads=[pt] + CONST, writes=[yp_tok])
        for g8 in range(8):
            pb, pt = bank()
            for j in range(JT):
                op("pe", lambda pe, g8=g8, j=j, pb=pb: pe.matmul(pb[:, j * 128:(j + 1) * 128], lhsT=vT[:, j, g8 * 128:(g8 + 1) * 128],
                                                                 rhs=wsT[:, g8, :], start=True, stop=True),
                   reads=[vT_tok] + CONST, writes=[pt])
            tmp = scrv(1, 512)
            for j in range(JT):
                op("dve", lambda v, g8=g8, j=j, pb=pb, tmp=tmp: v.tensor_tensor(out=tmp[:, j * 128:(j + 1) * 128], in0=pb[:, j * 128:(j + 1) * 128],
                                                                               in1=bspB[:, g8, :], op=ALU.add),
                   reads=[pt] + CONST, writes=[scr_tok[1]])
            op("dve", lambda v, g8=g8, tmp=tmp: v.tensor_tensor(out=uT[:, g8, :], in0=uT[:, g8, :], in1=tmp, op=ALU.mult),
               reads=[scr_tok[1], uT_tok], writes=[uT_tok])

        for cb in range(4):
            u_ab, u_g1, u_g2 = blk["mrg"][cb]
            for gi2, ug_ in enumerate((u_g1, u_g2)):
                s_g, t_g = wget(ug_)
                wgm = wview(s_g, 16, 512)
                for mm in range(4):
                    m = cb * 4 + mm
                    p1, p1t = bank()
                    for k in range(16):
                        op("pe", lambda pe, k=k, mm=mm, p1=p1, wgm=wgm: pe.matmul(p1[:, :], lhsT=wgm[:, k, mm * 128:(mm + 1) * 128], rhs=hT[:, k, :],
                                                                                 start=(k == 0), stop=(k == 15)),
                           reads=[t_g, hT_tok], writes=[p1t])
                    sgt = scrv(mm)[:, gi2 * 512:(gi2 + 1) * 512]
                    op("act", lambda a, m=m, p1=p1, sgt=sgt, gi2=gi2: a.activation(out=sgt, in_=p1[:, :], func=AF.Sigmoid,
                                                                                    bias=cvec[:, CV_BMG + 16 * gi2 + m:CV_BMG + 16 * gi2 + m + 1], scale=1.0),
                       reads=[p1t] + CONST, writes=[scr_tok[mm]])
                wdone(ug_)
            s_ab, t_ab = wget(u_ab)
            wa = pages[s_ab][:, 0:4096].rearrange("p (k n) -> p k n", k=8)
            wb = pages[s_ab][:, 4096:8192].rearrange("p (k n) -> p k n", k=8)
            for mm in range(4):
                m = cb * 4 + mm
                pa, pat = bank()
                for k in range(8):
                    op("pe", lambda pe, k=k, mm=mm, pa=pa, wa=wa: pe.matmul(pa[:, :], lhsT=wa[:, k, mm * 128:(mm + 1) * 128], rhs=ypT[:, k, :],
                                                                           start=(k == 0), stop=(k == 7)),
                       reads=[t_ab, yp_tok], writes=[pat])
                pbk, pbt = bank()
                for k in range(8):
                    op("pe", lambda pe, k=k, mm=mm, pbk=pbk, wb=wb: pe.matmul(pbk[:, :], lhsT=wb[:, k, mm * 128:(mm + 1) * 128], rhs=uT[:, k, :],
                                                                             start=(k == 0), stop=(k == 7)),
                       reads=[t_ab, uT_tok], writes=[pbt])
                s1 = scrv(mm)[:, 0:512]
                s2 = scrv(mm)[:, 512:1024]
                op("dve", lambda v, pa=pa, s1=s1: v.tensor_tensor(out=s1, in0=s1, in1=pa[:, :], op=ALU.mult),
                   reads=[pat, scr_tok[mm]], writes=[scr_tok[mm]])
                op("dve", lambda v, pbk=pbk, s2=s2: v.tensor_tensor(out=s2, in0=s2, in1=pbk[:, :], op=ALU.mult),
                   reads=[pbt, scr_tok[mm]], writes=[scr_tok[mm]])
                op("dve", lambda v, m=m, s1=s1, s2=s2: v.tensor_tensor(out=mT[:, m, :], in0=s1, in1=s2, op=ALU.add),
                   reads=[scr_tok[mm]], writes=[mT_tok])
            wdone(u_ab)

        for cb in range(4):
            s, st = wget(blk["wout"][cb])
            wv = wview(s, 16, 512)
            for j in range(JT):
                pb, pt = bank()
                for k in range(16):
                    op("pe", lambda pe, k=k, j=j, pb=pb, wv=wv: pe.matmul(pb[:, :], lhsT=mT[:, k, j * 128:(j + 1) * 128], rhs=wv[:, k, :],
                                                                         start=(k == 0), stop=(k == 15)),
                       reads=[st, mT_tok], writes=[pt])
                op("dve", lambda v, j=j, cb=cb, pb=pb: v.tensor_tensor(out=Xv[j][:, cb * 512:(cb + 1) * 512], in0=Xv[j][:, cb * 512:(cb + 1) * 512],
                                                                      in1=pb[:, :], op=ALU.add),
                   reads=[pt, X_tok[j]], writes=[X_tok[j]])
            wdone(blk["wout"][cb])
        for j in range(JT):
            ti = b * JT + j
            if stage == 1:
                dma("sp", y_d[ti * 128:(ti + 1) * 128, :], Xv[j], reads=[X_tok[j]], writes=[x1s_tok[ti]])
                continue
            dma("sp", x1_d[ti * 128:(ti + 1) * 128, :], Xv[j], reads=[X_tok[j]], writes=[x1s_tok[ti]])
            xb = xn[j % 2]
            xt = xn_tok[j % 2]
            rms_rstd("act", Xv[j], [X_tok[j]], xb[:, :], [xt], 8)
            op("act", lambda a, j=j, xb=xb: a.activation(out=xb[:, :], in_=Xv[j], func=AF.Copy, scale=sm[:, 8:9]),
               reads=[X_tok[j], sm_tok], writes=[xt])
            x1T = scr[:, 0:2048].rearrange("p (k t) -> p k t", k=16)
            for q in range(4):
                pb, pt = bank()
                for kk in range(4):
                    k = q * 4 + kk
                    op("pe", lambda pe, k=k, kk=kk, pb=pb, j=j: pe.transpose(out=pb[:, kk * 128:(kk + 1) * 128], in_=Xv[j][:, k * 128:(k + 1) * 128],
                                                                            identity=ident_f[:, :]),
                       reads=[X_tok[j]] + CONST, writes=[pt])
                for kk in range(4):
                    k = q * 4 + kk
                    op("dve", lambda v, k=k, kk=kk, pb=pb: v.tensor_scalar(out=x1T[:, k, :], in0=pb[:, kk * 128:(kk + 1) * 128],
                                                                          scalar1=cvec[:, CV_GFFN + k:CV_GFFN + k + 1], scalar2=None, op0=ALU.mult),
                       reads=[pt] + CONST, writes=[scr_tok[0], scr_tok[1]])
            pl, plt = bank()
            for k in range(16):
                op("pe", lambda pe, k=k, pl=pl: pe.matmul(pl[:, 0:36], lhsT=x1T[:, k, :], rhs=wr_sb[:, k, :], start=(k == 0), stop=(k == 15)),
                   reads=[scr_tok[0], scr_tok[1]] + CONST, writes=[plt])
            LG = sm[:, 32:68]
            GL = sm[:, 32:36]
            EL = sm[:, 36:68]
            GOH = sm[:, 68:72]
            PEN = sm[:, 72:76]
            EM = sm[:, 76:108]
            TOP = sm[:, 108:116]
            M0 = sm[:, 116:148]
            M1 = sm[:, 148:180]
            MM = sm[:, 180:212]
            RP = sm[:, 212:244]
            OK_ = sm[:, 244:276]
            JK = sm[:, 276:308]
            SC = sm[:, 308:324]
            GEX = sm[:, 324:328]
            R = [sm_tok]
            op("dve", lambda v, pl=pl: v.scalar_tensor_tensor(out=LG, in0=pl[:, 0:36], scalar=sm[:, 8:9], in1=rbB[:, :],
                                                              op0=ALU.mult, op1=ALU.add), reads=[plt, sm_tok] + CONST, writes=R)
            op("dve", lambda v: v.tensor_reduce(out=SC[:, 0:1], in_=GL, axis=AX.X, op=ALU.max), reads=R, writes=R)
            op("dve", lambda v: v.tensor_scalar(out=GOH, in0=GL, scalar1=SC[:, 0:1], scalar2=None, op0=ALU.is_ge), reads=R, writes=R)
            op("dve", lambda v: v.tensor_scalar(out=SC[:, 1:2], in0=SC[:, 0:1], scalar1=-1.0, scalar2=None, op0=ALU.mult), reads=R, writes=R)
            op("act", lambda a: a.activation(out=GEX, in_=GL, func=AF.Exp, bias=SC[:, 1:2], scale=1.0, accum_out=SC[:, 2:3]),
               reads=R, writes=R)
            op("dve", lambda v: v.reciprocal(out=SC[:, 3:4], in_=SC[:, 2:3]), reads=R, writes=R)
            op("dve", lambda v: v.tensor_scalar(out=PEN, in0=GOH, scalar1=1e30, scalar2=-1e30, op0=ALU.mult, op1=ALU.add), reads=R, writes=R)
            for g4 in range(4):
                op("dve", lambda v, g4=g4: v.tensor_scalar(out=EM[:, g4 * 8:(g4 + 1) * 8], in0=EL[:, g4 * 8:(g4 + 1) * 8],
                                                           scalar1=PEN[:, g4:g4 + 1], scalar2=None, op0=ALU.add), reads=R, writes=R)
            op("dve", lambda v: v.max(out=TOP, in_=EM), reads=R, writes=R)
            op("dve", lambda v: v.tensor_scalar(out=M0, in0=EM, scalar1=TOP[:, 0:1], scalar2=None, op0=ALU.is_equal), reads=R, writes=R)
            op("dve", lambda v: v.tensor_scalar(out=M1, in0=EM, scalar1=TOP[:, 1:2], scalar2=None, op0=ALU.is_equal), reads=R, writes=R)
            op("dve", lambda v: v.tensor_scalar(out=SC[:, 4:5], in0=TOP[:, 0:1], scalar1=-1.0, scalar2=None, op0=ALU.mult), reads=R, writes=R)
            op("act", lambda a: a.activation(out=SC[:, 5:6], in_=TOP[:, 1:2], func=AF.Exp, bias=SC[:, 4:5], scale=1.0), reads=R, writes=R)
            op("dve", lambda v: v.tensor_scalar(out=SC[:, 6:7], in0=SC[:, 5:6], scalar1=1.0, scalar2=None, op0=ALU.add), reads=R, writes=R)
            op("dve", lambda v: v.reciprocal(out=SC[:, 6:7], in_=SC[:, 6:7]), reads=R, writes=R)
            op("dve", lambda v: v.tensor_tensor(out=MM, in0=M0, in1=M1, op=ALU.add), reads=R, writes=R)
            pr, prt = bank()
            op("pe", lambda pe, pr=pr: pe.matmul(pr[:, 0:NE], lhsT=ltri_f[:, :], rhs=MM, start=True, stop=False),
               reads=R + CONST + [rt_tok], writes=[prt])
            op("pe", lambda pe, pr=pr: pe.matmul(pr[:, 0:NE], lhsT=ones_f[:, :], rhs=msum[:, :], start=False, stop=True),
               reads=R + CONST + [rt_tok], writes=[prt])
            op("dve", lambda v, pr=pr: v.tensor_tensor(out=RP, in0=pr[:, 0:NE], in1=offs[:, :], op=ALU.add), reads=[prt] + R + CONST, writes=R)
            op("dve", lambda v, pr=pr: v.tensor_scalar(out=OK_, in0=pr[:, 0:NE], scalar1=float(CAP), scalar2=None, op0=ALU.is_le),
               reads=[prt] + R, writes=R)
            op("dve", lambda v: v.tensor_tensor(out=msum[:, :], in0=msum[:, :], in1=MM, op=ALU.add), reads=R + [rt_tok], writes=[rt_tok])
            for kk, MK in enumerate((M0, M1)):
                op("dve", lambda v, kk=kk, MK=MK: v.tensor_tensor(out=JK, in0=MK, in1=RP, op=ALU.mult), reads=R, writes=R)
                op("dve", lambda v, kk=kk: v.tensor_reduce(out=SC[:, 7 + kk:8 + kk], in_=JK, axis=AX.X, op=ALU.add), reads=R, writes=R)
                op("dve", lambda v, kk=kk, MK=MK: v.tensor_tensor(out=JK, in0=MK, in1=OK_, op=ALU.mult), reads=R, writes=R)
                op("dve", lambda v, kk=kk: v.tensor_reduce(out=SC[:, 9 + kk:10 + kk], in_=JK, axis=AX.X, op=ALU.add), reads=R, writes=R)
                op("dve", lambda v, kk=kk: v.tensor_scalar(out=SC[:, 7 + kk:8 + kk], in0=SC[:, 7 + kk:8 + kk], scalar1=-BIG,
                                                           scalar2=SC[:, 9 + kk:10 + kk], op0=ALU.add, op1=ALU.mult), reads=R, writes=R)
                op("dve", lambda v, kk=kk: v.tensor_scalar(out=SC[:, 7 + kk:8 + kk], in0=SC[:, 7 + kk:8 + kk], scalar1=BIG,
                                                           scalar2=None, op0=ALU.add), reads=R, writes=R)
                op("dve", lambda v, kk=kk, ti=ti: v.tensor_copy(out=idx_all[:, ti, kk:kk + 1], in_=SC[:, 7 + kk:8 + kk]),
                   reads=R, writes=[rt_tok])
            op("dve", lambda v, ti=ti: v.tensor_scalar(out=wt_all[:, ti, 0:1], in0=SC[:, 3:4], scalar1=SC[:, 6:7], scalar2=SC[:, 9:10],
                                                       op0=ALU.mult, op1=ALU.mult), reads=R, writes=[rt_tok])
            op("dve", lambda v, ti=ti: v.tensor_scalar(out=SC[:, 11:12], in0=SC[:, 3:4], scalar1=SC[:, 6:7], scalar2=SC[:, 5:6],
                                                       op0=ALU.mult, op1=ALU.mult), reads=R, writes=R)
            op("dve", lambda v, ti=ti: v.tensor_scalar(out=wt_all[:, ti, 1:2], in0=SC[:, 11:12], scalar1=SC[:, 10:11], scalar2=None,
                                                       op0=ALU.mult), reads=R, writes=[rt_tok])
            for kk in range(2):
                dma("pool", xs_d[:, :], xb[:, :], reads=[xt, rt_tok], writes=[], dtok=xs_tok, indirect=True,
                    out_offset=bass.IndirectOffsetOnAxis(ap=idx_all[:, ti, kk:kk + 1], axis=0), in_offset=None,
                    bounds_check=bc_reg, oob_is_err=False)
            xs_tok.w = {xs_tok.dkey: xs_tok.dval}

    if stage == 1:
        fin = {}
        for t in x1s_tok:
            Ctx._merge(fin, t.w)
        cx._wait("sp", fin)
        return nc

    barrier = cx.snapshot()
    moe_slots = [0, 1, 2, 5, 6, 7]
    for i in (5, 6, 7):
        slot_tok[i].w = dict(barrier)
    Ysb = Xv
    Y_tok = [cx.dma_tok(f"Ysb{j}", init=barrier) for j in range(4)]
    XeT = [pages[8][:, i * 4096:(i + 1) * 4096].rearrange("p (k r) -> p k r", k=16) for i in range(2)]
    XeT_tok = [cx.tok(f"XeT{i}", init=barrier) for i in range(2)]
    hidT = [aT[:, :, :].rearrange("p k t -> p (k t)")[:, i * 1024:(i + 1) * 1024].rearrange("p (m r) -> p m r", m=4) for i in range(2)]
    hid_tok = [cx.tok(f"hid{i}", init=barrier) for i in range(2)]
    ys_tok = cx.dma_tok("ys")
    slot_rr[0] = 0
    exp_units = []
    for e in range(NE):
        ug = sched_unit(lambda s, e=e: [(wview(s, 16, 512), wsrc(w_eg_d[e], 0, D, 0, 512))], moe_slots)
        uu = sched_unit(lambda s, e=e: [(wview(s, 16, 512), wsrc(w_eu_d[e], 0, D, 0, 512))], moe_slots)
        ud = sched_unit(lambda s, e=e: [(wview(s, 4, 2048), wsrc(w_ed_d[e], 0, 512, 0, D))], moe_slots)
        exp_units.append((ug, uu, ud))

    def wget_moe(u):
        ws.ensure(u)
        s = ws.units[u][0]
        return s, slot_tok[s]

    ws.ensure(exp_units[0][0] + 5)

    yrr = [0]
    for e in range(NE):
        ug, uu, ud = exp_units[e]
        xe = XeT[e % 2]
        xet = XeT_tok[e % 2]
        for r in range(2):
            dma("sp", xn[r][:, :], xs_d[e * CAP + r * 128:e * CAP + (r + 1) * 128, :], reads=[xs_tok], writes=[xn_tok[r]])
            transposes_bf(xn[r], [xn_tok[r]], lambda k, r=r, xe=xe: xe[:, k, r * 128:(r + 1) * 128], [xet], CV_GFFN, evac_rr)
        sg_, tg_ = wget_moe(ug)
        su_, tu_ = wget_moe(uu)
        sd_, td_ = wget_moe(ud)
        wg = wview(sg_, 16, 512)
        wu = wview(su_, 16, 512)
        wd = wview(sd_, 4, 2048)
        hd = hidT[e % 2]
        hdt = hid_tok[e % 2]
        gu_banks = [bank() for _ in range(4)]
        for m in range(4):
            pb, pt = gu_banks[m]
            for k in range(16):
                op("pe", lambda pe, k=k, m=m, pb=pb: pe.matmul(pb[:, 0:CAP], lhsT=wg[:, k, m * 128:(m + 1) * 128], rhs=xe[:, k, :],
                                                               start=(k == 0), stop=(k == 15)), reads=[tg_, xet], writes=[pt])
        wdone(ug, 6)
        for m in range(4):
            pb, pt = gu_banks[m]
            for k in range(16):
                op("pe", lambda pe, k=k, m=m, pb=pb: pe.matmul(pb[:, CAP:2 * CAP], lhsT=wu[:, k, m * 128:(m + 1) * 128], rhs=xe[:, k, :],
                                                               start=(k == 0), stop=(k == 15)), reads=[tu_, xet], writes=[pt])
        wdone(uu, 6)
        for m in range(4):
            pb, pt = gu_banks[m]
            si = m % 2 + 2
            sg = scrv(si, CAP)
            op("act", lambda a, pb=pb, sg=sg: a.activation(out=sg, in_=pb[:, 0:CAP], func=AF.Silu), reads=[pt], writes=[scr_tok[si]])
            op("dve", lambda v, pb=pb, sg=sg, m=m: v.tensor_tensor(out=hd[:, m, :], in0=sg, in1=pb[:, CAP:2 * CAP], op=ALU.mult),
               reads=[pt, scr_tok[si]], writes=[hdt])
        for r in range(2):
            yi = yrr[0] % 4
            yrr[0] += 1
            for cb in range(4):
                pb, pt = bank()
                for m in range(4):
                    op("pe", lambda pe, m=m, r=r, cb=cb, pb=pb: pe.matmul(pb[:, :], lhsT=hd[:, m, r * 128:(r + 1) * 128],
                                                                         rhs=wd[:, m, cb * 512:(cb + 1) * 512], start=(m == 0), stop=(m == 3)),
                       reads=[td_, hdt], writes=[pt])
                if cb % 2 == 0:
                    op("act", lambda a, yi=yi, cb=cb, pb=pb: a.activation(out=Ysb[yi][:, cb * 512:(cb + 1) * 512], in_=pb[:, :], func=AF.Copy),
                       reads=[pt], writes=[Y_tok[yi]])
                else:
                    op("dve", lambda v, yi=yi, cb=cb, pb=pb: v.tensor_copy(out=Ysb[yi][:, cb * 512:(cb + 1) * 512], in_=pb[:, :]),
                       reads=[pt], writes=[Y_tok[yi]])
            dma("sp", ys_d[e * CAP + r * 128:e * CAP + (r + 1) * 128, :], Ysb[yi], reads=[Y_tok[yi]], writes=[], dtok=ys_tok)
        wdone(ud, 6)
    ys_tok.w = {ys_tok.dkey: ys_tok.dval}

    barrier2 = cx.snapshot()
    pg_pages = [0, 1, 2, 5]
    wpg_tok = cx.dma_tok("wpg", init=barrier2)
    for ci, pgi in enumerate(pg_pages):
        dma("pool", wview(pgi, 16, 512), wsrc(w_pg_d, 0, D, ci * 512, 512), writes=[wpg_tok])
    wpu = pages[6][:, 0:4096].rearrange("p (k n) -> p k n", k=2)
    dma("pool", wpu, w_pu_d.rearrange("(k p) n -> p k n", p=128), writes=[wpg_tok])
    gfin = pages[6][:, 4096:8192].bitcast(F32)
    bpg_b = scr[0:1, 3 * 1088:3 * 1088 + 1024].bitcast(BF16)
    scr_tok[3].w = dict(barrier2)
    bpg_tok = cx.dma_tok("bpg", init=barrier2)
    dma("pool", bpg_b, rows_d[0:1, R_BPG:R_BPG + D], writes=[bpg_tok])
    dma("sp", gfin, rows_d[0:1, R_GFIN:R_GFIN + D].partition_broadcast(128), writes=[wpg_tok])
    Gv = [pages[7 + (i // 2)][:, (i % 2) * 4096:(i % 2 + 1) * 4096].bitcast(F32) for i in range(4)]
    G_tok = [cx.dma_tok(f"G{i}", init=barrier2) for i in range(4)]
    for i in range(4):
        op("dve", lambda v, i=i: v.memset(Gv[i], 0.0), writes=[G_tok[i]])
    Xf_tok = [cx.dma_tok(f"Xf{j}", init=barrier2) for j in range(4)]
    h3T = [aT[:, :, :].rearrange("p k t -> p (k t)")[:, i * 2048:(i + 1) * 2048].rearrange("p (k t) -> p k t", k=16) for i in range(2)]
    h3_tok = [cx.tok(f"h3T{i}", init=barrier2) for i in range(2)]
    pbf = [sb(f"pbf{i}", [128, 256], BF16) for i in range(2)]
    pbf_tok = [cx.dma_tok(f"pbf{i}") for i in range(2)]
    pT = [sb(f"pT{i}", [128, 2, 128], BF16) for i in range(2)]
    pT_tok = [cx.tok(f"pT{i}") for i in range(2)]
    out_tok = cx.dma_tok("out")
    for st_ in scr_tok:
        pass
    for ti in range(NT):
        j = ti % 4
        Xt, Xk = Xv[j], Xf_tok[j]
        dma("sp", Xt, x1_d[ti * 128:(ti + 1) * 128, :], reads=[x1s_tok[ti]], writes=[Xk])
        for kk in range(2):
            gi = (ti % 2) * 2 + kk
            dma("pool", Gv[gi], ys_d[:, :], reads=[ys_tok, rt_tok], writes=[G_tok[gi]], indirect=True, out_offset=None,
                in_offset=bass.IndirectOffsetOnAxis(ap=idx_all[:, ti, kk:kk + 1], axis=0),
                bounds_check=bc_reg, oob_is_err=False)
        dma("pool", pbf[ti % 2][:, :], p_d[ti * 128:(ti + 1) * 128, :], writes=[pbf_tok[ti % 2]])
        for kk in range(2):
            gi = (ti % 2) * 2 + kk
            op("dve", lambda v, gi=gi, ti=ti, kk=kk, Xt=Xt: v.scalar_tensor_tensor(out=Xt, in0=Gv[gi], scalar=wt_all[:, ti, kk:kk + 1], in1=Xt,
                                                                                  op0=ALU.mult, op1=ALU.add),
               reads=[G_tok[gi], rt_tok, Xk], writes=[Xk])
        xb = xn[ti % 2]
        xt = xn_tok[ti % 2]
        rms_rstd("act", Xt, [Xk], xb[:, :], [xt], 9)
        op("act", lambda a, xb=xb, Xt=Xt: a.activation(out=xb[:, :], in_=Xt, func=AF.Copy, scale=sm[:, 9:10]),
           reads=[Xk, sm_tok], writes=[xt])
        h3 = h3T[ti % 2]
        h3t = h3_tok[ti % 2]
        transposes_bf(xb, [xt], lambda k, h3=h3: h3[:, k, :], [h3t], CV_GPLE, evac_rr)
        pb, pt = bank()
        pbb = pb[:, :].bitcast(BF16)
        for kk in range(2):
            op("pe", lambda pe, kk=kk, pbb=pbb, ti=ti: pe.transpose(out=pbb[:, kk * 128:(kk + 1) * 128], in_=pbf[ti % 2][:, kk * 128:(kk + 1) * 128],
                                                                   identity=ident_b[:, :]), reads=[pbf_tok[ti % 2]] + CONST, writes=[pt])
        op("dve", lambda v, pbb=pbb, ti=ti: v.tensor_copy(out=pT[ti % 2][:, :, :], in_=pbb[:, 0:256].rearrange("p (k t) -> p k t", k=2)),
           reads=[pt], writes=[pT_tok[ti % 2]])
        for cb in range(4):
            wg_ = wview(pg_pages[cb], 16, 512)
            pg_, pgt = bank()
            for k in range(16):
                op("pe", lambda pe, k=k, pg_=pg_, wg_=wg_, h3=h3: pe.matmul(pg_[:, :], lhsT=h3[:, k, :], rhs=wg_[:, k, :], start=(k == 0), stop=False),
                   reads=[wpg_tok, h3t], writes=[pgt])
            op("pe", lambda pe, pg_=pg_, cb=cb: pe.matmul(pg_[:, :], lhsT=ones_b[0:1, :], rhs=bpg_b[0:1, cb * 512:(cb + 1) * 512], start=False, stop=True),
               reads=CONST + [bpg_tok], writes=[pgt])
            pu_, put = bank()
            for k in range(2):
                op("pe", lambda pe, k=k, pu_=pu_, cb=cb, ti=ti: pe.matmul(pu_[:, :], lhsT=pT[ti % 2][:, k, :], rhs=wpu[:, k, cb * 512:(cb + 1) * 512],
                                                                         start=(k == 0), stop=(k == 1)), reads=[wpg_tok, pT_tok[ti % 2]], writes=[put])
            si = cb % 2
            sg = scrv(si, 512)
            op("act", lambda a, pg_=pg_, sg=sg: a.activation(out=sg, in_=pg_[:, :], func=AF.Sigmoid), reads=[pgt], writes=[scr_tok[si]])
            op("dve", lambda v, pu_=pu_, sg=sg: v.tensor_tensor(out=sg, in0=sg, in1=pu_[:, :], op=ALU.mult), reads=[put, scr_tok[si]], writes=[scr_tok[si]])
            op("dve", lambda v, sg=sg, cb=cb, Xt=Xt: v.tensor_tensor(out=Xt[:, cb * 512:(cb + 1) * 512], in0=Xt[:, cb * 512:(cb + 1) * 512], in1=sg, op=ALU.add),
               reads=[scr_tok[si], Xk], writes=[Xk])
        rms_rstd("act", Xt, [Xk], xb[:, :], [xt], 10)
        op("dve", lambda v, Xt=Xt: v.scalar_tensor_tensor(out=Xt, in0=Xt, scalar=sm[:, 10:11], in1=gfin, op0=ALU.mult, op1=ALU.mult),
           reads=[Xk, sm_tok, wpg_tok], writes=[Xk])
        dma("sp", y_d[ti * 128:(ti + 1) * 128, :], Xt, reads=[Xk], writes=[], dtok=out_tok)
    if DEBUG_X1:
        dbg_w = nc.dram_tensor("dbg_w", [128, NT * 2], F32, kind="ExternalOutput").ap()
        dbg_i = nc.dram_tensor("dbg_i", [128, NT * 2], I32, kind="ExternalOutput").ap()
        dma("sp", dbg_w[:, :], wt_all[:, :, :].rearrange("p a b -> p (a b)"), reads=[rt_tok], writes=[], dtok=out_tok)
        dma("sp", dbg_i[:, :], idx_all[:, :, :].rearrange("p a b -> p (a b)"), reads=[rt_tok], writes=[], dtok=out_tok)
    cx._wait("sp", {out_tok.dkey: out_tok.dval})
    return nc


_NC_CACHE = {}


def _prep_inputs(inp):
    f = lambda a: np.ascontiguousarray(np.asarray(a, dtype=np.float32))
    x = f(inp["x"]).reshape(4, 4096, D)
    p = f(inp["p"]).reshape(4, 4096, 256)

    def pk(v):
        v = f(v).reshape(-1)
        return v.reshape(-1, 128).T

    cvec = np.concatenate([pk(inp["g_mix"]), pk(inp["g_ffn"]), pk(inp["g_ple"]), pk(inp["pool_scale"]),
                           pk(inp["b_merge_gate"])], axis=1)
    assert cvec.shape == (128, CV_N)
    wrg = f(inp["w_router_group"]).reshape(D, 4)
    wre = f(inp["w_router_expert"]).reshape(4, D, 8).transpose(1, 0, 2).reshape(D, 32)
    wr = np.concatenate([wrg, wre], axis=1)
    wr = wr.reshape(16, 128, 36).transpose(1, 0, 2).reshape(128, 16 * 36)
    rb = np.concatenate([f(inp["b_router_group"]).reshape(-1), f(inp["b_router_expert"]).reshape(-1)])
    shared = {
        "cvec": np.ascontiguousarray(cvec),
        "wr": np.ascontiguousarray(wr),
        "w_in": f(inp["w_in"]).reshape(D, 3072),
        "w_pool": f(inp["w_pool"]).reshape(4, 256, 256),
        "w_ba": f(inp["w_branch_a"]).reshape(1024, D),
        "w_bb": f(inp["w_branch_b"]).reshape(1024, D),
        "w_sp": f(inp["w_spatial"]).reshape(8, 128, 128),
        "w_mg": f(inp["w_merge_gate"]).reshape(D, 2 * D),
        "w_out": f(inp["w_out"]).reshape(D, D),
        "w_eg": f(inp["w_exp_gate"]).reshape(NE, D, 512),
        "w_eu": f(inp["w_exp_up"]).reshape(NE, D, 512),
        "w_ed": f(inp["w_exp_down"]).reshape(NE, 512, D),
        "w_pg": f(inp["w_ple_gate"]).reshape(D, D),
        "w_pu": f(inp["w_ple_up"]).reshape(256, D),
    }
    maps = []
    for c in range(NCORES):
        bi, half = c // 2, c % 2
        t0 = half * T
        rows = np.zeros((1, R_N), np.float32)
        rows[0, R_LNG:R_LNG + 1024] = f(inp["sgu_ln_g"]).reshape(-1)
        rows[0, R_LNB:R_LNB + 1024] = f(inp["sgu_ln_b"]).reshape(-1)
        rows[0, R_BSP:R_BSP + 1024] = f(inp["b_spatial"]).reshape(-1)
        rows[0, R_RB:R_RB + 36] = rb
        rows[0, R_GFIN:R_GFIN + D] = f(inp["g_final"]).reshape(-1)
        rows[0, R_BPG:R_BPG + D] = f(inp["b_ple_gate"]).reshape(-1)
        rows[0, R_POS:R_POS + HALO] = t0 + 1 + np.arange(HALO)
        xh = np.zeros((HALO, D), np.float32)
        if half == 1:
            xh[:] = x[bi, t0 - HALO:t0]
        m = dict(shared)
        m["x"] = np.ascontiguousarray(x[bi, t0:t0 + T])
        m["xh"] = xh
        m["p"] = np.ascontiguousarray(p[bi, t0:t0 + T])
        m["rows"] = rows
        maps.append(m)
    return maps


def kernel(**inputs):
    stage = 2
    if stage not in _NC_CACHE:
        _NC_CACHE[stage] = build(stage)
    nc = _NC_CACHE[stage]
    maps = _prep_inputs(inputs)
    res = run_bass_kernel_spmd(nc, maps, core_ids=list(range(NCORES)))
    out = np.empty((4, 4096, D), np.float32)
    for c in range(NCORES):
        out[c // 2, (c % 2) * T:(c % 2 + 1) * T] = res.results[c]["y"]
    return out
```

```python
import numpy as np
import concourse.bass as bass
import concourse.mybir as mybir
from concourse.bass_utils import run_bass_kernel_spmd

F32 = mybir.dt.float32
BF16 = mybir.dt.bfloat16
I32 = mybir.dt.int32
AF = mybir.ActivationFunctionType
ALU = mybir.AluOpType
AX = mybir.AxisListType

NCORES = 8
T = 2048
D = 2048
NT = T // 128
TB = 512
NB = T // TB
JT = TB // 128
HALO = 16
CAP = 256
NE = 32
EPS = 1e-6
BIG = float(1 << 20)
EPOCH = 6000
SAME_ENGINE_SYNC = True
DEBUG_X1 = False

CV_GMIX, CV_GFFN, CV_GPLE, CV_PSC, CV_BMG, CV_N = 0, 16, 32, 48, 56, 88
R_LNG, R_LNB, R_BSP, R_RB, R_GFIN, R_BPG, R_POS, R_N = 0, 1024, 2048, 3072, 3108, 5156, 7204, 7220


class Tok:
    __slots__ = ("w", "r", "name", "dkey", "dval")

    def __init__(self, name, init=None):
        self.name = name
        self.w = dict(init) if init else {}
        self.r = {}


class Ctx:
    def __init__(self):
        self.nc = bass.Bass("TRN2", target_bir_lowering=False)
        nc = self.nc
        self.eng = dict(pe=nc.tensor, act=nc.scalar, dve=nc.vector, pool=nc.gpsimd, sp=nc.sync)
        self.semh = {}
        self.cur = {}
        self.cnt = {}
        self.known = {e: {} for e in self.eng}
        self.nsem = 0
        for e in self.eng:
            self._new_epoch(e, 0)
        self.dma_sems = []

    def _sem(self, name):
        self.nsem += 1
        return self.nc.semaphore(name).__enter__()

    def _new_epoch(self, e, ep):
        key = (e, ep)
        self.semh[key] = self._sem(f"s_{e}_{ep}")
        self.cur[e] = key
        self.cnt[e] = 0

    def tok(self, name, init=None):
        return Tok(name, init)

    def dma_tok(self, name, init=None):
        t = Tok(name, init)
        key = ("dma", name)
        self.semh[key] = self._sem(f"d_{name}")
        t.dkey = key
        t.dval = 0
        return t

    def snapshot(self):
        d = {}
        for e in self.eng:
            if self.cnt[e] > 0:
                d[self.cur[e]] = self.cnt[e]
            elif self.cur[e][1] > 0:
                d[(e, self.cur[e][1] - 1)] = EPOCH
        for t in self.dma_sems:
            if t.dval > 0:
                d[t.dkey] = t.dval
        return d

    def _wait(self, e, deps):
        eng = self.eng[e]
        kn = self.known[e]
        for key, val in deps.items():
            if kn.get(key, 0) >= val:
                continue
            if key[0] == e and not SAME_ENGINE_SYNC:
                continue
            if key[0] == e and e in ("pe",):
                continue
            eng.wait_ge(self.semh[key], val)
            kn[key] = val

    @staticmethod
    def _merge(d, src):
        for k, v in src.items():
            if d.get(k, 0) < v:
                d[k] = v

    def op(self, e, fn, reads=(), writes=()):
        deps = {}
        for t in reads:
            self._merge(deps, t.w)
        for t in writes:
            self._merge(deps, t.w)
            self._merge(deps, t.r)
        self._wait(e, deps)
        ins = fn(self.eng[e])
        if self.cnt[e] >= EPOCH:
            self._new_epoch(e, self.cur[e][1] + 1)
        self.cnt[e] += 1
        key, val = self.cur[e], self.cnt[e]
        ins.then_inc(self.semh[key], 1)
        for t in reads:
            t.r[key] = val
        for t in writes:
            t.w = {key: val}
            t.r = {}
        return ins

    def dma(self, e, out, in_, reads=(), writes=(), dtok=None, **kw):
        deps = {}
        for t in reads:
            self._merge(deps, t.w)
        for t in writes:
            self._merge(deps, t.w)
            self._merge(deps, t.r)
        self._wait(e, deps)
        if dtok is None:
            dtok = writes[0]
        if dtok not in self.dma_sems:
            self.dma_sems.append(dtok)
        eng = self.eng[e]
        if "indirect" in kw:
            kw = dict(kw)
            kw.pop("indirect")
            ins = eng.indirect_dma_start(out=out, in_=in_, **kw)
        else:
            ins = eng.dma_start(out=out, in_=in_, **kw)
        dtok.dval += 16
        ins.then_inc(self.semh[dtok.dkey], 16)
        key, val = dtok.dkey, dtok.dval
        for t in reads:
            t.r[key] = val
        for t in writes:
            if t is dtok:
                t.w = {key: val}
                t.r = {}
            else:
                t.w = {key: val}
                t.r = {}
        return ins


def build(stage=2):
    cx = Ctx()
    nc = cx.nc
    op, dma = cx.op, cx.dma

    def din(name, shape, dt=F32):
        return nc.dram_tensor(name, list(shape), dt, kind="ExternalInput").ap()

    x_d = din("x", [T, D])
    xh_d = din("xh", [HALO, D])
    p_d = din("p", [T, 256])
    cvec_d = din("cvec", [128, CV_N])
    wr_d = din("wr", [128, 16 * 36])
    rows_d = din("rows", [1, R_N])
    w_in_d = din("w_in", [D, 3072])
    w_pool_d = din("w_pool", [4, 256, 256])
    w_ba_d = din("w_ba", [1024, D])
    w_bb_d = din("w_bb", [1024, D])
    w_sp_d = din("w_sp", [8, 128, 128])
    w_mg_d = din("w_mg", [D, 2 * D])
    w_out_d = din("w_out", [D, D])
    w_eg_d = din("w_eg", [NE, D, 512])
    w_eu_d = din("w_eu", [NE, D, 512])
    w_ed_d = din("w_ed", [NE, 512, D])
    w_pg_d = din("w_pg", [D, D])
    w_pu_d = din("w_pu", [256, D])
    y_d = nc.dram_tensor("y", [T, D], F32, kind="ExternalOutput").ap()
    xs_d = nc.dram_tensor("xs_scr", [NE * CAP, D], BF16).ap()
    ys_d = nc.dram_tensor("ys_scr", [NE * CAP, D], F32).ap()
    x1_d = nc.dram_tensor("x1_scr", [T, D], F32, kind=("ExternalOutput" if DEBUG_X1 else "Internal")).ap()

    def sb(name, shape, dt):
        return nc.alloc_sbuf_tensor("sb_" + name, list(shape), dt)

    bc_reg = nc.gpsimd.to_reg(NE * CAP - 1)
    NPAGE = 9
    pages = [sb(f"pg{i}", [128, 8192], BF16) for i in range(NPAGE)]
    aT = sb("aT", [128, 8, HALO + TB], BF16)
    xn = [sb(f"xn{i}", [128, D], BF16) for i in range(2)]
    scr = sb("scr", [128, 4 * 1088], F32)
    ident_b = sb("ident_b", [128, 128], BF16)
    ident_f = sb("ident_f", [128, 128], F32)
    ltri_f = sb("ltri_f", [128, 128], F32)
    ones_f = sb("ones_f", [128, 128], F32)
    ones_b = sb("ones_b", [1, 128], BF16)
    cvec = sb("cvec", [128, CV_N], F32)
    wr_sb = sb("wr_sb", [128, 16, 36], F32)
    wsT = sb("wsT", [128, 8, 128], BF16)
    bspB = sb("bspB", [128, 8, 128], F32)
    wpool = sb("wpool", [128, 4, 2, 256], BF16)
    lnG = sb("lnG", [128, 1024], F32)
    lnB = sb("lnB", [128, 1024], F32)
    rbB = sb("rbB", [128, 36], F32)
    corr = sb("corr", [128, 4, HALO], F32)
    offs = sb("offs", [128, NE], F32)
    offs_i = sb("offs_i", [128, NE], I32)
    msum = sb("msum", [128, NE], F32)
    wt_all = sb("wt_all", [128, NT, 2], F32)
    idx_all = sb("idx_all", [128, NT, 2], I32)
    sm = sb("sm", [128, 336], F32)
    posb = sb("posb", [128, HALO], F32)
    epsb = sb("epsb", [128, 1], F32)

    psb = [nc.alloc_psum_tensor(f"ps{i}", [128, 512], F32) for i in range(8)]
    ps_tok = [cx.tok(f"ps{i}") for i in range(8)]
    ps_i = [0]

    def bank():
        i = ps_i[0] % 8
        ps_i[0] += 1
        return psb[i], ps_tok[i]

    t_const = cx.dma_tok("const")
    t_cst2 = cx.tok("cst2")

    dma("sp", cvec[:, :], cvec_d[:, :], writes=[t_const])
    dma("sp", wr_sb[:, :, :], wr_d[:, :].rearrange("p (k n) -> p k n", k=16), writes=[t_const])
    dma("sp", lnG[:, :], rows_d[0:1, R_LNG:R_LNG + 1024].partition_broadcast(128), writes=[t_const])
    dma("sp", lnB[:, :], rows_d[0:1, R_LNB:R_LNB + 1024].partition_broadcast(128), writes=[t_const])
    dma("sp", bspB[:, :, :], rows_d[0:1, R_BSP:R_BSP + 1024].partition_broadcast(128), writes=[t_const])
    dma("sp", rbB[:, :], rows_d[0:1, R_RB:R_RB + 36].partition_broadcast(128), writes=[t_const])
    dma("sp", posb[:, :], rows_d[0:1, R_POS:R_POS + HALO].partition_broadcast(128), writes=[t_const])
    dma("pool", wpool[:, :, :, :], w_pool_d.rearrange("g (k p) n -> p g k n", p=128), writes=[t_const])

    op("pool", lambda g: g.memset(ones_f[:, :], 1.0), writes=[t_cst2])
    op("pool", lambda g: g.memset(ident_f[:, :], 1.0), writes=[t_cst2])
    op("pool", lambda g: g.affine_select(out=ident_f[:, :], in_=ident_f[:, :], pattern=[[1, 128]],
                                         compare_op=ALU.is_equal, fill=0.0, base=0, channel_multiplier=-1),
       writes=[t_cst2])
    op("pool", lambda g: g.memset(ltri_f[:, :], 1.0), writes=[t_cst2])
    op("pool", lambda g: g.affine_select(out=ltri_f[:, :], in_=ltri_f[:, :], pattern=[[1, 128]],
                                         compare_op=ALU.is_ge, fill=0.0, base=0, channel_multiplier=-1),
       writes=[t_cst2])
    op("pool", lambda g: g.iota(offs_i[:, :], pattern=[[CAP, NE]], base=-1, channel_multiplier=0), writes=[t_cst2])
    op("dve", lambda v: v.tensor_copy(out=ident_b[:, :], in_=ident_f[:, :]), reads=[t_cst2], writes=[t_cst2])
    op("dve", lambda v: v.tensor_copy(out=offs[:, :], in_=offs_i[:, :]), reads=[t_cst2], writes=[t_cst2])
    op("dve", lambda v: v.memset(ones_b[:, :], 1.0), writes=[t_cst2])
    op("dve", lambda v: v.memset(msum[:, :], 0.0), writes=[t_cst2])
    op("dve", lambda v: v.memset(epsb[:, :], EPS), writes=[t_cst2])
    for gi, w in enumerate((2, 4, 8, 16)):
        op("dve", lambda v, gi=gi, w=w: v.tensor_scalar(out=corr[:, gi, :], in0=posb[:, :], scalar1=float(w),
                                                        scalar2=None, op0=ALU.min),
           reads=[t_const], writes=[t_cst2])
        op("dve", lambda v, gi=gi: v.reciprocal(out=corr[:, gi, :], in_=corr[:, gi, :]), reads=[t_cst2], writes=[t_cst2])
        op("dve", lambda v, gi=gi, w=w: v.tensor_scalar(out=corr[:, gi, :], in0=corr[:, gi, :], scalar1=float(w),
                                                        scalar2=None, op0=ALU.mult),
           reads=[t_cst2], writes=[t_cst2])
    t_wsl = cx.dma_tok("wsl")
    for g8 in range(8):
        stg = scr[:, 0:128]
        dma("sp", stg, w_sp_d[g8], writes=[t_wsl])
        pb, pt = bank()
        op("pe", lambda pe, pb=pb: pe.transpose(out=pb[:, 0:128], in_=scr[:, 0:128], identity=ident_f[:, :]),
           reads=[t_wsl, t_cst2], writes=[pt])
        op("dve", lambda v, pb=pb, g8=g8: v.tensor_copy(out=wsT[:, g8, :], in_=pb[:, 0:128]), reads=[pt], writes=[t_cst2])
        t_wsl.r.update(pt.w)
    op("dve", lambda v: v.memset(wsT[64:128, :, 0:64], 0.0), reads=[t_cst2], writes=[t_cst2])
    CONST = [t_const, t_cst2]

    NSLOT_MIX = 3
    slot_pages = [0, 1, 2]
    slot_tok = {i: cx.dma_tok(f"wslot{i}") for i in range(NPAGE)}

    class WStream:
        def __init__(self):
            self.units = []
            self.issued = 0
            self.slots = list(slot_pages)

        def add(self, parts):
            self.units.append(parts)
            return len(self.units) - 1

        def slot_of(self, i):
            return self.units[i][0]

        def ensure(self, upto):
            while self.issued <= min(upto, len(self.units) - 1):
                pg_i, parts = self.units[self.issued]
                for dst, src in parts:
                    dma("pool", dst, src, writes=[slot_tok[pg_i]])
                self.issued += 1

    ws = WStream()

    def wview(pg_i, kc, n):
        return pages[pg_i][:, 0:kc * n].rearrange("p (k n) -> p k n", k=kc)

    def wsrc(w_ap, r0, nrows, c0, ncols):
        return w_ap[r0:r0 + nrows, c0:c0 + ncols].rearrange("(k p) n -> p k n", p=128)

    unit_seq = []
    slot_rr = [0]

    def next_slot(slots):
        s = slots[slot_rr[0] % len(slots)]
        slot_rr[0] += 1
        return s

    mix_slots = [0, 1, 2]

    def sched_unit(parts_fn, slots):
        s = next_slot(slots)
        return ws.add((s, parts_fn(s)))

    Xv = [pages[3 + (j // 2)][:, (j % 2) * 4096:(j % 2 + 1) * 4096].bitcast(F32) for j in range(4)]
    X_tok = [cx.dma_tok(f"X{j}") for j in range(4)]
    hT = pages[5][:, :].rearrange("p (k t) -> p k t", k=16)
    hT_tok = cx.tok("hT")
    mT = pages[6][:, :].rearrange("p (k t) -> p k t", k=16)
    mT_tok = cx.tok("mT")
    uT = pages[7][:, 0:4096].rearrange("p (k t) -> p k t", k=8)
    uT_tok = cx.tok("uT")
    vT = pages[7][:, 4096:8192].rearrange("p (j c) -> p j c", j=JT)
    vT_tok = cx.tok("vT")
    pzT = pages[8][:, 0:4096].rearrange("p (k t) -> p k t", k=8)
    pz_tok = cx.tok("pzT")
    ypT = pages[8][:, 4096:8192].rearrange("p (k t) -> p k t", k=8)
    yp_tok = cx.tok("ypT")
    aT_tok = cx.tok("aT")
    xn_tok = [cx.dma_tok(f"xn{i}") for i in range(2)]
    scr_tok = [cx.tok(f"scr{i}") for i in range(4)]
    sm_tok = cx.tok("sm")
    rt_tok = cx.tok("route")
    xs_tok = cx.dma_tok("xs")
    x1s_tok = [cx.dma_tok(f"x1s{i}") for i in range(NT)]
    hh_tok = cx.tok("hhalo")
    hhalo = sb("hhalo", [128, 16, HALO], BF16)

    def scrv(i, n=1088):
        return scr[:, i * 1088:i * 1088 + n]

    WIN_ORDER = (4, 5, 0, 1, 2, 3)
    plan = []
    for b in range(NB):
        blk = {}
        blk["win"] = {cb: sched_unit(lambda s, cb=cb: [(wview(s, 16, 512), wsrc(w_in_d, 0, D, cb * 512, 512))], mix_slots)
                      for cb in WIN_ORDER}
        blk["mrg"] = []
        for cb in range(4):
            u_g1 = sched_unit(lambda s, cb=cb: [(wview(s, 16, 512), wsrc(w_mg_d, 0, D, cb * 512, 512))], mix_slots)
            u_g2 = sched_unit(lambda s, cb=cb: [(wview(s, 16, 512), wsrc(w_mg_d, 0, D, D + cb * 512, 512))], mix_slots)
            u_ab = sched_unit(lambda s, cb=cb: [
                (pages[s][:, 0:4096].rearrange("p (k n) -> p k n", k=8), wsrc(w_ba_d, 0, 1024, cb * 512, 512)),
                (pages[s][:, 4096:8192].rearrange("p (k n) -> p k n", k=8), wsrc(w_bb_d, 0, 1024, cb * 512, 512))],
                mix_slots)
            blk["mrg"].append((u_ab, u_g1, u_g2))
        blk["wout"] = [sched_unit(lambda s, cb=cb: [(wview(s, 16, 512), wsrc(w_out_d, 0, D, cb * 512, 512))], mix_slots)
                       for cb in range(4)]
        plan.append(blk)
    n_mix_units = len(ws.units)

    def wget(u):
        ws.ensure(u)
        s = ws.units[u][0]
        return s, slot_tok[s]

    def wdone(u, nslots=3):
        ws.ensure(u + nslots)

    def rms_rstd(e_src, src_ap, src_toks, junk_ap, junk_toks, col, npart=128):
        sc = sm[0:npart, col:col + 1]
        op("act", lambda a: a.activation(out=junk_ap, in_=src_ap, func=AF.Square, accum_out=sc),
           reads=src_toks, writes=junk_toks + [sm_tok])
        op("act", lambda a: a.activation(out=sc, in_=sc, func=AF.Sqrt, scale=1.0 / D, bias=epsb[0:npart, :]),
           reads=[sm_tok] + CONST, writes=[sm_tok])
        op("dve", lambda v: v.reciprocal(out=sc, in_=sc), reads=[sm_tok], writes=[sm_tok])

    def transposes_bf(src_rows_ap, src_toks, dst_fn, dst_toks, gcol, evac_rr):
        for q in range(4):
            pb, pt = bank()
            pbb = pb[:, :].bitcast(BF16)
            for kk in range(4):
                k = q * 4 + kk
                op("pe", lambda pe, k=k, kk=kk, pbb=pbb: pe.transpose(out=pbb[:, kk * 128:(kk + 1) * 128],
                                                                       in_=src_rows_ap[:, k * 128:(k + 1) * 128],
                                                                       identity=ident_b[:, :]),
                   reads=src_toks + CONST, writes=[pt])
            for kk in range(4):
                k = q * 4 + kk
                if (evac_rr[0] % 2) == 0:
                    op("act", lambda a, k=k, kk=kk, pbb=pbb: a.activation(out=dst_fn(k), in_=pbb[:, kk * 128:(kk + 1) * 128],
                                                                          func=AF.Copy, scale=cvec[:, gcol + k:gcol + k + 1]),
                       reads=[pt] + CONST, writes=dst_toks)
                else:
                    op("dve", lambda v, k=k, kk=kk, pbb=pbb: v.tensor_scalar(out=dst_fn(k), in0=pbb[:, kk * 128:(kk + 1) * 128],
                                                                             scalar1=cvec[:, gcol + k:gcol + k + 1], scalar2=None,
                                                                             op0=ALU.mult),
                       reads=[pt] + CONST, writes=dst_toks)
                evac_rr[0] += 1

    evac_rr = [0]

    def emit_layernorm():
            for j in range(JT):
                vg = scrv(j, 1024)
                st6 = sm[:, 16:28]
                op("dve", lambda v, vg=vg: v.bn_stats(out=sm[:, 16:22], in_=vg[:, 0:512]), reads=[scr_tok[j]], writes=[sm_tok])
                op("dve", lambda v, vg=vg: v.bn_stats(out=sm[:, 22:28], in_=vg[:, 512:1024]), reads=[scr_tok[j], sm_tok], writes=[sm_tok])
                op("dve", lambda v: v.bn_aggr(out=sm[:, 28:30], in_=sm[:, 16:28]), reads=[sm_tok], writes=[sm_tok])
                op("act", lambda a: a.activation(out=sm[:, 29:30], in_=sm[:, 29:30], func=AF.Sqrt, scale=1.0, bias=epsb[:, :]),
                   reads=[sm_tok] + CONST, writes=[sm_tok])
                op("dve", lambda v: v.reciprocal(out=sm[:, 29:30], in_=sm[:, 29:30]), reads=[sm_tok], writes=[sm_tok])
                op("dve", lambda v, vg=vg: v.scalar_tensor_tensor(out=vg, in0=vg, scalar=sm[:, 28:29], in1=lnG[:, :],
                                                                  op0=ALU.subtract, op1=ALU.mult),
                   reads=[scr_tok[j], sm_tok] + CONST, writes=[scr_tok[j]])
                op("dve", lambda v, vg=vg, j=j: v.scalar_tensor_tensor(out=vT[:, j, :], in0=vg, scalar=sm[:, 29:30], in1=lnB[:, :],
                                                                       op0=ALU.mult, op1=ALU.add),
                   reads=[scr_tok[j], sm_tok] + CONST, writes=[vT_tok])


    pg6f = pages[6][:, :].bitcast(F32)

    def emit_pool_dve(b):
        W = HALO + TB
        for gi, w in enumerate((2, 4, 8, 16)):
            ca = aT[:, 2 * gi:2 * gi + 2, :]
            bufA = pg6f[:, 0:2 * W].rearrange("p (k t) -> p k t", k=2)
            bufB = pg6f[:, 2 * W:4 * W].rearrange("p (k t) -> p k t", k=2)
            cur, cur_t = ca, aT_tok
            sh = 1
            lvl = 0
            while sh < w:
                dst = bufA if lvl % 2 == 0 else bufB
                lo = 2 * sh - 1
                op("dve", lambda v, dst=dst, cur=cur, lo=lo, sh=sh: v.tensor_tensor(out=dst[:, :, lo:W], in0=cur[:, :, lo:W],
                                                                                     in1=cur[:, :, lo - sh:W - sh], op=ALU.add),
                   reads=[cur_t], writes=[mT_tok])
                cur, cur_t = dst, mT_tok
                sh *= 2
                lvl += 1
            if b == 0:
                for c2 in range(2):
                    op("dve", lambda v, cur=cur, gi=gi, c2=c2: v.tensor_tensor(out=cur[:, c2, HALO:2 * HALO], in0=cur[:, c2, HALO:2 * HALO],
                                                                              in1=corr[:, gi, :], op=ALU.mult),
                       reads=[cur_t] + CONST, writes=[cur_t])
            op("dve", lambda v, cur=cur, ca=ca, gi=gi, w=w: v.scalar_tensor_tensor(out=pzT[:, 2 * gi:2 * gi + 2, :], in0=cur[:, :, HALO:W],
                                                                                   scalar=1.0 / w, in1=ca[:, :, HALO:W],
                                                                                   op0=ALU.mult, op1=ALU.subtract),
               reads=[cur_t, aT_tok], writes=[pz_tok])

    def stepA_halo():
        dma("sp", Xv[0][0:HALO, :], xh_d[:, :], writes=[X_tok[0]])
        rms_rstd("act", Xv[0][0:HALO, :], [X_tok[0]], xn[0][0:HALO, :], [xn_tok[0]], 0, npart=HALO)
        op("act", lambda a: a.activation(out=xn[0][0:HALO, :], in_=Xv[0][0:HALO, :], func=AF.Copy, scale=sm[0:HALO, 0:1]),
           reads=[X_tok[0], sm_tok], writes=[xn_tok[0]])
        for q in range(4):
            pb, pt = bank()
            pbb = pb[:, :].bitcast(BF16)
            for kk in range(4):
                k = q * 4 + kk
                op("pe", lambda pe, k=k, kk=kk, pbb=pbb: pe.transpose(out=pbb[:, kk * HALO:(kk + 1) * HALO],
                                                                       in_=xn[0][0:HALO, k * 128:(k + 1) * 128],
                                                                       identity=ident_b[0:HALO, 0:HALO]),
                   reads=[xn_tok[0]] + CONST, writes=[pt])
            for kk in range(4):
                k = q * 4 + kk
                op("dve", lambda v, k=k, kk=kk, pbb=pbb: v.tensor_scalar(out=hhalo[:, k, :], in0=pbb[:, kk * HALO:(kk + 1) * HALO],
                                                                         scalar1=cvec[:, CV_GMIX + k:CV_GMIX + k + 1],
                                                                         scalar2=None, op0=ALU.mult),
                   reads=[pt] + CONST, writes=[hh_tok])

    def stepA_load(b, j):
        t0 = b * TB
        dma("sp", Xv[j], x_d[t0 + j * 128:t0 + (j + 1) * 128, :], writes=[X_tok[j]])

    def stepA_tile(b, j, xi):
        xb = xn[xi]
        xt = xn_tok[xi]
        rms_rstd("act", Xv[j], [X_tok[j]], xb[:, :], [xt], j)
        op("act", lambda a, j=j, xb=xb: a.activation(out=xb[:, :], in_=Xv[j], func=AF.Copy, scale=sm[:, j:j + 1]),
           reads=[X_tok[j], sm_tok], writes=[xt])
        transposes_bf(xb, [xt], lambda k, j=j: hT[:, k, j * 128:(j + 1) * 128], [hT_tok], CV_GMIX, evac_rr)

    stepA_halo()
    for j in range(JT):
        stepA_load(0, j)
    for j in range(JT):
        stepA_tile(0, j, j % 2)

    for b in range(NB):
        blk = plan[b]
        t0 = b * TB
        if b > 0:
            op("dve", lambda v: v.tensor_copy(out=aT[:, :, 0:HALO], in_=aT[:, :, TB:TB + HALO]), reads=[aT_tok], writes=[aT_tok])
        for cbi, cb in enumerate(WIN_ORDER):
            if cbi > 0:
                wdone(blk["win"][WIN_ORDER[cbi - 1]])
            if cbi == 2:
                emit_layernorm()
            if cbi == 4:
                emit_pool_dve(b)
            s, st = wget(blk["win"][cb])
            wv = wview(s, 16, 512)
            if cb < 4:
                for mm in range(4):
                    m = (cb % 2) * 4 + mm
                    pb, pt = bank()
                    for k in range(16):
                        op("pe", lambda pe, k=k, mm=mm, pb=pb, wv=wv: pe.matmul(pb[:, :], lhsT=wv[:, k, mm * 128:(mm + 1) * 128],
                                                                               rhs=hT[:, k, :], start=(k == 0), stop=(k == 15)),
                           reads=[st, hT_tok], writes=[pt])
                    if cb < 2:
                        op("dve", lambda v, m=m, pb=pb: v.tensor_copy(out=aT[:, m, HALO:HALO + TB], in_=pb[:, :]),
                           reads=[pt], writes=[aT_tok])
                        if b == 0:
                            pb2, pt2 = bank()
                            for k in range(16):
                                op("pe", lambda pe, k=k, mm=mm, pb2=pb2, wv=wv: pe.matmul(pb2[:, 0:HALO], lhsT=wv[:, k, mm * 128:(mm + 1) * 128],
                                                                                         rhs=hhalo[:, k, :], start=(k == 0), stop=(k == 15)),
                                   reads=[st, hh_tok], writes=[pt2])
                            op("dve", lambda v, m=m, pb2=pb2: v.tensor_copy(out=aT[:, m, 0:HALO], in_=pb2[:, 0:HALO]),
                               reads=[pt2], writes=[aT_tok])
                    else:
                        op("act", lambda a, m=m, pb=pb: a.activation(out=uT[:, m, :], in_=pb[:, :], func=AF.Gelu_apprx_tanh),
                           reads=[pt], writes=[uT_tok])
            else:
                half = cb - 4
                for j in range(JT):
                    pb, pt = bank()
                    for k in range(16):
                        op("pe", lambda pe, k=k, j=j, pb=pb, wv=wv: pe.matmul(pb[:, :], lhsT=hT[:, k, j * 128:(j + 1) * 128],
                                                                             rhs=wv[:, k, :], start=(k == 0), stop=(k == 15)),
                           reads=[st, hT_tok], writes=[pt])
                    op("act", lambda a, j=j, half=half, pb=pb: a.activation(out=scrv(j)[:, half * 512:(half + 1) * 512], in_=pb[:, :],
                                                                            func=AF.Gelu_apprx_tanh),
                       reads=[pt], writes=[scr_tok[j]])
        wdone(blk["win"][WIN_ORDER[-1]])
        for gi in range(4):
            for mo in range(2):
                pb, pt = bank()
                for ki in range(2):
                    op("pe", lambda pe, gi=gi, mo=mo, ki=ki, pb=pb: pe.matmul(pb[:, :], lhsT=wpool[:, gi, ki, mo * 128:(mo + 1) * 128],
                                                                             rhs=pzT[:, 2 * gi + ki, :], start=(ki == 0), stop=(ki == 1)),
                       reads=[pz_tok] + CONST, writes=[pt])
                c = 2 * gi + mo
                op("act", lambda a, c=c, pb=pb: a.activation(out=ypT[:, c, :], in_=pb[:, :], func=AF.Copy,
                                                             scale=cvec[:, CV_PSC + c:CV_PSC + c + 1]),
                   reads=[pt] + CONST, writes=[yp_tok])
        for g8 in range(8):
            pb, pt = bank()
            for j in range(JT):
                op("pe", lambda pe, g8=g8, j=j, pb=pb: pe.matmul(pb[:, j * 128:(j + 1) * 128], lhsT=vT[:, j, g8 * 128:(g8 + 1) * 128],
                                                                 rhs=wsT[:, g8, :], start=True, stop=True),
                   reads=[vT_tok] + CONST, writes=[pt])
            tmp = scrv(1, 512)
            for j in range(JT):
                op("dve", lambda v, g8=g8, j=j, pb=pb, tmp=tmp: v.tensor_tensor(out=tmp[:, j * 128:(j + 1) * 128], in0=pb[:, j * 128:(j + 1) * 128],
                                                                               in1=bspB[:, g8, :], op=ALU.add),
                   reads=[pt] + CONST, writes=[scr_tok[1]])
            op("dve", lambda v, g8=g8, tmp=tmp: v.tensor_tensor(out=uT[:, g8, :], in0=uT[:, g8, :], in1=tmp, op=ALU.mult),
               reads=[scr_tok[1], uT_tok], writes=[uT_tok])

        for cb in range(4):
            u_ab, u_g1, u_g2 = blk["mrg"][cb]
            for gi2, ug_ in enumerate((u_g1, u_g2)):
                s_g, t_g = wget(ug_)
                wgm = wview(s_g, 16, 512)
                for mm in range(4):
                    m = cb * 4 + mm
                    p1, p1t = bank()
                    for k in range(16):
                        op("pe", lambda pe, k=k, mm=mm, p1=p1, wgm=wgm: pe.matmul(p1[:, :], lhsT=wgm[:, k, mm * 128:(mm + 1) * 128], rhs=hT[:, k, :],
                                                                                 start=(k == 0), stop=(k == 15)),
                           reads=[t_g, hT_tok], writes=[p1t])
                    sgt = scrv(mm)[:, gi2 * 512:(gi2 + 1) * 512]
                    op("act", lambda a, m=m, p1=p1, sgt=sgt, gi2=gi2: a.activation(out=sgt, in_=p1[:, :], func=AF.Sigmoid,
                                                                                    bias=cvec[:, CV_BMG + 16 * gi2 + m:CV_BMG + 16 * gi2 + m + 1], scale=1.0),
                       reads=[p1t] + CONST, writes=[scr_tok[mm]])
                wdone(ug_)
            s_ab, t_ab = wget(u_ab)
            wa = pages[s_ab][:, 0:4096].rearrange("p (k n) -> p k n", k=8)
            wb = pages[s_ab][:, 4096:8192].rearrange("p (k n) -> p k n", k=8)
            for mm in range(4):
                m = cb * 4 + mm
                pa, pat = bank()
                for k in range(8):
                    op("pe", lambda pe, k=k, mm=mm, pa=pa, wa=wa: pe.matmul(pa[:, :], lhsT=wa[:, k, mm * 128:(mm + 1) * 128], rhs=ypT[:, k, :],
                                                                           start=(k == 0), stop=(k == 7)),
                       reads=[t_ab, yp_tok], writes=[pat])
                pbk, pbt = bank()
                for k in range(8):
                    op("pe", lambda pe, k=k, mm=mm, pbk=pbk, wb=wb: pe.matmul(pbk[:, :], lhsT=wb[:, k, mm * 128:(mm + 1) * 128], rhs=uT[:, k, :],
                                                                             start=(k == 0), stop=(k == 7)),
                       reads=[t_ab, uT_tok], writes=[pbt])
                s1 = scrv(mm)[:, 0:512]
                s2 = scrv(mm)[:, 512:1024]
                op("dve", lambda v, pa=pa, s1=s1: v.tensor_tensor(out=s1, in0=s1, in1=pa[:, :], op=ALU.mult),
                   reads=[pat, scr_tok[mm]], writes=[scr_tok[mm]])
                op("dve", lambda v, pbk=pbk, s2=s2: v.tensor_tensor(out=s2, in0=s2, in1=pbk[:, :], op=ALU.mult),
                   reads=[pbt, scr_tok[mm]], writes=[scr_tok[mm]])
                op("dve", lambda v, m=m, s1=s1, s2=s2: v.tensor_tensor(out=mT[:, m, :], in0=s1, in1=s2, op=ALU.add),
                   reads=[scr_tok[mm]], writes=[mT_tok])
            wdone(u_ab)

        for cb in range(4):
            s, st = wget(blk["wout"][cb])
            wv = wview(s, 16, 512)
            for j in range(JT):
                pb, pt = bank()
                for k in range(16):
                    op("pe", lambda pe, k=k, j=j, pb=pb, wv=wv: pe.matmul(pb[:, :], lhsT=mT[:, k, j * 128:(j + 1) * 128], rhs=wv[:, k, :],
                                                                         start=(k == 0), stop=(k == 15)),
                       reads=[st, mT_tok], writes=[pt])
                op("dve", lambda v, j=j, cb=cb, pb=pb: v.tensor_tensor(out=Xv[j][:, cb * 512:(cb + 1) * 512], in0=Xv[j][:, cb * 512:(cb + 1) * 512],
                                                                      in1=pb[:, :], op=ALU.add),
                   reads=[pt, X_tok[j]], writes=[X_tok[j]])
            wdone(blk["wout"][cb])
        for j in range(JT):
            ti = b * JT + j
            if stage == 1:
                dma("sp", y_d[ti * 128:(ti + 1) * 128, :], Xv[j], reads=[X_tok[j]], writes=[x1s_tok[ti]])
                if b + 1 < NB:
                    stepA_load(b + 1, j)
                    stepA_tile(b + 1, j, (j + 1) % 2)
                continue
            dma("sp", x1_d[ti * 128:(ti + 1) * 128, :], Xv[j], reads=[X_tok[j]], writes=[x1s_tok[ti]])
            xb = xn[j % 2]
            xt = xn_tok[j % 2]
            rms_rstd("act", Xv[j], [X_tok[j]], xb[:, :], [xt], 8)
            op("act", lambda a, j=j, xb=xb: a.activation(out=xb[:, :], in_=Xv[j], func=AF.Copy, scale=sm[:, 8:9]),
               reads=[X_tok[j], sm_tok], writes=[xt])
            x1T = scr[:, 0:2048].rearrange("p (k t) -> p k t", k=16)
            for q in range(4):
                pb, pt = bank()
                for kk in range(4):
                    k = q * 4 + kk
                    op("pe", lambda pe, k=k, kk=kk, pb=pb, j=j: pe.transpose(out=pb[:, kk * 128:(kk + 1) * 128], in_=Xv[j][:, k * 128:(k + 1) * 128],
                                                                            identity=ident_f[:, :]),
                       reads=[X_tok[j]] + CONST, writes=[pt])
                for kk in range(4):
                    k = q * 4 + kk
                    op("dve", lambda v, k=k, kk=kk, pb=pb: v.tensor_scalar(out=x1T[:, k, :], in0=pb[:, kk * 128:(kk + 1) * 128],
                                                                          scalar1=cvec[:, CV_GFFN + k:CV_GFFN + k + 1], scalar2=None, op0=ALU.mult),
                       reads=[pt] + CONST, writes=[scr_tok[0], scr_tok[1]])
            pl, plt = bank()
            for k in range(16):
                op("pe", lambda pe, k=k, pl=pl: pe.matmul(pl[:, 0:36], lhsT=x1T[:, k, :], rhs=wr_sb[:, k, :], start=(k == 0), stop=(k == 15)),
                   reads=[scr_tok[0], scr_tok[1]] + CONST, writes=[plt])
            LG = sm[:, 32:68]
            GL = sm[:, 32:36]
            EL = sm[:, 36:68]
            GOH = sm[:, 68:72]
            PEN = sm[:, 72:76]
            EM = sm[:, 76:108]
            TOP = sm[:, 108:116]
            M0 = sm[:, 116:148]
            M1 = sm[:, 148:180]
            MM = sm[:, 180:212]
            RP = sm[:, 212:244]
            OK_ = sm[:, 244:276]
            JK = sm[:, 276:308]
            SC = sm[:, 308:324]
            GEX = sm[:, 324:328]
            R = [sm_tok]
            op("dve", lambda v, pl=pl: v.scalar_tensor_tensor(out=LG, in0=pl[:, 0:36], scalar=sm[:, 8:9], in1=rbB[:, :],
                                                              op0=ALU.mult, op1=ALU.add), reads=[plt, sm_tok] + CONST, writes=R)
            op("dve", lambda v: v.tensor_reduce(out=SC[:, 0:1], in_=GL, axis=AX.X, op=ALU.max), reads=R, writes=R)
            op("dve", lambda v: v.tensor_scalar(out=GOH, in0=GL, scalar1=SC[:, 0:1], scalar2=None, op0=ALU.is_ge), reads=R, writes=R)
            op("dve", lambda v: v.tensor_scalar(out=SC[:, 1:2], in0=SC[:, 0:1], scalar1=-1.0, scalar2=None, op0=ALU.mult), reads=R, writes=R)
            op("act", lambda a: a.activation(out=GEX, in_=GL, func=AF.Exp, bias=SC[:, 1:2], scale=1.0, accum_out=SC[:, 2:3]),
               reads=R, writes=R)
            op("dve", lambda v: v.reciprocal(out=SC[:, 3:4], in_=SC[:, 2:3]), reads=R, writes=R)
            op("dve", lambda v: v.tensor_scalar(out=PEN, in0=GOH, scalar1=1e30, scalar2=-1e30, op0=ALU.mult, op1=ALU.add), reads=R, writes=R)
            for g4 in range(4):
                op("dve", lambda v, g4=g4: v.tensor_scalar(out=EM[:, g4 * 8:(g4 + 1) * 8], in0=EL[:, g4 * 8:(g4 + 1) * 8],
                                                           scalar1=PEN[:, g4:g4 + 1], scalar2=None, op0=ALU.add), reads=R, writes=R)
            op("dve", lambda v: v.max(out=TOP, in_=EM), reads=R, writes=R)
            op("dve", lambda v: v.tensor_scalar(out=M0, in0=EM, scalar1=TOP[:, 0:1], scalar2=None, op0=ALU.is_equal), reads=R, writes=R)
            op("dve", lambda v: v.tensor_scalar(out=M1, in0=EM, scalar1=TOP[:, 1:2], scalar2=None, op0=ALU.is_equal), reads=R, writes=R)
            op("dve", lambda v: v.tensor_scalar(out=SC[:, 4:5], in0=TOP[:, 0:1], scalar1=-1.0, scalar2=None, op0=ALU.mult), reads=R, writes=R)
            op("act", lambda a: a.activation(out=SC[:, 5:6], in_=TOP[:, 1:2], func=AF.Exp, bias=SC[:, 4:5], scale=1.0), reads=R, writes=R)
            op("dve", lambda v: v.tensor_scalar(out=SC[:, 6:7], in0=SC[:, 5:6], scalar1=1.0, scalar2=None, op0=ALU.add), reads=R, writes=R)
            op("dve", lambda v: v.reciprocal(out=SC[:, 6:7], in_=SC[:, 6:7]), reads=R, writes=R)
            op("dve", lambda v: v.tensor_tensor(out=MM, in0=M0, in1=M1, op=ALU.add), reads=R, writes=R)
            pr, prt = bank()
            op("pe", lambda pe, pr=pr: pe.matmul(pr[:, 0:NE], lhsT=ltri_f[:, :], rhs=MM, start=True, stop=False),
               reads=R + CONST + [rt_tok], writes=[prt])
            op("pe", lambda pe, pr=pr: pe.matmul(pr[:, 0:NE], lhsT=ones_f[:, :], rhs=msum[:, :], start=False, stop=True),
               reads=R + CONST + [rt_tok], writes=[prt])
            op("dve", lambda v, pr=pr: v.tensor_tensor(out=RP, in0=pr[:, 0:NE], in1=offs[:, :], op=ALU.add), reads=[prt] + R + CONST, writes=R)
            op("dve", lambda v, pr=pr: v.tensor_scalar(out=OK_, in0=pr[:, 0:NE], scalar1=float(CAP), scalar2=None, op0=ALU.is_le),
               reads=[prt] + R, writes=R)
            op("dve", lambda v: v.tensor_tensor(out=msum[:, :], in0=msum[:, :], in1=MM, op=ALU.add), reads=R + [rt_tok], writes=[rt_tok])
            for kk, MK in enumerate((M0, M1)):
                op("dve", lambda v, kk=kk, MK=MK: v.tensor_tensor(out=JK, in0=MK, in1=RP, op=ALU.mult), reads=R, writes=R)
                op("dve", lambda v, kk=kk: v.tensor_reduce(out=SC[:, 7 + kk:8 + kk], in_=JK, axis=AX.X, op=ALU.add), reads=R, writes=R)
                op("dve", lambda v, kk=kk, MK=MK: v.tensor_tensor(out=JK, in0=MK, in1=OK_, op=ALU.mult), reads=R, writes=R)
                op("dve", lambda v, kk=kk: v.tensor_reduce(out=SC[:, 9 + kk:10 + kk], in_=JK, axis=AX.X, op=ALU.add), reads=R, writes=R)
                op("dve", lambda v, kk=kk: v.tensor_scalar(out=SC[:, 7 + kk:8 + kk], in0=SC[:, 7 + kk:8 + kk], scalar1=-BIG,
                                                           scalar2=SC[:, 9 + kk:10 + kk], op0=ALU.add, op1=ALU.mult), reads=R, writes=R)
                op("dve", lambda v, kk=kk: v.tensor_scalar(out=SC[:, 7 + kk:8 + kk], in0=SC[:, 7 + kk:8 + kk], scalar1=BIG,
                                                           scalar2=None, op0=ALU.add), reads=R, writes=R)
                op("dve", lambda v, kk=kk, ti=ti: v.tensor_copy(out=idx_all[:, ti, kk:kk + 1], in_=SC[:, 7 + kk:8 + kk]),
                   reads=R, writes=[rt_tok])
            op("dve", lambda v, ti=ti: v.tensor_scalar(out=wt_all[:, ti, 0:1], in0=SC[:, 3:4], scalar1=SC[:, 6:7], scalar2=SC[:, 9:10],
                                                       op0=ALU.mult, op1=ALU.mult), reads=R, writes=[rt_tok])
            op("dve", lambda v, ti=ti: v.tensor_scalar(out=SC[:, 11:12], in0=SC[:, 3:4], scalar1=SC[:, 6:7], scalar2=SC[:, 5:6],
                                                       op0=ALU.mult, op1=ALU.mult), reads=R, writes=R)
            op("dve", lambda v, ti=ti: v.tensor_scalar(out=wt_all[:, ti, 1:2], in0=SC[:, 11:12], scalar1=SC[:, 10:11], scalar2=None,
                                                       op0=ALU.mult), reads=R, writes=[rt_tok])
            for kk in range(2):
                dma("pool", xs_d[:, :], xb[:, :], reads=[xt, rt_tok], writes=[], dtok=xs_tok, indirect=True,
                    out_offset=bass.IndirectOffsetOnAxis(ap=idx_all[:, ti, kk:kk + 1], axis=0), in_offset=None,
                    bounds_check=bc_reg, oob_is_err=False)
            xs_tok.w = {xs_tok.dkey: xs_tok.dval}
            if b + 1 < NB:
                stepA_load(b + 1, j)
                stepA_tile(b + 1, j, (j + 1) % 2)

    if stage == 1:
        fin = {}
        for t in x1s_tok:
            Ctx._merge(fin, t.w)
        cx._wait("sp", fin)
        return nc

    barrier = cx.snapshot()
    moe_slots = [0, 1, 2, 5, 6, 7]
    for i in (5, 6, 7):
        slot_tok[i].w = dict(barrier)
    Ysb = Xv
    Y_tok = [cx.dma_tok(f"Ysb{j}", init=barrier) for j in range(4)]
    XeT = [pages[8][:, i * 4096:(i + 1) * 4096].rearrange("p (k r) -> p k r", k=16) for i in range(2)]
    XeT_tok = [cx.tok(f"XeT{i}", init=barrier) for i in range(2)]
    hidT = [aT[:, :, :].rearrange("p k t -> p (k t)")[:, i * 1024:(i + 1) * 1024].rearrange("p (m r) -> p m r", m=4) for i in range(2)]
    hid_tok = [cx.tok(f"hid{i}", init=barrier) for i in range(2)]
    ys_tok = cx.dma_tok("ys")
    slot_rr[0] = 0
    exp_units = []
    for e in range(NE):
        ug = sched_unit(lambda s, e=e: [(wview(s, 16, 512), wsrc(w_eg_d[e], 0, D, 0, 512))], moe_slots)
        uu = sched_unit(lambda s, e=e: [(wview(s, 16, 512), wsrc(w_eu_d[e], 0, D, 0, 512))], moe_slots)
        ud = sched_unit(lambda s, e=e: [(wview(s, 4, 2048), wsrc(w_ed_d[e], 0, 512, 0, D))], moe_slots)
        exp_units.append((ug, uu, ud))

    def wget_moe(u):
        ws.ensure(u)
        s = ws.units[u][0]
        return s, slot_tok[s]

    ws.ensure(exp_units[0][0] + 5)

    yrr = [0]

    def moe_rows_load(e):
        for r in range(2):
            dma("sp", xn[r][:, :], xs_d[e * CAP + r * 128:e * CAP + (r + 1) * 128, :], reads=[xs_tok], writes=[xn_tok[r]])

    def moe_transposes(e):
        xe = XeT[e % 2]
        xet = XeT_tok[e % 2]
        for r in range(2):
            transposes_bf(xn[r], [xn_tok[r]], lambda k, r=r, xe=xe: xe[:, k, r * 128:(r + 1) * 128], [xet], CV_GFFN, evac_rr)

    moe_rows_load(0)
    moe_transposes(0)
    for e in range(NE):
        ug, uu, ud = exp_units[e]
        xe = XeT[e % 2]
        xet = XeT_tok[e % 2]
        if e + 1 < NE:
            moe_rows_load(e + 1)
        sg_, tg_ = wget_moe(ug)
        su_, tu_ = wget_moe(uu)
        sd_, td_ = wget_moe(ud)
        wg = wview(sg_, 16, 512)
        wu = wview(su_, 16, 512)
        wd = wview(sd_, 4, 2048)
        hd = hidT[e % 2]
        hdt = hid_tok[e % 2]
        gu_banks = [bank() for _ in range(4)]
        for m in range(4):
            pb, pt = gu_banks[m]
            for k in range(16):
                op("pe", lambda pe, k=k, m=m, pb=pb: pe.matmul(pb[:, 0:CAP], lhsT=wg[:, k, m * 128:(m + 1) * 128], rhs=xe[:, k, :],
                                                               start=(k == 0), stop=(k == 15)), reads=[tg_, xet], writes=[pt])
        wdone(ug, 6)
        for m in range(4):
            pb, pt = gu_banks[m]
            for k in range(16):
                op("pe", lambda pe, k=k, m=m, pb=pb: pe.matmul(pb[:, CAP:2 * CAP], lhsT=wu[:, k, m * 128:(m + 1) * 128], rhs=xe[:, k, :],
                                                               start=(k == 0), stop=(k == 15)), reads=[tu_, xet], writes=[pt])
        wdone(uu, 6)
        for m in range(4):
            pb, pt = gu_banks[m]
            si = m % 2 + 2
            sg = scrv(si, CAP)
            op("act", lambda a, pb=pb, sg=sg: a.activation(out=sg, in_=pb[:, 0:CAP], func=AF.Silu), reads=[pt], writes=[scr_tok[si]])
            op("dve", lambda v, pb=pb, sg=sg, m=m: v.tensor_tensor(out=hd[:, m, :], in0=sg, in1=pb[:, CAP:2 * CAP], op=ALU.mult),
               reads=[pt, scr_tok[si]], writes=[hdt])
        if e + 1 < NE:
            moe_transposes(e + 1)
        for r in range(2):
            yi = yrr[0] % 4
            yrr[0] += 1
            for cb in range(4):
                pb, pt = bank()
                for m in range(4):
                    op("pe", lambda pe, m=m, r=r, cb=cb, pb=pb: pe.matmul(pb[:, :], lhsT=hd[:, m, r * 128:(r + 1) * 128],
                                                                         rhs=wd[:, m, cb * 512:(cb + 1) * 512], start=(m == 0), stop=(m == 3)),
                       reads=[td_, hdt], writes=[pt])
                if cb % 2 == 0:
                    op("act", lambda a, yi=yi, cb=cb, pb=pb: a.activation(out=Ysb[yi][:, cb * 512:(cb + 1) * 512], in_=pb[:, :], func=AF.Copy),
                       reads=[pt], writes=[Y_tok[yi]])
                else:
                    op("dve", lambda v, yi=yi, cb=cb, pb=pb: v.tensor_copy(out=Ysb[yi][:, cb * 512:(cb + 1) * 512], in_=pb[:, :]),
                       reads=[pt], writes=[Y_tok[yi]])
            dma("sp", ys_d[e * CAP + r * 128:e * CAP + (r + 1) * 128, :], Ysb[yi], reads=[Y_tok[yi]], writes=[], dtok=ys_tok)
        wdone(ud, 6)
    ys_tok.w = {ys_tok.dkey: ys_tok.dval}

    barrier2 = cx.snapshot()
    pg_pages = [0, 1, 2, 5]
    wpg_tok = cx.dma_tok("wpg", init=barrier2)
    for ci, pgi in enumerate(pg_pages):
        dma("pool", wview(pgi, 16, 512), wsrc(w_pg_d, 0, D, ci * 512, 512), writes=[wpg_tok])
    wpu = pages[6][:, 0:4096].rearrange("p (k n) -> p k n", k=2)
    dma("pool", wpu, w_pu_d.rearrange("(k p) n -> p k n", p=128), writes=[wpg_tok])
    gfin = pages[6][:, 4096:8192].bitcast(F32)
    bpg_b = scr[0:1, 3 * 1088:3 * 1088 + 1024].bitcast(BF16)
    scr_tok[3].w = dict(barrier2)
    bpg_tok = cx.dma_tok("bpg", init=barrier2)
    dma("pool", bpg_b, rows_d[0:1, R_BPG:R_BPG + D], writes=[bpg_tok])
    dma("sp", gfin, rows_d[0:1, R_GFIN:R_GFIN + D].partition_broadcast(128), writes=[wpg_tok])
    Gv = [pages[7 + (i // 2)][:, (i % 2) * 4096:(i % 2 + 1) * 4096].bitcast(F32) for i in range(4)]
    G_tok = [cx.dma_tok(f"G{i}", init=barrier2) for i in range(4)]
    for i in range(4):
        op("dve", lambda v, i=i: v.memset(Gv[i], 0.0), writes=[G_tok[i]])
    Xf_tok = [cx.dma_tok(f"Xf{j}", init=barrier2) for j in range(4)]
    h3T = [aT[:, :, :].rearrange("p k t -> p (k t)")[:, i * 2048:(i + 1) * 2048].rearrange("p (k t) -> p k t", k=16) for i in range(2)]
    h3_tok = [cx.tok(f"h3T{i}", init=barrier2) for i in range(2)]
    pbf = [sb(f"pbf{i}", [128, 256], BF16) for i in range(2)]
    pbf_tok = [cx.dma_tok(f"pbf{i}") for i in range(2)]
    pT = [sb(f"pT{i}", [128, 2, 128], BF16) for i in range(2)]
    pT_tok = [cx.tok(f"pT{i}") for i in range(2)]
    out_tok = cx.dma_tok("out")
    for st_ in scr_tok:
        pass
    def ple_A(ti):
        j = ti % 4
        Xt, Xk = Xv[j], Xf_tok[j]
        dma("sp", Xt, x1_d[ti * 128:(ti + 1) * 128, :], reads=[x1s_tok[ti]], writes=[Xk])
        for kk in range(2):
            gi = (ti % 2) * 2 + kk
            dma("pool", Gv[gi], ys_d[:, :], reads=[ys_tok, rt_tok], writes=[G_tok[gi]], indirect=True, out_offset=None,
                in_offset=bass.IndirectOffsetOnAxis(ap=idx_all[:, ti, kk:kk + 1], axis=0),
                bounds_check=bc_reg, oob_is_err=False)
        dma("pool", pbf[ti % 2][:, :], p_d[ti * 128:(ti + 1) * 128, :], writes=[pbf_tok[ti % 2]])
        for kk in range(2):
            gi = (ti % 2) * 2 + kk
            op("dve", lambda v, gi=gi, ti=ti, kk=kk, Xt=Xt: v.scalar_tensor_tensor(out=Xt, in0=Gv[gi], scalar=wt_all[:, ti, kk:kk + 1], in1=Xt,
                                                                                  op0=ALU.mult, op1=ALU.add),
               reads=[G_tok[gi], rt_tok, Xk], writes=[Xk])
        xb = xn[ti % 2]
        xt = xn_tok[ti % 2]
        rms_rstd("act", Xt, [Xk], xb[:, :], [xt], 9)
        op("act", lambda a, xb=xb, Xt=Xt: a.activation(out=xb[:, :], in_=Xt, func=AF.Copy, scale=sm[:, 9:10]),
           reads=[Xk, sm_tok], writes=[xt])

    def ple_C(ti):
        xb = xn[ti % 2]
        xt = xn_tok[ti % 2]
        h3 = h3T[ti % 2]
        h3t = h3_tok[ti % 2]
        transposes_bf(xb, [xt], lambda k, h3=h3: h3[:, k, :], [h3t], CV_GPLE, evac_rr)
        pb, pt = bank()
        pbb = pb[:, :].bitcast(BF16)
        for kk in range(2):
            op("pe", lambda pe, kk=kk, pbb=pbb, ti=ti: pe.transpose(out=pbb[:, kk * 128:(kk + 1) * 128], in_=pbf[ti % 2][:, kk * 128:(kk + 1) * 128],
                                                                   identity=ident_b[:, :]), reads=[pbf_tok[ti % 2]] + CONST, writes=[pt])
        op("dve", lambda v, pbb=pbb, ti=ti: v.tensor_copy(out=pT[ti % 2][:, :, :], in_=pbb[:, 0:256].rearrange("p (k t) -> p k t", k=2)),
           reads=[pt], writes=[pT_tok[ti % 2]])

    def ple_B(ti):
        j = ti % 4
        Xt, Xk = Xv[j], Xf_tok[j]
        xb = xn[ti % 2]
        xt = xn_tok[ti % 2]
        h3 = h3T[ti % 2]
        h3t = h3_tok[ti % 2]
        for cb in range(4):
            wg_ = wview(pg_pages[cb], 16, 512)
            pg_, pgt = bank()
            for k in range(16):
                op("pe", lambda pe, k=k, pg_=pg_, wg_=wg_, h3=h3: pe.matmul(pg_[:, :], lhsT=h3[:, k, :], rhs=wg_[:, k, :], start=(k == 0), stop=False),
                   reads=[wpg_tok, h3t], writes=[pgt])
            op("pe", lambda pe, pg_=pg_, cb=cb: pe.matmul(pg_[:, :], lhsT=ones_b[0:1, :], rhs=bpg_b[0:1, cb * 512:(cb + 1) * 512], start=False, stop=True),
               reads=CONST + [bpg_tok], writes=[pgt])
            pu_, put = bank()
            for k in range(2):
                op("pe", lambda pe, k=k, pu_=pu_, cb=cb, ti=ti: pe.matmul(pu_[:, :], lhsT=pT[ti % 2][:, k, :], rhs=wpu[:, k, cb * 512:(cb + 1) * 512],
                                                                         start=(k == 0), stop=(k == 1)), reads=[wpg_tok, pT_tok[ti % 2]], writes=[put])
            si = cb % 2
            sg = scrv(si, 512)
            op("act", lambda a, pg_=pg_, sg=sg: a.activation(out=sg, in_=pg_[:, :], func=AF.Sigmoid), reads=[pgt], writes=[scr_tok[si]])
            op("dve", lambda v, pu_=pu_, sg=sg: v.tensor_tensor(out=sg, in0=sg, in1=pu_[:, :], op=ALU.mult), reads=[put, scr_tok[si]], writes=[scr_tok[si]])
            op("dve", lambda v, sg=sg, cb=cb, Xt=Xt: v.tensor_tensor(out=Xt[:, cb * 512:(cb + 1) * 512], in0=Xt[:, cb * 512:(cb + 1) * 512], in1=sg, op=ALU.add),
               reads=[scr_tok[si], Xk], writes=[Xk])
        rms_rstd("act", Xt, [Xk], xb[:, :], [xt], 10)
        op("dve", lambda v, Xt=Xt: v.scalar_tensor_tensor(out=Xt, in0=Xt, scalar=sm[:, 10:11], in1=gfin, op0=ALU.mult, op1=ALU.mult),
           reads=[Xk, sm_tok, wpg_tok], writes=[Xk])
        dma("sp", y_d[ti * 128:(ti + 1) * 128, :], Xt, reads=[Xk], writes=[], dtok=out_tok)

    ple_A(0)
    ple_C(0)
    for ti in range(NT):
        if ti + 1 < NT:
            ple_A(ti + 1)
        ple_B(ti)
        if ti + 1 < NT:
            ple_C(ti + 1)
    if DEBUG_X1:
        dbg_w = nc.dram_tensor("dbg_w", [128, NT * 2], F32, kind="ExternalOutput").ap()
        dbg_i = nc.dram_tensor("dbg_i", [128, NT * 2], I32, kind="ExternalOutput").ap()
        dma("sp", dbg_w[:, :], wt_all[:, :, :].rearrange("p a b -> p (a b)"), reads=[rt_tok], writes=[], dtok=out_tok)
        dma("sp", dbg_i[:, :], idx_all[:, :, :].rearrange("p a b -> p (a b)"), reads=[rt_tok], writes=[], dtok=out_tok)
    cx._wait("sp", {out_tok.dkey: out_tok.dval})
    return nc


_NC_CACHE = {}


def _prep_inputs(inp):
    f = lambda a: np.ascontiguousarray(np.asarray(a, dtype=np.float32))
    x = f(inp["x"]).reshape(4, 4096, D)
    p = f(inp["p"]).reshape(4, 4096, 256)

    def pk(v):
        v = f(v).reshape(-1)
        return v.reshape(-1, 128).T

    cvec = np.concatenate([pk(inp["g_mix"]), pk(inp["g_ffn"]), pk(inp["g_ple"]), pk(inp["pool_scale"]),
                           pk(inp["b_merge_gate"])], axis=1)
    assert cvec.shape == (128, CV_N)
    wrg = f(inp["w_router_group"]).reshape(D, 4)
    wre = f(inp["w_router_expert"]).reshape(4, D, 8).transpose(1, 0, 2).reshape(D, 32)
    wr = np.concatenate([wrg, wre], axis=1)
    wr = wr.reshape(16, 128, 36).transpose(1, 0, 2).reshape(128, 16 * 36)
    rb = np.concatenate([f(inp["b_router_group"]).reshape(-1), f(inp["b_router_expert"]).reshape(-1)])
    shared = {
        "cvec": np.ascontiguousarray(cvec),
        "wr": np.ascontiguousarray(wr),
        "w_in": f(inp["w_in"]).reshape(D, 3072),
        "w_pool": f(inp["w_pool"]).reshape(4, 256, 256),
        "w_ba": f(inp["w_branch_a"]).reshape(1024, D),
        "w_bb": f(inp["w_branch_b"]).reshape(1024, D),
        "w_sp": f(inp["w_spatial"]).reshape(8, 128, 128),
        "w_mg": f(inp["w_merge_gate"]).reshape(D, 2 * D),
        "w_out": f(inp["w_out"]).reshape(D, D),
        "w_eg": f(inp["w_exp_gate"]).reshape(NE, D, 512),
        "w_eu": f(inp["w_exp_up"]).reshape(NE, D, 512),
        "w_ed": f(inp["w_exp_down"]).reshape(NE, 512, D),
        "w_pg": f(inp["w_ple_gate"]).reshape(D, D),
        "w_pu": f(inp["w_ple_up"]).reshape(256, D),
    }
    maps = []
    for c in range(NCORES):
        bi, half = c // 2, c % 2
        t0 = half * T
        rows = np.zeros((1, R_N), np.float32)
        rows[0, R_LNG:R_LNG + 1024] = f(inp["sgu_ln_g"]).reshape(-1)
        rows[0, R_LNB:R_LNB + 1024] = f(inp["sgu_ln_b"]).reshape(-1)
        rows[0, R_BSP:R_BSP + 1024] = f(inp["b_spatial"]).reshape(-1)
        rows[0, R_RB:R_RB + 36] = rb
        rows[0, R_GFIN:R_GFIN + D] = f(inp["g_final"]).reshape(-1)
        rows[0, R_BPG:R_BPG + D] = f(inp["b_ple_gate"]).reshape(-1)
        rows[0, R_POS:R_POS + HALO] = t0 + 1 + np.arange(HALO)
        xh = np.zeros((HALO, D), np.float32)
        if half == 1:
            xh[:] = x[bi, t0 - HALO:t0]
        m = dict(shared)
        m["x"] = np.ascontiguousarray(x[bi, t0:t0 + T])
        m["xh"] = xh
        m["p"] = np.ascontiguousarray(p[bi, t0:t0 + T])
        m["rows"] = rows
        maps.append(m)
    return maps


def kernel(**inputs):
    stage = 2
    if stage not in _NC_CACHE:
        _NC_CACHE[stage] = build(stage)
    nc = _NC_CACHE[stage]
    maps = _prep_inputs(inputs)
    res = run_bass_kernel_spmd(nc, maps, core_ids=list(range(NCORES)))
    out = np.empty((4, 4096, D), np.float32)
    for c in range(NCORES):
        out[c // 2, (c % 2) * T:(c % 2 + 1) * T] = res.results[c]["y"]
    return out
```

```python
import numpy as np
import concourse.bass as bass
import concourse.mybir as mybir
from concourse.bass_utils import run_bass_kernel_spmd

F32 = mybir.dt.float32
BF16 = mybir.dt.bfloat16
I32 = mybir.dt.int32
AF = mybir.ActivationFunctionType
ALU = mybir.AluOpType
AX = mybir.AxisListType

NCORES = 8
T = 2048
D = 2048
NT = T // 128
TB = 512
NB = T // TB
JT = TB // 128
HALO = 16
CAP = 256
NE = 32
EPS = 1e-6
BIG = float(1 << 20)
EPOCH = 6000
SAME_ENGINE_SYNC = True
DEBUG_X1 = False

CV_GMIX, CV_GFFN, CV_GPLE, CV_PSC, CV_BMG, CV_N = 0, 16, 32, 48, 56, 88
R_LNG, R_LNB, R_BSP, R_RB, R_GFIN, R_BPG, R_POS, R_N = 0, 1024, 2048, 3072, 3108, 5156, 7204, 7220


class Tok:
    __slots__ = ("w", "r", "name", "dkey", "dval")

    def __init__(self, name, init=None):
        self.name = name
        self.w = dict(init) if init else {}
        self.r = {}


class Ctx:
    def __init__(self):
        self.nc = bass.Bass("TRN2", target_bir_lowering=False)
        nc = self.nc
        self.eng = dict(pe=nc.tensor, act=nc.scalar, dve=nc.vector, pool=nc.gpsimd, sp=nc.sync)
        self.semh = {}
        self.cur = {}
        self.cnt = {}
        self.known = {e: {} for e in self.eng}
        self.nsem = 0
        for e in self.eng:
            self._new_epoch(e, 0)
        self.dma_sems = []

    def _sem(self, name):
        self.nsem += 1
        return self.nc.semaphore(name).__enter__()

    def _new_epoch(self, e, ep):
        key = (e, ep)
        self.semh[key] = self._sem(f"s_{e}_{ep}")
        self.cur[e] = key
        self.cnt[e] = 0

    def tok(self, name, init=None):
        return Tok(name, init)

    def dma_tok(self, name, init=None):
        t = Tok(name, init)
        key = ("dma", name)
        self.semh[key] = self._sem(f"d_{name}")
        t.dkey = key
        t.dval = 0
        return t

    def snapshot(self):
        d = {}
        for e in self.eng:
            if self.cnt[e] > 0:
                d[self.cur[e]] = self.cnt[e]
            elif self.cur[e][1] > 0:
                d[(e, self.cur[e][1] - 1)] = EPOCH
        for t in self.dma_sems:
            if t.dval > 0:
                d[t.dkey] = t.dval
        return d

    def _wait(self, e, deps):
        eng = self.eng[e]
        kn = self.known[e]
        for key, val in deps.items():
            if kn.get(key, 0) >= val:
                continue
            if key[0] == e and not SAME_ENGINE_SYNC:
                continue
            if key[0] == e and e in ("pe",):
                continue
            eng.wait_ge(self.semh[key], val)
            kn[key] = val

    @staticmethod
    def _merge(d, src):
        for k, v in src.items():
            if d.get(k, 0) < v:
                d[k] = v

    def op(self, e, fn, reads=(), writes=()):
        deps = {}
        for t in reads:
            self._merge(deps, t.w)
        for t in writes:
            self._merge(deps, t.w)
            self._merge(deps, t.r)
        self._wait(e, deps)
        ins = fn(self.eng[e])
        if self.cnt[e] >= EPOCH:
            self._new_epoch(e, self.cur[e][1] + 1)
        self.cnt[e] += 1
        key, val = self.cur[e], self.cnt[e]
        ins.then_inc(self.semh[key], 1)
        for t in reads:
            t.r[key] = val
        for t in writes:
            t.w = {key: val}
            t.r = {}
        return ins

    def dma(self, e, out, in_, reads=(), writes=(), dtok=None, **kw):
        deps = {}
        for t in reads:
            self._merge(deps, t.w)
        for t in writes:
            self._merge(deps, t.w)
            self._merge(deps, t.r)
        self._wait(e, deps)
        if dtok is None:
            dtok = writes[0]
        if dtok not in self.dma_sems:
            self.dma_sems.append(dtok)
        eng = self.eng[e]
        if "indirect" in kw:
            kw = dict(kw)
            kw.pop("indirect")
            ins = eng.indirect_dma_start(out=out, in_=in_, **kw)
        else:
            ins = eng.dma_start(out=out, in_=in_, **kw)
        dtok.dval += 16
        ins.then_inc(self.semh[dtok.dkey], 16)
        key, val = dtok.dkey, dtok.dval
        for t in reads:
            t.r[key] = val
        for t in writes:
            if t is dtok:
                t.w = {key: val}
                t.r = {}
            else:
                t.w = {key: val}
                t.r = {}
        return ins


def build(stage=2):
    cx = Ctx()
    nc = cx.nc
    op, dma = cx.op, cx.dma

    def din(name, shape, dt=F32):
        return nc.dram_tensor(name, list(shape), dt, kind="ExternalInput").ap()

    x_d = din("x", [T, D])
    xh_d = din("xh", [HALO, D])
    p_d = din("p", [T, 256])
    cvec_d = din("cvec", [128, CV_N])
    wr_d = din("wr", [128, 16 * 36])
    rows_d = din("rows", [1, R_N])
    w_in_d = din("w_in", [D, 3072])
    w_pool_d = din("w_pool", [4, 256, 256])
    w_ba_d = din("w_ba", [1024, D])
    w_bb_d = din("w_bb", [1024, D])
    w_sp_d = din("w_sp", [8, 128, 128])
    w_mg_d = din("w_mg", [D, 2 * D])
    w_out_d = din("w_out", [D, D])
    w_eg_d = din("w_eg", [NE, D, 512])
    w_eu_d = din("w_eu", [NE, D, 512])
    w_ed_d = din("w_ed", [NE, 512, D])
    w_pg_d = din("w_pg", [D, D])
    w_pu_d = din("w_pu", [256, D])
    y_d = nc.dram_tensor("y", [T, D], F32, kind="ExternalOutput").ap()
    xs_d = nc.dram_tensor("xs_scr", [NE * CAP, D], BF16).ap()
    ys_d = nc.dram_tensor("ys_scr", [NE * CAP, D], F32).ap()
    x1_d = nc.dram_tensor("x1_scr", [T, D], F32, kind=("ExternalOutput" if DEBUG_X1 else "Internal")).ap()

    def sb(name, shape, dt):
        return nc.alloc_sbuf_tensor("sb_" + name, list(shape), dt)

    bc_reg = nc.gpsimd.to_reg(NE * CAP - 1)
    NPAGE = 9
    pages = [sb(f"pg{i}", [128, 8192], BF16) for i in range(NPAGE)]
    aT = sb("aT", [128, 8, HALO + TB], BF16)
    xn = [sb(f"xn{i}", [128, D], BF16) for i in range(2)]
    scr = sb("scr", [128, 4 * 1088], F32)
    ident_b = sb("ident_b", [128, 128], BF16)
    ident_f = sb("ident_f", [128, 128], F32)
    ltri_f = sb("ltri_f", [128, 128], F32)
    ones_f = sb("ones_f", [128, 128], F32)
    ones_b = sb("ones_b", [1, 128], BF16)
    cvec = sb("cvec", [128, CV_N], F32)
    wr_sb = sb("wr_sb", [128, 16, 36], F32)
    wsT = sb("wsT", [128, 8, 128], BF16)
    bspB = sb("bspB", [128, 8, 128], F32)
    wpool = sb("wpool", [128, 4, 2, 256], BF16)
    lnG = sb("lnG", [128, 1024], F32)
    lnB = sb("lnB", [128, 1024], F32)
    rbB = sb("rbB", [128, 36], F32)
    corr = sb("corr", [128, 4, HALO], F32)
    offs = sb("offs", [128, NE], F32)
    offs_i = sb("offs_i", [128, NE], I32)
    msum = sb("msum", [128, NE], F32)
    wt_all = sb("wt_all", [128, NT, 2], F32)
    idx_all = sb("idx_all", [128, NT, 2], I32)
    sm = sb("sm", [128, 336], F32)
    posb = sb("posb", [128, HALO], F32)
    epsb = sb("epsb", [128, 1], F32)

    psb = [nc.alloc_psum_tensor(f"ps{i}", [128, 512], F32) for i in range(8)]
    ps_tok = [cx.tok(f"ps{i}") for i in range(8)]
    ps_i = [0]

    def bank():
        i = ps_i[0] % 8
        ps_i[0] += 1
        return psb[i], ps_tok[i]

    t_const = cx.dma_tok("const")
    t_cst2 = cx.tok("cst2")

    dma("sp", cvec[:, :], cvec_d[:, :], writes=[t_const])
    dma("sp", wr_sb[:, :, :], wr_d[:, :].rearrange("p (k n) -> p k n", k=16), writes=[t_const])
    dma("sp", lnG[:, :], rows_d[0:1, R_LNG:R_LNG + 1024].partition_broadcast(128), writes=[t_const])
    dma("sp", lnB[:, :], rows_d[0:1, R_LNB:R_LNB + 1024].partition_broadcast(128), writes=[t_const])
    dma("sp", bspB[:, :, :], rows_d[0:1, R_BSP:R_BSP + 1024].partition_broadcast(128), writes=[t_const])
    dma("sp", rbB[:, :], rows_d[0:1, R_RB:R_RB + 36].partition_broadcast(128), writes=[t_const])
    dma("sp", posb[:, :], rows_d[0:1, R_POS:R_POS + HALO].partition_broadcast(128), writes=[t_const])
    dma("pool", wpool[:, :, :, :], w_pool_d.rearrange("g (k p) n -> p g k n", p=128), writes=[t_const])

    op("pool", lambda g: g.memset(ones_f[:, :], 1.0), writes=[t_cst2])
    op("pool", lambda g: g.memset(ident_f[:, :], 1.0), writes=[t_cst2])
    op("pool", lambda g: g.affine_select(out=ident_f[:, :], in_=ident_f[:, :], pattern=[[1, 128]],
                                         compare_op=ALU.is_equal, fill=0.0, base=0, channel_multiplier=-1),
       writes=[t_cst2])
    op("pool", lambda g: g.memset(ltri_f[:, :], 1.0), writes=[t_cst2])
    op("pool", lambda g: g.affine_select(out=ltri_f[:, :], in_=ltri_f[:, :], pattern=[[1, 128]],
                                         compare_op=ALU.is_ge, fill=0.0, base=0, channel_multiplier=-1),
       writes=[t_cst2])
    op("pool", lambda g: g.iota(offs_i[:, :], pattern=[[CAP, NE]], base=-1, channel_multiplier=0), writes=[t_cst2])
    op("dve", lambda v: v.tensor_copy(out=ident_b[:, :], in_=ident_f[:, :]), reads=[t_cst2], writes=[t_cst2])
    op("dve", lambda v: v.tensor_copy(out=offs[:, :], in_=offs_i[:, :]), reads=[t_cst2], writes=[t_cst2])
    op("dve", lambda v: v.memset(ones_b[:, :], 1.0), writes=[t_cst2])
    op("dve", lambda v: v.memset(msum[:, :], 0.0), writes=[t_cst2])
    op("dve", lambda v: v.memset(epsb[:, :], EPS), writes=[t_cst2])
    for gi, w in enumerate((2, 4, 8, 16)):
        op("dve", lambda v, gi=gi, w=w: v.tensor_scalar(out=corr[:, gi, :], in0=posb[:, :], scalar1=float(w),
                                                        scalar2=None, op0=ALU.min),
           reads=[t_const], writes=[t_cst2])
        op("dve", lambda v, gi=gi: v.reciprocal(out=corr[:, gi, :], in_=corr[:, gi, :]), reads=[t_cst2], writes=[t_cst2])
        op("dve", lambda v, gi=gi, w=w: v.tensor_scalar(out=corr[:, gi, :], in0=corr[:, gi, :], scalar1=float(w),
                                                        scalar2=None, op0=ALU.mult),
           reads=[t_cst2], writes=[t_cst2])
    t_wsl = cx.dma_tok("wsl")
    for g8 in range(8):
        stg = scr[:, 0:128]
        dma("sp", stg, w_sp_d[g8], writes=[t_wsl])
        pb, pt = bank()
        op("pe", lambda pe, pb=pb: pe.transpose(out=pb[:, 0:128], in_=scr[:, 0:128], identity=ident_f[:, :]),
           reads=[t_wsl, t_cst2], writes=[pt])
        op("dve", lambda v, pb=pb, g8=g8: v.tensor_copy(out=wsT[:, g8, :], in_=pb[:, 0:128]), reads=[pt], writes=[t_cst2])
        t_wsl.r.update(pt.w)
    op("dve", lambda v: v.memset(wsT[64:128, :, 0:64], 0.0), reads=[t_cst2], writes=[t_cst2])
    for k in range(16):
        op("dve", lambda v, k=k: v.tensor_scalar(out=wr_sb[:, k, :], in0=wr_sb[:, k, :], scalar1=cvec[:, CV_GFFN + k:CV_GFFN + k + 1],
                                                 scalar2=None, op0=ALU.mult), reads=[t_const, t_cst2], writes=[t_cst2])
    CONST = [t_const, t_cst2]

    NSLOT_MIX = 3
    slot_pages = [0, 1, 2]
    slot_tok = {i: cx.dma_tok(f"wslot{i}") for i in range(NPAGE)}

    class WStream:
        def __init__(self):
            self.units = []
            self.issued = 0
            self.slots = list(slot_pages)

        def add(self, parts):
            self.units.append(parts)
            return len(self.units) - 1

        def slot_of(self, i):
            return self.units[i][0]

        def ensure(self, upto):
            while self.issued <= min(upto, len(self.units) - 1):
                pg_i, parts = self.units[self.issued]
                for dst, src in parts:
                    dma("pool", dst, src, writes=[slot_tok[pg_i]])
                self.issued += 1

    ws = WStream()

    def wview(pg_i, kc, n):
        return pages[pg_i][:, 0:kc * n].rearrange("p (k n) -> p k n", k=kc)

    def wsrc(w_ap, r0, nrows, c0, ncols):
        return w_ap[r0:r0 + nrows, c0:c0 + ncols].rearrange("(k p) n -> p k n", p=128)

    unit_seq = []
    slot_rr = [0]

    def next_slot(slots):
        s = slots[slot_rr[0] % len(slots)]
        slot_rr[0] += 1
        return s

    mix_slots = [0, 1, 2]

    def sched_unit(parts_fn, slots):
        s = next_slot(slots)
        return ws.add((s, parts_fn(s)))

    Xv = [pages[3 + (j // 2)][:, (j % 2) * 4096:(j % 2 + 1) * 4096].bitcast(F32) for j in range(4)]
    X_tok = [cx.dma_tok(f"X{j}") for j in range(4)]
    hT = pages[5][:, :].rearrange("p (k t) -> p k t", k=16)
    hT_tok = cx.tok("hT")
    mT = pages[6][:, :].rearrange("p (k t) -> p k t", k=16)
    mT_tok = cx.tok("mT")
    uT = pages[7][:, 0:4096].rearrange("p (k t) -> p k t", k=8)
    uT_tok = cx.tok("uT")
    vT = pages[7][:, 4096:8192].rearrange("p (j c) -> p j c", j=JT)
    vT_tok = cx.tok("vT")
    pzT = pages[8][:, 0:4096].rearrange("p (k t) -> p k t", k=8)
    pz_tok = cx.tok("pzT")
    ypT = pages[8][:, 4096:8192].rearrange("p (k t) -> p k t", k=8)
    yp_tok = cx.tok("ypT")
    aT_tok = cx.tok("aT")
    xn_tok = [cx.dma_tok(f"xn{i}") for i in range(2)]
    scr_tok = [cx.tok(f"scr{i}") for i in range(4)]
    sm_tok = cx.tok("sm")
    rt_tok = cx.tok("route")
    xs_tok = cx.dma_tok("xs")
    x1s_tok = [cx.dma_tok(f"x1s{i}") for i in range(NT)]
    hh_tok = cx.tok("hhalo")
    hhalo = sb("hhalo", [128, 16, HALO], BF16)

    def scrv(i, n=1088):
        return scr[:, i * 1088:i * 1088 + n]

    WIN_ORDER = (4, 5, 0, 1, 2, 3)
    plan = []
    for b in range(NB):
        blk = {}
        blk["win"] = {cb: sched_unit(lambda s, cb=cb: [(wview(s, 16, 512), wsrc(w_in_d, 0, D, cb * 512, 512))], mix_slots)
                      for cb in WIN_ORDER}
        blk["mrg"] = []
        for cb in range(4):
            u_g1 = sched_unit(lambda s, cb=cb: [(wview(s, 16, 512), wsrc(w_mg_d, 0, D, cb * 512, 512))], mix_slots)
            u_g2 = sched_unit(lambda s, cb=cb: [(wview(s, 16, 512), wsrc(w_mg_d, 0, D, D + cb * 512, 512))], mix_slots)
            u_ab = sched_unit(lambda s, cb=cb: [
                (pages[s][:, 0:4096].rearrange("p (k n) -> p k n", k=8), wsrc(w_ba_d, 0, 1024, cb * 512, 512)),
                (pages[s][:, 4096:8192].rearrange("p (k n) -> p k n", k=8), wsrc(w_bb_d, 0, 1024, cb * 512, 512))],
                mix_slots)
            blk["mrg"].append((u_ab, u_g1, u_g2))
        blk["wout"] = [sched_unit(lambda s, cb=cb: [(wview(s, 16, 512), wsrc(w_out_d, 0, D, cb * 512, 512))], mix_slots)
                       for cb in range(4)]
        plan.append(blk)
    n_mix_units = len(ws.units)

    def wget(u):
        ws.ensure(u)
        s = ws.units[u][0]
        return s, slot_tok[s]

    def wdone(u, nslots=3):
        ws.ensure(u + nslots)

    def rms_rstd(e_src, src_ap, src_toks, junk_ap, junk_toks, col, npart=128):
        sc = sm[0:npart, col:col + 1]
        op("act", lambda a: a.activation(out=junk_ap, in_=src_ap, func=AF.Square, accum_out=sc),
           reads=src_toks, writes=junk_toks + [sm_tok])
        op("act", lambda a: a.activation(out=sc, in_=sc, func=AF.Sqrt, scale=1.0 / D, bias=epsb[0:npart, :]),
           reads=[sm_tok] + CONST, writes=[sm_tok])
        op("dve", lambda v: v.reciprocal(out=sc, in_=sc), reads=[sm_tok], writes=[sm_tok])

    def transposes_bf(src_rows_ap, src_toks, dst_fn, dst_toks, gcol, evac_rr):
        for q in range(4):
            pb, pt = bank()
            pbb = pb[:, :].bitcast(BF16)
            for kk in range(4):
                k = q * 4 + kk
                op("pe", lambda pe, k=k, kk=kk, pbb=pbb: pe.transpose(out=pbb[:, kk * 128:(kk + 1) * 128],
                                                                       in_=src_rows_ap[:, k * 128:(k + 1) * 128],
                                                                       identity=ident_b[:, :]),
                   reads=src_toks + CONST, writes=[pt])
            for kk in range(4):
                k = q * 4 + kk
                if (evac_rr[0] % 2) == 0:
                    op("act", lambda a, k=k, kk=kk, pbb=pbb: a.activation(out=dst_fn(k), in_=pbb[:, kk * 128:(kk + 1) * 128],
                                                                          func=AF.Copy, scale=cvec[:, gcol + k:gcol + k + 1]),
                       reads=[pt] + CONST, writes=dst_toks)
                else:
                    op("dve", lambda v, k=k, kk=kk, pbb=pbb: v.tensor_scalar(out=dst_fn(k), in0=pbb[:, kk * 128:(kk + 1) * 128],
                                                                             scalar1=cvec[:, gcol + k:gcol + k + 1], scalar2=None,
                                                                             op0=ALU.mult),
                       reads=[pt] + CONST, writes=dst_toks)
                evac_rr[0] += 1

    evac_rr = [0]

    def emit_layernorm():
            for j in range(JT):
                vg = scrv(j, 1024)
                st6 = sm[:, 16:28]
                op("dve", lambda v, vg=vg: v.bn_stats(out=sm[:, 16:22], in_=vg[:, 0:512]), reads=[scr_tok[j]], writes=[sm_tok])
                op("dve", lambda v, vg=vg: v.bn_stats(out=sm[:, 22:28], in_=vg[:, 512:1024]), reads=[scr_tok[j], sm_tok], writes=[sm_tok])
                op("dve", lambda v: v.bn_aggr(out=sm[:, 28:30], in_=sm[:, 16:28]), reads=[sm_tok], writes=[sm_tok])
                op("act", lambda a: a.activation(out=sm[:, 29:30], in_=sm[:, 29:30], func=AF.Sqrt, scale=1.0, bias=epsb[:, :]),
                   reads=[sm_tok] + CONST, writes=[sm_tok])
                op("dve", lambda v: v.reciprocal(out=sm[:, 29:30], in_=sm[:, 29:30]), reads=[sm_tok], writes=[sm_tok])
                op("dve", lambda v, vg=vg: v.scalar_tensor_tensor(out=vg, in0=vg, scalar=sm[:, 28:29], in1=lnG[:, :],
                                                                  op0=ALU.subtract, op1=ALU.mult),
                   reads=[scr_tok[j], sm_tok] + CONST, writes=[scr_tok[j]])
                op("dve", lambda v, vg=vg, j=j: v.scalar_tensor_tensor(out=vT[:, j, :], in0=vg, scalar=sm[:, 29:30], in1=lnB[:, :],
                                                                       op0=ALU.mult, op1=ALU.add),
                   reads=[scr_tok[j], sm_tok] + CONST, writes=[vT_tok])


    pg6f = pages[6][:, :].bitcast(F32)

    def emit_pool_dve(b):
        W = HALO + TB
        for gi, w in enumerate((2, 4, 8, 16)):
            ca = aT[:, 2 * gi:2 * gi + 2, :]
            bufA = pg6f[:, 0:2 * W].rearrange("p (k t) -> p k t", k=2)
            bufB = pg6f[:, 2 * W:4 * W].rearrange("p (k t) -> p k t", k=2)
            cur, cur_t = ca, aT_tok
            sh = 1
            lvl = 0
            while sh < w:
                dst = bufA if lvl % 2 == 0 else bufB
                lo = 2 * sh - 1
                op("dve", lambda v, dst=dst, cur=cur, lo=lo, sh=sh: v.tensor_tensor(out=dst[:, :, lo:W], in0=cur[:, :, lo:W],
                                                                                     in1=cur[:, :, lo - sh:W - sh], op=ALU.add),
                   reads=[cur_t], writes=[mT_tok])
                cur, cur_t = dst, mT_tok
                sh *= 2
                lvl += 1
            if b == 0:
                for c2 in range(2):
                    op("dve", lambda v, cur=cur, gi=gi, c2=c2: v.tensor_tensor(out=cur[:, c2, HALO:2 * HALO], in0=cur[:, c2, HALO:2 * HALO],
                                                                              in1=corr[:, gi, :], op=ALU.mult),
                       reads=[cur_t] + CONST, writes=[cur_t])
            op("dve", lambda v, cur=cur, ca=ca, gi=gi, w=w: v.scalar_tensor_tensor(out=pzT[:, 2 * gi:2 * gi + 2, :], in0=cur[:, :, HALO:W],
                                                                                   scalar=1.0 / w, in1=ca[:, :, HALO:W],
                                                                                   op0=ALU.mult, op1=ALU.subtract),
               reads=[cur_t, aT_tok], writes=[pz_tok])

    def stepA_halo():
        dma("sp", Xv[0][0:HALO, :], xh_d[:, :], writes=[X_tok[0]])
        rms_rstd("act", Xv[0][0:HALO, :], [X_tok[0]], xn[0][0:HALO, :], [xn_tok[0]], 0, npart=HALO)
        op("act", lambda a: a.activation(out=xn[0][0:HALO, :], in_=Xv[0][0:HALO, :], func=AF.Copy, scale=sm[0:HALO, 0:1]),
           reads=[X_tok[0], sm_tok], writes=[xn_tok[0]])
        for q in range(4):
            pb, pt = bank()
            pbb = pb[:, :].bitcast(BF16)
            for kk in range(4):
                k = q * 4 + kk
                op("pe", lambda pe, k=k, kk=kk, pbb=pbb: pe.transpose(out=pbb[:, kk * HALO:(kk + 1) * HALO],
                                                                       in_=xn[0][0:HALO, k * 128:(k + 1) * 128],
                                                                       identity=ident_b[0:HALO, 0:HALO]),
                   reads=[xn_tok[0]] + CONST, writes=[pt])
            for kk in range(4):
                k = q * 4 + kk
                op("dve", lambda v, k=k, kk=kk, pbb=pbb: v.tensor_scalar(out=hhalo[:, k, :], in0=pbb[:, kk * HALO:(kk + 1) * HALO],
                                                                         scalar1=cvec[:, CV_GMIX + k:CV_GMIX + k + 1],
                                                                         scalar2=None, op0=ALU.mult),
                   reads=[pt] + CONST, writes=[hh_tok])

    def stepA_load(b, j):
        t0 = b * TB
        dma("sp", Xv[j], x_d[t0 + j * 128:t0 + (j + 1) * 128, :], writes=[X_tok[j]])

    def stepA_tile(b, j, xi):
        xb = xn[xi]
        xt = xn_tok[xi]
        rms_rstd("act", Xv[j], [X_tok[j]], xb[:, :], [xt], j)
        op("act", lambda a, j=j, xb=xb: a.activation(out=xb[:, :], in_=Xv[j], func=AF.Copy, scale=sm[:, j:j + 1]),
           reads=[X_tok[j], sm_tok], writes=[xt])
        transposes_bf(xb, [xt], lambda k, j=j: hT[:, k, j * 128:(j + 1) * 128], [hT_tok], CV_GMIX, evac_rr)

    stepA_halo()
    for j in range(JT):
        stepA_load(0, j)
    for j in range(JT):
        stepA_tile(0, j, j % 2)

    for b in range(NB):
        blk = plan[b]
        t0 = b * TB
        if b > 0:
            op("dve", lambda v: v.tensor_copy(out=aT[:, :, 0:HALO], in_=aT[:, :, TB:TB + HALO]), reads=[aT_tok], writes=[aT_tok])
        for cbi, cb in enumerate(WIN_ORDER):
            if cbi > 0:
                wdone(blk["win"][WIN_ORDER[cbi - 1]])
            if cbi == 2:
                emit_layernorm()
            if cbi == 4:
                emit_pool_dve(b)
            s, st = wget(blk["win"][cb])
            wv = wview(s, 16, 512)
            if cb < 4:
                for mm in range(4):
                    m = (cb % 2) * 4 + mm
                    pb, pt = bank()
                    for k in range(16):
                        op("pe", lambda pe, k=k, mm=mm, pb=pb, wv=wv: pe.matmul(pb[:, :], lhsT=wv[:, k, mm * 128:(mm + 1) * 128],
                                                                               rhs=hT[:, k, :], start=(k == 0), stop=(k == 15)),
                           reads=[st, hT_tok], writes=[pt])
                    if cb < 2:
                        op("dve", lambda v, m=m, pb=pb: v.tensor_copy(out=aT[:, m, HALO:HALO + TB], in_=pb[:, :]),
                           reads=[pt], writes=[aT_tok])
                        if b == 0:
                            pb2, pt2 = bank()
                            for k in range(16):
                                op("pe", lambda pe, k=k, mm=mm, pb2=pb2, wv=wv: pe.matmul(pb2[:, 0:HALO], lhsT=wv[:, k, mm * 128:(mm + 1) * 128],
                                                                                         rhs=hhalo[:, k, :], start=(k == 0), stop=(k == 15)),
                                   reads=[st, hh_tok], writes=[pt2])
                            op("dve", lambda v, m=m, pb2=pb2: v.tensor_copy(out=aT[:, m, 0:HALO], in_=pb2[:, 0:HALO]),
                               reads=[pt2], writes=[aT_tok])
                    else:
                        op("act", lambda a, m=m, pb=pb: a.activation(out=uT[:, m, :], in_=pb[:, :], func=AF.Gelu_apprx_tanh),
                           reads=[pt], writes=[uT_tok])
            else:
                half = cb - 4
                for j in range(JT):
                    pb, pt = bank()
                    for k in range(16):
                        op("pe", lambda pe, k=k, j=j, pb=pb, wv=wv: pe.matmul(pb[:, :], lhsT=hT[:, k, j * 128:(j + 1) * 128],
                                                                             rhs=wv[:, k, :], start=(k == 0), stop=(k == 15)),
                           reads=[st, hT_tok], writes=[pt])
                    op("act", lambda a, j=j, half=half, pb=pb: a.activation(out=scrv(j)[:, half * 512:(half + 1) * 512], in_=pb[:, :],
                                                                            func=AF.Gelu_apprx_tanh),
                       reads=[pt], writes=[scr_tok[j]])
        wdone(blk["win"][WIN_ORDER[-1]])
        for gi in range(4):
            for mo in range(2):
                pb, pt = bank()
                for ki in range(2):
                    op("pe", lambda pe, gi=gi, mo=mo, ki=ki, pb=pb: pe.matmul(pb[:, :], lhsT=wpool[:, gi, ki, mo * 128:(mo + 1) * 128],
                                                                             rhs=pzT[:, 2 * gi + ki, :], start=(ki == 0), stop=(ki == 1)),
                       reads=[pz_tok] + CONST, writes=[pt])
                c = 2 * gi + mo
                op("act", lambda a, c=c, pb=pb: a.activation(out=ypT[:, c, :], in_=pb[:, :], func=AF.Copy,
                                                             scale=cvec[:, CV_PSC + c:CV_PSC + c + 1]),
                   reads=[pt] + CONST, writes=[yp_tok])
        for g8 in range(8):
            pb, pt = bank()
            for j in range(JT):
                op("pe", lambda pe, g8=g8, j=j, pb=pb: pe.matmul(pb[:, j * 128:(j + 1) * 128], lhsT=vT[:, j, g8 * 128:(g8 + 1) * 128],
                                                                 rhs=wsT[:, g8, :], start=True, stop=True),
                   reads=[vT_tok] + CONST, writes=[pt])
            tmp = scrv(1, 512)
            for j in range(JT):
                op("dve", lambda v, g8=g8, j=j, pb=pb, tmp=tmp: v.tensor_tensor(out=tmp[:, j * 128:(j + 1) * 128], in0=pb[:, j * 128:(j + 1) * 128],
                                                                               in1=bspB[:, g8, :], op=ALU.add),
                   reads=[pt] + CONST, writes=[scr_tok[1]])
            op("dve", lambda v, g8=g8, tmp=tmp: v.tensor_tensor(out=uT[:, g8, :], in0=uT[:, g8, :], in1=tmp, op=ALU.mult),
               reads=[scr_tok[1], uT_tok], writes=[uT_tok])

        for cb in range(4):
            u_ab, u_g1, u_g2 = blk["mrg"][cb]
            for gi2, ug_ in enumerate((u_g1, u_g2)):
                s_g, t_g = wget(ug_)
                wgm = wview(s_g, 16, 512)
                for mm in range(4):
                    m = cb * 4 + mm
                    p1, p1t = bank()
                    for k in range(16):
                        op("pe", lambda pe, k=k, mm=mm, p1=p1, wgm=wgm: pe.matmul(p1[:, :], lhsT=wgm[:, k, mm * 128:(mm + 1) * 128], rhs=hT[:, k, :],
                                                                                 start=(k == 0), stop=(k == 15)),
                           reads=[t_g, hT_tok], writes=[p1t])
                    sgt = scrv(mm)[:, gi2 * 512:(gi2 + 1) * 512]
                    op("act", lambda a, m=m, p1=p1, sgt=sgt, gi2=gi2: a.activation(out=sgt, in_=p1[:, :], func=AF.Sigmoid,
                                                                                    bias=cvec[:, CV_BMG + 16 * gi2 + m:CV_BMG + 16 * gi2 + m + 1], scale=1.0),
                       reads=[p1t] + CONST, writes=[scr_tok[mm]])
                wdone(ug_)
            s_ab, t_ab = wget(u_ab)
            wa = pages[s_ab][:, 0:4096].rearrange("p (k n) -> p k n", k=8)
            wb = pages[s_ab][:, 4096:8192].rearrange("p (k n) -> p k n", k=8)
            for mm in range(4):
                m = cb * 4 + mm
                pa, pat = bank()
                for k in range(8):
                    op("pe", lambda pe, k=k, mm=mm, pa=pa, wa=wa: pe.matmul(pa[:, :], lhsT=wa[:, k, mm * 128:(mm + 1) * 128], rhs=ypT[:, k, :],
                                                                           start=(k == 0), stop=(k == 7)),
                       reads=[t_ab, yp_tok], writes=[pat])
                pbk, pbt = bank()
                for k in range(8):
                    op("pe", lambda pe, k=k, mm=mm, pbk=pbk, wb=wb: pe.matmul(pbk[:, :], lhsT=wb[:, k, mm * 128:(mm + 1) * 128], rhs=uT[:, k, :],
                                                                             start=(k == 0), stop=(k == 7)),
                       reads=[t_ab, uT_tok], writes=[pbt])
                s1 = scrv(mm)[:, 0:512]
                s2 = scrv(mm)[:, 512:1024]
                op("dve", lambda v, pa=pa, s1=s1: v.tensor_tensor(out=s1, in0=s1, in1=pa[:, :], op=ALU.mult),
                   reads=[pat, scr_tok[mm]], writes=[scr_tok[mm]])
                op("dve", lambda v, pbk=pbk, s2=s2: v.tensor_tensor(out=s2, in0=s2, in1=pbk[:, :], op=ALU.mult),
                   reads=[pbt, scr_tok[mm]], writes=[scr_tok[mm]])
                op("dve", lambda v, m=m, s1=s1, s2=s2: v.tensor_tensor(out=mT[:, m, :], in0=s1, in1=s2, op=ALU.add),
                   reads=[scr_tok[mm]], writes=[mT_tok])
            wdone(u_ab)

        for cb in range(4):
            s, st = wget(blk["wout"][cb])
            wv = wview(s, 16, 512)
            for j in range(JT):
                pb, pt = bank()
                for k in range(16):
                    op("pe", lambda pe, k=k, j=j, pb=pb, wv=wv: pe.matmul(pb[:, :], lhsT=mT[:, k, j * 128:(j + 1) * 128], rhs=wv[:, k, :],
                                                                         start=(k == 0), stop=(k == 15)),
                       reads=[st, mT_tok], writes=[pt])
                op("dve", lambda v, j=j, cb=cb, pb=pb: v.tensor_tensor(out=Xv[j][:, cb * 512:(cb + 1) * 512], in0=Xv[j][:, cb * 512:(cb + 1) * 512],
                                                                      in1=pb[:, :], op=ALU.add),
                   reads=[pt, X_tok[j]], writes=[X_tok[j]])
            wdone(blk["wout"][cb])
        for j in range(JT):
            ti = b * JT + j
            if stage == 1:
                dma("sp", y_d[ti * 128:(ti + 1) * 128, :], Xv[j], reads=[X_tok[j]], writes=[x1s_tok[ti]])
                if b + 1 < NB:
                    stepA_load(b + 1, j)
                    stepA_tile(b + 1, j, (j + 1) % 2)
                continue
            dma("sp", x1_d[ti * 128:(ti + 1) * 128, :], Xv[j], reads=[X_tok[j]], writes=[x1s_tok[ti]])
            xb = xn[j % 2]
            xt = xn_tok[j % 2]
            rms_rstd("act", Xv[j], [X_tok[j]], xb[:, :], [xt], 8)
            op("act", lambda a, j=j, xb=xb: a.activation(out=xb[:, :], in_=Xv[j], func=AF.Copy, scale=sm[:, 8:9]),
               reads=[X_tok[j], sm_tok], writes=[xt])
            x1T = pages[8][:, 0:4096].bitcast(F32).rearrange("p (k t) -> p k t", k=16)
            for q in range(4):
                pb, pt = bank()
                for kk in range(4):
                    k = q * 4 + kk
                    op("pe", lambda pe, k=k, kk=kk, pb=pb, j=j: pe.transpose(out=pb[:, kk * 128:(kk + 1) * 128], in_=Xv[j][:, k * 128:(k + 1) * 128],
                                                                            identity=ident_f[:, :]),
                       reads=[X_tok[j]] + CONST, writes=[pt])
                x1q = x1T[:, q * 4:(q + 1) * 4, :]
                pbq = pb[:, :].rearrange("p (k t) -> p k t", k=4)
                if q % 2 == 0:
                    op("act", lambda a, pbq=pbq, x1q=x1q: a.activation(out=x1q, in_=pbq, func=AF.Copy), reads=[pt], writes=[pz_tok])
                else:
                    op("dve", lambda v, pbq=pbq, x1q=x1q: v.tensor_copy(out=x1q, in_=pbq), reads=[pt], writes=[pz_tok])
            pl, plt = bank()
            for k in range(16):
                op("pe", lambda pe, k=k, pl=pl: pe.matmul(pl[:, 0:36], lhsT=x1T[:, k, :], rhs=wr_sb[:, k, :], start=(k == 0), stop=(k == 15)),
                   reads=[pz_tok] + CONST, writes=[plt])
            if b + 1 < NB:
                stepA_load(b + 1, j)
                stepA_tile(b + 1, j, (j + 1) % 2)
            LG = sm[:, 32:68]
            GL = sm[:, 32:36]
            EL = sm[:, 36:68]
            GOH = sm[:, 68:72]
            PEN = sm[:, 72:76]
            EM = sm[:, 76:108]
            TOP = sm[:, 108:116]
            M0 = sm[:, 116:148]
            M1 = sm[:, 148:180]
            MM = sm[:, 180:212]
            RP = sm[:, 212:244]
            OK_ = sm[:, 244:276]
            JK = sm[:, 276:308]
            SC = sm[:, 308:324]
            GEX = sm[:, 324:328]
            R = [sm_tok]
            op("dve", lambda v, pl=pl: v.scalar_tensor_tensor(out=LG, in0=pl[:, 0:36], scalar=sm[:, 8:9], in1=rbB[:, :],
                                                              op0=ALU.mult, op1=ALU.add), reads=[plt, sm_tok] + CONST, writes=R)
            op("dve", lambda v: v.tensor_reduce(out=SC[:, 0:1], in_=GL, axis=AX.X, op=ALU.max), reads=R, writes=R)
            op("dve", lambda v: v.tensor_scalar(out=GOH, in0=GL, scalar1=SC[:, 0:1], scalar2=None, op0=ALU.is_ge), reads=R, writes=R)
            op("dve", lambda v: v.tensor_scalar(out=SC[:, 1:2], in0=SC[:, 0:1], scalar1=-1.0, scalar2=None, op0=ALU.mult), reads=R, writes=R)
            op("act", lambda a: a.activation(out=GEX, in_=GL, func=AF.Exp, bias=SC[:, 1:2], scale=1.0, accum_out=SC[:, 2:3]),
               reads=R, writes=R)
            op("dve", lambda v: v.reciprocal(out=SC[:, 3:4], in_=SC[:, 2:3]), reads=R, writes=R)
            op("dve", lambda v: v.tensor_scalar(out=PEN, in0=GOH, scalar1=1e30, scalar2=-1e30, op0=ALU.mult, op1=ALU.add), reads=R, writes=R)
            for g4 in range(4):
                op("dve", lambda v, g4=g4: v.tensor_scalar(out=EM[:, g4 * 8:(g4 + 1) * 8], in0=EL[:, g4 * 8:(g4 + 1) * 8],
                                                           scalar1=PEN[:, g4:g4 + 1], scalar2=None, op0=ALU.add), reads=R, writes=R)
            op("dve", lambda v: v.max(out=TOP, in_=EM), reads=R, writes=R)
            op("dve", lambda v: v.tensor_scalar(out=M0, in0=EM, scalar1=TOP[:, 0:1], scalar2=None, op0=ALU.is_equal), reads=R, writes=R)
            op("dve", lambda v: v.tensor_scalar(out=M1, in0=EM, scalar1=TOP[:, 1:2], scalar2=None, op0=ALU.is_equal), reads=R, writes=R)
            op("dve", lambda v: v.tensor_scalar(out=SC[:, 4:5], in0=TOP[:, 0:1], scalar1=-1.0, scalar2=None, op0=ALU.mult), reads=R, writes=R)
            op("act", lambda a: a.activation(out=SC[:, 5:6], in_=TOP[:, 1:2], func=AF.Exp, bias=SC[:, 4:5], scale=1.0), reads=R, writes=R)
            op("dve", lambda v: v.tensor_scalar(out=SC[:, 6:7], in0=SC[:, 5:6], scalar1=1.0, scalar2=None, op0=ALU.add), reads=R, writes=R)
            op("dve", lambda v: v.reciprocal(out=SC[:, 6:7], in_=SC[:, 6:7]), reads=R, writes=R)
            op("dve", lambda v: v.tensor_tensor(out=MM, in0=M0, in1=M1, op=ALU.add), reads=R, writes=R)
            pr, prt = bank()
            op("pe", lambda pe, pr=pr: pe.matmul(pr[:, 0:NE], lhsT=ltri_f[:, :], rhs=MM, start=True, stop=False),
               reads=R + CONST + [rt_tok], writes=[prt])
            op("pe", lambda pe, pr=pr: pe.matmul(pr[:, 0:NE], lhsT=ones_f[:, :], rhs=msum[:, :], start=False, stop=True),
               reads=R + CONST + [rt_tok], writes=[prt])
            op("dve", lambda v, pr=pr: v.tensor_tensor(out=RP, in0=pr[:, 0:NE], in1=offs[:, :], op=ALU.add), reads=[prt] + R + CONST, writes=R)
            op("dve", lambda v, pr=pr: v.tensor_scalar(out=OK_, in0=pr[:, 0:NE], scalar1=float(CAP), scalar2=None, op0=ALU.is_le),
               reads=[prt] + R, writes=R)
            op("dve", lambda v: v.tensor_tensor(out=msum[:, :], in0=msum[:, :], in1=MM, op=ALU.add), reads=R + [rt_tok], writes=[rt_tok])
            for kk, MK in enumerate((M0, M1)):
                op("dve", lambda v, kk=kk, MK=MK: v.tensor_tensor(out=JK, in0=MK, in1=RP, op=ALU.mult), reads=R, writes=R)
                op("dve", lambda v, kk=kk: v.tensor_reduce(out=SC[:, 7 + kk:8 + kk], in_=JK, axis=AX.X, op=ALU.add), reads=R, writes=R)
                op("dve", lambda v, kk=kk, MK=MK: v.tensor_tensor(out=JK, in0=MK, in1=OK_, op=ALU.mult), reads=R, writes=R)
                op("dve", lambda v, kk=kk: v.tensor_reduce(out=SC[:, 9 + kk:10 + kk], in_=JK, axis=AX.X, op=ALU.add), reads=R, writes=R)
                op("dve", lambda v, kk=kk: v.tensor_scalar(out=SC[:, 7 + kk:8 + kk], in0=SC[:, 7 + kk:8 + kk], scalar1=-BIG,
                                                           scalar2=SC[:, 9 + kk:10 + kk], op0=ALU.add, op1=ALU.mult), reads=R, writes=R)
                op("dve", lambda v, kk=kk: v.tensor_scalar(out=SC[:, 7 + kk:8 + kk], in0=SC[:, 7 + kk:8 + kk], scalar1=BIG,
                                                           scalar2=None, op0=ALU.add), reads=R, writes=R)
                op("dve", lambda v, kk=kk, ti=ti: v.tensor_copy(out=idx_all[:, ti, kk:kk + 1], in_=SC[:, 7 + kk:8 + kk]),
                   reads=R, writes=[rt_tok])
            op("dve", lambda v, ti=ti: v.tensor_scalar(out=wt_all[:, ti, 0:1], in0=SC[:, 3:4], scalar1=SC[:, 6:7], scalar2=SC[:, 9:10],
                                                       op0=ALU.mult, op1=ALU.mult), reads=R, writes=[rt_tok])
            op("dve", lambda v, ti=ti: v.tensor_scalar(out=SC[:, 11:12], in0=SC[:, 3:4], scalar1=SC[:, 6:7], scalar2=SC[:, 5:6],
                                                       op0=ALU.mult, op1=ALU.mult), reads=R, writes=R)
            op("dve", lambda v, ti=ti: v.tensor_scalar(out=wt_all[:, ti, 1:2], in0=SC[:, 11:12], scalar1=SC[:, 10:11], scalar2=None,
                                                       op0=ALU.mult), reads=R, writes=[rt_tok])
            for kk in range(2):
                dma("pool", xs_d[:, :], xb[:, :], reads=[xt, rt_tok], writes=[], dtok=xs_tok, indirect=True,
                    out_offset=bass.IndirectOffsetOnAxis(ap=idx_all[:, ti, kk:kk + 1], axis=0), in_offset=None,
                    bounds_check=bc_reg, oob_is_err=False)
            xs_tok.w = {xs_tok.dkey: xs_tok.dval}

    if stage == 1:
        fin = {}
        for t in x1s_tok:
            Ctx._merge(fin, t.w)
        cx._wait("sp", fin)
        return nc

    barrier = cx.snapshot()
    moe_slots = [0, 1, 2, 5, 6, 7]
    for i in (5, 6, 7):
        slot_tok[i].w = dict(barrier)
    Ysb = Xv
    Y_tok = [cx.dma_tok(f"Ysb{j}", init=barrier) for j in range(4)]
    XeT = [pages[8][:, i * 4096:(i + 1) * 4096].rearrange("p (k r) -> p k r", k=16) for i in range(2)]
    XeT_tok = [cx.tok(f"XeT{i}", init=barrier) for i in range(2)]
    hidT = [aT[:, :, :].rearrange("p k t -> p (k t)")[:, i * 1024:(i + 1) * 1024].rearrange("p (m r) -> p m r", m=4) for i in range(2)]
    hid_tok = [cx.tok(f"hid{i}", init=barrier) for i in range(2)]
    ys_tok = cx.dma_tok("ys")
    slot_rr[0] = 0
    exp_units = []
    for e in range(NE):
        ug = sched_unit(lambda s, e=e: [(wview(s, 16, 512), wsrc(w_eg_d[e], 0, D, 0, 512))], moe_slots)
        uu = sched_unit(lambda s, e=e: [(wview(s, 16, 512), wsrc(w_eu_d[e], 0, D, 0, 512))], moe_slots)
        ud = sched_unit(lambda s, e=e: [(wview(s, 4, 2048), wsrc(w_ed_d[e], 0, 512, 0, D))], moe_slots)
        exp_units.append((ug, uu, ud))

    def wget_moe(u):
        ws.ensure(u)
        s = ws.units[u][0]
        return s, slot_tok[s]

    ws.ensure(exp_units[0][0] + 5)

    yrr = [0]

    def moe_rows_load(e):
        for r in range(2):
            dma("sp", xn[r][:, :], xs_d[e * CAP + r * 128:e * CAP + (r + 1) * 128, :], reads=[xs_tok], writes=[xn_tok[r]])

    def moe_transposes(e):
        xe = XeT[e % 2]
        xet = XeT_tok[e % 2]
        for r in range(2):
            transposes_bf(xn[r], [xn_tok[r]], lambda k, r=r, xe=xe: xe[:, k, r * 128:(r + 1) * 128], [xet], CV_GFFN, evac_rr)

    moe_rows_load(0)
    moe_transposes(0)
    for e in range(NE):
        ug, uu, ud = exp_units[e]
        xe = XeT[e % 2]
        xet = XeT_tok[e % 2]
        if e + 1 < NE:
            moe_rows_load(e + 1)
        sg_, tg_ = wget_moe(ug)
        su_, tu_ = wget_moe(uu)
        sd_, td_ = wget_moe(ud)
        wg = wview(sg_, 16, 512)
        wu = wview(su_, 16, 512)
        wd = wview(sd_, 4, 2048)
        hd = hidT[e % 2]
        hdt = hid_tok[e % 2]
        gu_banks = [bank() for _ in range(4)]
        for m in range(4):
            pb, pt = gu_banks[m]
            for k in range(16):
                op("pe", lambda pe, k=k, m=m, pb=pb: pe.matmul(pb[:, 0:CAP], lhsT=wg[:, k, m * 128:(m + 1) * 128], rhs=xe[:, k, :],
                                                               start=(k == 0), stop=(k == 15)), reads=[tg_, xet], writes=[pt])
        wdone(ug, 6)
        for m in range(4):
            pb, pt = gu_banks[m]
            for k in range(16):
                op("pe", lambda pe, k=k, m=m, pb=pb: pe.matmul(pb[:, CAP:2 * CAP], lhsT=wu[:, k, m * 128:(m + 1) * 128], rhs=xe[:, k, :],
                                                               start=(k == 0), stop=(k == 15)), reads=[tu_, xet], writes=[pt])
        wdone(uu, 6)
        for m in range(4):
            pb, pt = gu_banks[m]
            si = m % 2 + 2
            sg = scrv(si, CAP)
            op("act", lambda a, pb=pb, sg=sg: a.activation(out=sg, in_=pb[:, 0:CAP], func=AF.Silu), reads=[pt], writes=[scr_tok[si]])
            op("dve", lambda v, pb=pb, sg=sg, m=m: v.tensor_tensor(out=hd[:, m, :], in0=sg, in1=pb[:, CAP:2 * CAP], op=ALU.mult),
               reads=[pt, scr_tok[si]], writes=[hdt])
        if e + 1 < NE:
            moe_transposes(e + 1)
        for r in range(2):
            yi = yrr[0] % 4
            yrr[0] += 1
            for cb in range(4):
                pb, pt = bank()
                for m in range(4):
                    op("pe", lambda pe, m=m, r=r, cb=cb, pb=pb: pe.matmul(pb[:, :], lhsT=hd[:, m, r * 128:(r + 1) * 128],
                                                                         rhs=wd[:, m, cb * 512:(cb + 1) * 512], start=(m == 0), stop=(m == 3)),
                       reads=[td_, hdt], writes=[pt])
                if cb % 2 == 0:
                    op("act", lambda a, yi=yi, cb=cb, pb=pb: a.activation(out=Ysb[yi][:, cb * 512:(cb + 1) * 512], in_=pb[:, :], func=AF.Copy),
                       reads=[pt], writes=[Y_tok[yi]])
                else:
                    op("dve", lambda v, yi=yi, cb=cb, pb=pb: v.tensor_copy(out=Ysb[yi][:, cb * 512:(cb + 1) * 512], in_=pb[:, :]),
                       reads=[pt], writes=[Y_tok[yi]])
            dma("sp", ys_d[e * CAP + r * 128:e * CAP + (r + 1) * 128, :], Ysb[yi], reads=[Y_tok[yi]], writes=[], dtok=ys_tok)
        wdone(ud, 6)
    ys_tok.w = {ys_tok.dkey: ys_tok.dval}

    barrier2 = cx.snapshot()
    pg_pages = [0, 1, 2, 5]
    for ci, pgi in enumerate(pg_pages):
        dma("pool", wview(pgi, 16, 512), wsrc(w_pg_d, 0, D, ci * 512, 512), writes=[slot_tok[pgi]])
    wpg_tok = slot_tok[6]
    wpu = pages[6][:, 0:4096].rearrange("p (k n) -> p k n", k=2)
    dma("pool", wpu, w_pu_d.rearrange("(k p) n -> p k n", p=128), writes=[wpg_tok])
    gfin = pages[6][:, 4096:8192].bitcast(F32)
    bpg_b = scr[0:1, 3 * 1088:3 * 1088 + 1024].bitcast(BF16)
    scr_tok[3].w = dict(barrier2)
    bpg_tok = cx.dma_tok("bpg", init=barrier2)
    dma("pool", bpg_b, rows_d[0:1, R_BPG:R_BPG + D], writes=[bpg_tok])
    dma("sp", gfin, rows_d[0:1, R_GFIN:R_GFIN + D].partition_broadcast(128), writes=[wpg_tok])
    Gv = [pages[7 + (i // 2)][:, (i % 2) * 4096:(i % 2 + 1) * 4096].bitcast(F32) for i in range(4)]
    G_tok = [cx.dma_tok(f"G{i}", init=barrier2) for i in range(4)]
    for i in range(4):
        op("dve", lambda v, i=i: v.memset(Gv[i], 0.0), writes=[G_tok[i]])
    Xf_tok = [cx.dma_tok(f"Xf{j}", init=barrier2) for j in range(4)]
    h3T = [aT[:, :, :].rearrange("p k t -> p (k t)")[:, i * 2048:(i + 1) * 2048].rearrange("p (k t) -> p k t", k=16) for i in range(2)]
    h3_tok = [cx.tok(f"h3T{i}", init=barrier2) for i in range(2)]
    pbf = [sb(f"pbf{i}", [128, 256], BF16) for i in range(2)]
    pbf_tok = [cx.dma_tok(f"pbf{i}") for i in range(2)]
    pT = [sb(f"pT{i}", [128, 2, 128], BF16) for i in range(2)]
    pT_tok = [cx.tok(f"pT{i}") for i in range(2)]
    out_tok = cx.dma_tok("out")
    for st_ in scr_tok:
        pass
    def ple_A(ti):
        j = ti % 4
        Xt, Xk = Xv[j], Xf_tok[j]
        dma("sp", Xt, x1_d[ti * 128:(ti + 1) * 128, :], reads=[x1s_tok[ti]], writes=[Xk])
        for kk in range(2):
            gi = (ti % 2) * 2 + kk
            dma("pool", Gv[gi], ys_d[:, :], reads=[ys_tok, rt_tok], writes=[G_tok[gi]], indirect=True, out_offset=None,
                in_offset=bass.IndirectOffsetOnAxis(ap=idx_all[:, ti, kk:kk + 1], axis=0),
                bounds_check=bc_reg, oob_is_err=False)
        dma("pool", pbf[ti % 2][:, :], p_d[ti * 128:(ti + 1) * 128, :], writes=[pbf_tok[ti % 2]])
        for kk in range(2):
            gi = (ti % 2) * 2 + kk
            op("dve", lambda v, gi=gi, ti=ti, kk=kk, Xt=Xt: v.scalar_tensor_tensor(out=Xt, in0=Gv[gi], scalar=wt_all[:, ti, kk:kk + 1], in1=Xt,
                                                                                  op0=ALU.mult, op1=ALU.add),
               reads=[G_tok[gi], rt_tok, Xk], writes=[Xk])
        xb = xn[ti % 2]
        xt = xn_tok[ti % 2]
        rms_rstd("act", Xt, [Xk], xb[:, :], [xt], 9)
        op("act", lambda a, xb=xb, Xt=Xt: a.activation(out=xb[:, :], in_=Xt, func=AF.Copy, scale=sm[:, 9:10]),
           reads=[Xk, sm_tok], writes=[xt])

    def ple_C(ti):
        xb = xn[ti % 2]
        xt = xn_tok[ti % 2]
        h3 = h3T[ti % 2]
        h3t = h3_tok[ti % 2]
        transposes_bf(xb, [xt], lambda k, h3=h3: h3[:, k, :], [h3t], CV_GPLE, evac_rr)
        pb, pt = bank()
        pbb = pb[:, :].bitcast(BF16)
        for kk in range(2):
            op("pe", lambda pe, kk=kk, pbb=pbb, ti=ti: pe.transpose(out=pbb[:, kk * 128:(kk + 1) * 128], in_=pbf[ti % 2][:, kk * 128:(kk + 1) * 128],
                                                                   identity=ident_b[:, :]), reads=[pbf_tok[ti % 2]] + CONST, writes=[pt])
        op("dve", lambda v, pbb=pbb, ti=ti: v.tensor_copy(out=pT[ti % 2][:, :, :], in_=pbb[:, 0:256].rearrange("p (k t) -> p k t", k=2)),
           reads=[pt], writes=[pT_tok[ti % 2]])

    def ple_B(ti):
        j = ti % 4
        Xt, Xk = Xv[j], Xf_tok[j]
        xb = xn[ti % 2]
        xt = xn_tok[ti % 2]
        h3 = h3T[ti % 2]
        h3t = h3_tok[ti % 2]
        for cb in range(4):
            wg_ = wview(pg_pages[cb], 16, 512)
            pg_, pgt = bank()
            for k in range(16):
                op("pe", lambda pe, k=k, pg_=pg_, wg_=wg_, h3=h3: pe.matmul(pg_[:, :], lhsT=h3[:, k, :], rhs=wg_[:, k, :], start=(k == 0), stop=False),
                   reads=[slot_tok[pg_pages[cb]], h3t], writes=[pgt])
            op("pe", lambda pe, pg_=pg_, cb=cb: pe.matmul(pg_[:, :], lhsT=ones_b[0:1, :], rhs=bpg_b[0:1, cb * 512:(cb + 1) * 512], start=False, stop=True),
               reads=CONST + [bpg_tok], writes=[pgt])
            pu_, put = bank()
            for k in range(2):
                op("pe", lambda pe, k=k, pu_=pu_, cb=cb, ti=ti: pe.matmul(pu_[:, :], lhsT=pT[ti % 2][:, k, :], rhs=wpu[:, k, cb * 512:(cb + 1) * 512],
                                                                         start=(k == 0), stop=(k == 1)), reads=[wpg_tok, pT_tok[ti % 2]], writes=[put])
            si = cb % 2
            sg = scrv(si, 512)
            op("act", lambda a, pg_=pg_, sg=sg: a.activation(out=sg, in_=pg_[:, :], func=AF.Sigmoid), reads=[pgt], writes=[scr_tok[si]])
            op("dve", lambda v, pu_=pu_, sg=sg: v.tensor_tensor(out=sg, in0=sg, in1=pu_[:, :], op=ALU.mult), reads=[put, scr_tok[si]], writes=[scr_tok[si]])
            op("dve", lambda v, sg=sg, cb=cb, Xt=Xt: v.tensor_tensor(out=Xt[:, cb * 512:(cb + 1) * 512], in0=Xt[:, cb * 512:(cb + 1) * 512], in1=sg, op=ALU.add),
               reads=[scr_tok[si], Xk], writes=[Xk])

    def ple_B2(ti):
        j = ti % 4
        Xt, Xk = Xv[j], Xf_tok[j]
        xb = xn[ti % 2]
        xt = xn_tok[ti % 2]
        rms_rstd("act", Xt, [Xk], xb[:, :], [xt], 10)
        op("dve", lambda v, Xt=Xt: v.scalar_tensor_tensor(out=Xt, in0=Xt, scalar=sm[:, 10:11], in1=gfin, op0=ALU.mult, op1=ALU.mult),
           reads=[Xk, sm_tok, wpg_tok], writes=[Xk])
        dma("sp", y_d[ti * 128:(ti + 1) * 128, :], Xt, reads=[Xk], writes=[], dtok=out_tok)

    ple_A(0)
    ple_C(0)
    for ti in range(NT):
        if ti + 1 < NT:
            ple_A(ti + 1)
        ple_B(ti)
        if ti + 1 < NT:
            ple_C(ti + 1)
        ple_B2(ti)
    if DEBUG_X1:
        dbg_w = nc.dram_tensor("dbg_w", [128, NT * 2], F32, kind="ExternalOutput").ap()
        dbg_i = nc.dram_tensor("dbg_i", [128, NT * 2], I32, kind="ExternalOutput").ap()
        dma("sp", dbg_w[:, :], wt_all[:, :, :].rearrange("p a b -> p (a b)"), reads=[rt_tok], writes=[], dtok=out_tok)
        dma("sp", dbg_i[:, :], idx_all[:, :, :].rearrange("p a b -> p (a b)"), reads=[rt_tok], writes=[], dtok=out_tok)
    cx._wait("sp", {out_tok.dkey: out_tok.dval})
    return nc


_NC_CACHE = {}


def _prep_inputs(inp):
    f = lambda a: np.ascontiguousarray(np.asarray(a, dtype=np.float32))
    x = f(inp["x"]).reshape(4, 4096, D)
    p = f(inp["p"]).reshape(4, 4096, 256)

    def pk(v):
        v = f(v).reshape(-1)
        return v.reshape(-1, 128).T

    cvec = np.concatenate([pk(inp["g_mix"]), pk(inp["g_ffn"]), pk(inp["g_ple"]), pk(inp["pool_scale"]),
                           pk(inp["b_merge_gate"])], axis=1)
    assert cvec.shape == (128, CV_N)
    wrg = f(inp["w_router_group"]).reshape(D, 4)
    wre = f(inp["w_router_expert"]).reshape(4, D, 8).transpose(1, 0, 2).reshape(D, 32)
    wr = np.concatenate([wrg, wre], axis=1)
    wr = wr.reshape(16, 128, 36).transpose(1, 0, 2).reshape(128, 16 * 36)
    rb = np.concatenate([f(inp["b_router_group"]).reshape(-1), f(inp["b_router_expert"]).reshape(-1)])
    shared = {
        "cvec": np.ascontiguousarray(cvec),
        "wr": np.ascontiguousarray(wr),
        "w_in": f(inp["w_in"]).reshape(D, 3072),
        "w_pool": f(inp["w_pool"]).reshape(4, 256, 256),
        "w_ba": f(inp["w_branch_a"]).reshape(1024, D),
        "w_bb": f(inp["w_branch_b"]).reshape(1024, D),
        "w_sp": f(inp["w_spatial"]).reshape(8, 128, 128),
        "w_mg": f(inp["w_merge_gate"]).reshape(D, 2 * D),
        "w_out": f(inp["w_out"]).reshape(D, D),
        "w_eg": f(inp["w_exp_gate"]).reshape(NE, D, 512),
        "w_eu": f(inp["w_exp_up"]).reshape(NE, D, 512),
        "w_ed": f(inp["w_exp_down"]).reshape(NE, 512, D),
        "w_pg": f(inp["w_ple_gate"]).reshape(D, D),
        "w_pu": f(inp["w_ple_up"]).reshape(256, D),
    }
    maps = []
    for c in range(NCORES):
        bi, half = c // 2, c % 2
        t0 = half * T
        rows = np.zeros((1, R_N), np.float32)
        rows[0, R_LNG:R_LNG + 1024] = f(inp["sgu_ln_g"]).reshape(-1)
        rows[0, R_LNB:R_LNB + 1024] = f(inp["sgu_ln_b"]).reshape(-1)
        rows[0, R_BSP:R_BSP + 1024] = f(inp["b_spatial"]).reshape(-1)
        rows[0, R_RB:R_RB + 36] = rb
        rows[0, R_GFIN:R_GFIN + D] = f(inp["g_final"]).reshape(-1)
        rows[0, R_BPG:R_BPG + D] = f(inp["b_ple_gate"]).reshape(-1)
        rows[0, R_POS:R_POS + HALO] = t0 + 1 + np.arange(HALO)
        xh = np.zeros((HALO, D), np.float32)
        if half == 1:
            xh[:] = x[bi, t0 - HALO:t0]
        m = dict(shared)
        m["x"] = np.ascontiguousarray(x[bi, t0:t0 + T])
        m["xh"] = xh
        m["p"] = np.ascontiguousarray(p[bi, t0:t0 + T])
        m["rows"] = rows
        maps.append(m)
    return maps


def kernel(**inputs):
    stage = 2
    if stage not in _NC_CACHE:
        _NC_CACHE[stage] = build(stage)
    nc = _NC_CACHE[stage]
    maps = _prep_inputs(inputs)
    res = run_bass_kernel_spmd(nc, maps, core_ids=list(range(NCORES)))
    out = np.empty((4, 4096, D), np.float32)
    for c in range(NCORES):
        out[c // 2, (c % 2) * T:(c % 2 + 1) * T] = res.results[c]["y"]
    return out
```

```python
import numpy as np
import concourse.bass as bass
import concourse.mybir as mybir
from concourse.bass_utils import run_bass_kernel_spmd

F32 = mybir.dt.float32
BF16 = mybir.dt.bfloat16
I32 = mybir.dt.int32
AF = mybir.ActivationFunctionType
ALU = mybir.AluOpType
AX = mybir.AxisListType

NCORES = 8
T = 2048
D = 2048
NT = T // 128
TB = 512
NB = T // TB
JT = TB // 128
HALO = 16
CAP = 256
NE = 32
EPS = 1e-6
BIG = float(1 << 20)
EPOCH = 6000
SAME_ENGINE_SYNC = True
DEBUG_X1 = False

CV_GMIX, CV_GFFN, CV_GPLE, CV_PSC, CV_BMG, CV_N = 0, 16, 32, 48, 56, 88
R_LNG, R_LNB, R_BSP, R_RB, R_GFIN, R_BPG, R_POS, R_N = 0, 1024, 2048, 3072, 3108, 5156, 7204, 7220


class Tok:
    __slots__ = ("w", "r", "name", "dkey", "dval")

    def __init__(self, name, init=None):
        self.name = name
        self.w = dict(init) if init else {}
        self.r = {}


class Ctx:
    def __init__(self):
        self.nc = bass.Bass("TRN2", target_bir_lowering=False)
        nc = self.nc
        self.eng = dict(pe=nc.tensor, act=nc.scalar, dve=nc.vector, pool=nc.gpsimd, sp=nc.sync)
        self.semh = {}
        self.cur = {}
        self.cnt = {}
        self.known = {e: {} for e in self.eng}
        self.nsem = 0
        for e in self.eng:
            self._new_epoch(e, 0)
        self.dma_sems = []

    def _sem(self, name):
        self.nsem += 1
        return self.nc.semaphore(name).__enter__()

    def _new_epoch(self, e, ep):
        key = (e, ep)
        self.semh[key] = self._sem(f"s_{e}_{ep}")
        self.cur[e] = key
        self.cnt[e] = 0

    def tok(self, name, init=None):
        return Tok(name, init)

    def dma_tok(self, name, init=None):
        t = Tok(name, init)
        key = ("dma", name)
        self.semh[key] = self._sem(f"d_{name}")
        t.dkey = key
        t.dval = 0
        return t

    def snapshot(self):
        d = {}
        for e in self.eng:
            if self.cnt[e] > 0:
                d[self.cur[e]] = self.cnt[e]
            elif self.cur[e][1] > 0:
                d[(e, self.cur[e][1] - 1)] = EPOCH
        for t in self.dma_sems:
            if t.dval > 0:
                d[t.dkey] = t.dval
        return d

    def _wait(self, e, deps):
        eng = self.eng[e]
        kn = self.known[e]
        for key, val in deps.items():
            if kn.get(key, 0) >= val:
                continue
            if key[0] == e and not SAME_ENGINE_SYNC:
                continue
            if key[0] == e and e in ("pe",):
                continue
            eng.wait_ge(self.semh[key], val)
            kn[key] = val

    @staticmethod
    def _merge(d, src):
        for k, v in src.items():
            if d.get(k, 0) < v:
                d[k] = v

    def op(self, e, fn, reads=(), writes=()):
        deps = {}
        for t in reads:
            self._merge(deps, t.w)
        for t in writes:
            self._merge(deps, t.w)
            self._merge(deps, t.r)
        self._wait(e, deps)
        ins = fn(self.eng[e])
        if self.cnt[e] >= EPOCH:
            self._new_epoch(e, self.cur[e][1] + 1)
        self.cnt[e] += 1
        key, val = self.cur[e], self.cnt[e]
        ins.then_inc(self.semh[key], 1)
        for t in reads:
            t.r[key] = val
        for t in writes:
            t.w = {key: val}
            t.r = {}
        return ins

    def dma(self, e, out, in_, reads=(), writes=(), dtok=None, **kw):
        deps = {}
        for t in reads:
            self._merge(deps, t.w)
        for t in writes:
            self._merge(deps, t.w)
            self._merge(deps, t.r)
        self._wait(e, deps)
        if dtok is None:
            dtok = writes[0]
        if dtok not in self.dma_sems:
            self.dma_sems.append(dtok)
        eng = self.eng[e]
        if "indirect" in kw:
            kw = dict(kw)
            kw.pop("indirect")
            ins = eng.indirect_dma_start(out=out, in_=in_, **kw)
        else:
            ins = eng.dma_start(out=out, in_=in_, **kw)
        dtok.dval += 16
        ins.then_inc(self.semh[dtok.dkey], 16)
        key, val = dtok.dkey, dtok.dval
        for t in reads:
            t.r[key] = val
        for t in writes:
            if t is dtok:
                t.w = {key: val}
                t.r = {}
            else:
                t.w = {key: val}
                t.r = {}
        return ins


def build(stage=2):
    cx = Ctx()
    nc = cx.nc
    op, dma = cx.op, cx.dma

    def din(name, shape, dt=F32):
        return nc.dram_tensor(name, list(shape), dt, kind="ExternalInput").ap()

    x_d = din("x", [T, D])
    xh_d = din("xh", [HALO, D])
    p_d = din("p", [T, 256])
    cvec_d = din("cvec", [128, CV_N])
    wr_d = din("wr", [128, 16 * 36])
    rows_d = din("rows", [1, R_N])
    w_in_d = din("w_in", [D, 3072])
    w_pool_d = din("w_pool", [4, 256, 256])
    w_ba_d = din("w_ba", [1024, D])
    w_bb_d = din("w_bb", [1024, D])
    w_sp_d = din("w_sp", [8, 128, 128])
    w_mg_d = din("w_mg", [D, 2 * D])
    w_out_d = din("w_out", [D, D])
    w_eg_d = din("w_eg", [NE, D, 512])
    w_eu_d = din("w_eu", [NE, D, 512])
    w_ed_d = din("w_ed", [NE, 512, D])
    w_pg_d = din("w_pg", [D, D])
    w_pu_d = din("w_pu", [256, D])
    y_d = nc.dram_tensor("y", [T, D], F32, kind="ExternalOutput").ap()
    xs_d = nc.dram_tensor("xs_scr", [NE * CAP, D], BF16).ap()
    ys_d = nc.dram_tensor("ys_scr", [NE * CAP, D], F32).ap()
    x1_d = nc.dram_tensor("x1_scr", [T, D], F32, kind=("ExternalOutput" if DEBUG_X1 else "Internal")).ap()

    def sb(name, shape, dt):
        return nc.alloc_sbuf_tensor("sb_" + name, list(shape), dt)

    bc_reg = nc.gpsimd.to_reg(NE * CAP - 1)
    NPAGE = 9
    pages = [sb(f"pg{i}", [128, 8192], BF16) for i in range(NPAGE)]
    aT = sb("aT", [128, 8, HALO + TB], BF16)
    xn = [sb(f"xn{i}", [128, D], BF16) for i in range(2)]
    scr = sb("scr", [128, 4 * 1088], F32)
    ident_b = sb("ident_b", [128, 128], BF16)
    ident_f = sb("ident_f", [128, 128], F32)
    ltri_f = sb("ltri_f", [128, 128], F32)
    ones_f = sb("ones_f", [128, 128], F32)
    ones_b = sb("ones_b", [1, 128], BF16)
    cvec = sb("cvec", [128, CV_N], F32)
    wr_sb = sb("wr_sb", [128, 16, 36], F32)
    wsT = sb("wsT", [128, 8, 128], BF16)
    bspB = sb("bspB", [128, 8, 128], F32)
    wpool = sb("wpool", [128, 4, 2, 256], BF16)
    lnG = sb("lnG", [128, 1024], F32)
    lnB = sb("lnB", [128, 1024], F32)
    rbB = sb("rbB", [128, 36], F32)
    corr = sb("corr", [128, 4, HALO], F32)
    offs = sb("offs", [128, NE], F32)
    offs_i = sb("offs_i", [128, NE], I32)
    msum = sb("msum", [128, NE], F32)
    wt_all = sb("wt_all", [128, NT, 2], F32)
    idx_all = sb("idx_all", [128, NT, 2], I32)
    sm = sb("sm", [128, 336], F32)
    posb = sb("posb", [128, HALO], F32)
    rs2_all = sb("rs2_all", [128, NT], F32)
    LG4 = sb("LG4", [128, JT, 36], F32)
    epsb = sb("epsb", [128, 1], F32)

    psb = [nc.alloc_psum_tensor(f"ps{i}", [128, 512], F32) for i in range(8)]
    ps_tok = [cx.tok(f"ps{i}") for i in range(8)]
    ps_i = [0]

    def bank():
        i = ps_i[0] % 8
        ps_i[0] += 1
        return psb[i], ps_tok[i]

    t_const = cx.dma_tok("const")
    t_cst2 = cx.tok("cst2")

    dma("sp", cvec[:, :], cvec_d[:, :], writes=[t_const])
    dma("sp", wr_sb[:, :, :], wr_d[:, :].rearrange("p (k n) -> p k n", k=16), writes=[t_const])
    dma("sp", lnG[:, :], rows_d[0:1, R_LNG:R_LNG + 1024].partition_broadcast(128), writes=[t_const])
    dma("sp", lnB[:, :], rows_d[0:1, R_LNB:R_LNB + 1024].partition_broadcast(128), writes=[t_const])
    dma("sp", bspB[:, :, :], rows_d[0:1, R_BSP:R_BSP + 1024].partition_broadcast(128), writes=[t_const])
    dma("sp", rbB[:, :], rows_d[0:1, R_RB:R_RB + 36].partition_broadcast(128), writes=[t_const])
    dma("sp", posb[:, :], rows_d[0:1, R_POS:R_POS + HALO].partition_broadcast(128), writes=[t_const])
    dma("pool", wpool[:, :, :, :], w_pool_d.rearrange("g (k p) n -> p g k n", p=128), writes=[t_const])

    op("pool", lambda g: g.memset(ones_f[:, :], 1.0), writes=[t_cst2])
    op("pool", lambda g: g.memset(ident_f[:, :], 1.0), writes=[t_cst2])
    op("pool", lambda g: g.affine_select(out=ident_f[:, :], in_=ident_f[:, :], pattern=[[1, 128]],
                                         compare_op=ALU.is_equal, fill=0.0, base=0, channel_multiplier=-1),
       writes=[t_cst2])
    op("pool", lambda g: g.memset(ltri_f[:, :], 1.0), writes=[t_cst2])
    op("pool", lambda g: g.affine_select(out=ltri_f[:, :], in_=ltri_f[:, :], pattern=[[1, 128]],
                                         compare_op=ALU.is_ge, fill=0.0, base=0, channel_multiplier=-1),
       writes=[t_cst2])
    op("pool", lambda g: g.iota(offs_i[:, :], pattern=[[CAP, NE]], base=-1, channel_multiplier=0), writes=[t_cst2])
    op("dve", lambda v: v.tensor_copy(out=ident_b[:, :], in_=ident_f[:, :]), reads=[t_cst2], writes=[t_cst2])
    op("dve", lambda v: v.tensor_copy(out=offs[:, :], in_=offs_i[:, :]), reads=[t_cst2], writes=[t_cst2])
    op("dve", lambda v: v.memset(ones_b[:, :], 1.0), writes=[t_cst2])
    op("dve", lambda v: v.memset(msum[:, :], 0.0), writes=[t_cst2])
    op("dve", lambda v: v.memset(epsb[:, :], EPS), writes=[t_cst2])
    for gi, w in enumerate((2, 4, 8, 16)):
        op("dve", lambda v, gi=gi, w=w: v.tensor_scalar(out=corr[:, gi, :], in0=posb[:, :], scalar1=float(w),
                                                        scalar2=None, op0=ALU.min),
           reads=[t_const], writes=[t_cst2])
        op("dve", lambda v, gi=gi: v.reciprocal(out=corr[:, gi, :], in_=corr[:, gi, :]), reads=[t_cst2], writes=[t_cst2])
        op("dve", lambda v, gi=gi, w=w: v.tensor_scalar(out=corr[:, gi, :], in0=corr[:, gi, :], scalar1=float(w),
                                                        scalar2=None, op0=ALU.mult),
           reads=[t_cst2], writes=[t_cst2])
    t_wsl = cx.dma_tok("wsl")
    for g8 in range(8):
        stg = scr[:, 0:128]
        dma("sp", stg, w_sp_d[g8], writes=[t_wsl])
        pb, pt = bank()
        op("pe", lambda pe, pb=pb: pe.transpose(out=pb[:, 0:128], in_=scr[:, 0:128], identity=ident_f[:, :]),
           reads=[t_wsl, t_cst2], writes=[pt])
        op("dve", lambda v, pb=pb, g8=g8: v.tensor_copy(out=wsT[:, g8, :], in_=pb[:, 0:128]), reads=[pt], writes=[t_cst2])
        t_wsl.r.update(pt.w)
    op("dve", lambda v: v.memset(wsT[64:128, :, 0:64], 0.0), reads=[t_cst2], writes=[t_cst2])
    for k in range(16):
        op("dve", lambda v, k=k: v.tensor_scalar(out=wr_sb[:, k, :], in0=wr_sb[:, k, :], scalar1=cvec[:, CV_GFFN + k:CV_GFFN + k + 1],
                                                 scalar2=None, op0=ALU.mult), reads=[t_const, t_cst2], writes=[t_cst2])
    CONST = [t_const, t_cst2]

    NSLOT_MIX = 3
    slot_pages = [0, 1, 2]
    slot_tok = {i: cx.dma_tok(f"wslot{i}") for i in range(NPAGE)}

    class WStream:
        def __init__(self):
            self.units = []
            self.issued = 0
            self.slots = list(slot_pages)

        def add(self, parts):
            self.units.append(parts)
            return len(self.units) - 1

        def slot_of(self, i):
            return self.units[i][0]

        def ensure(self, upto):
            while self.issued <= min(upto, len(self.units) - 1):
                pg_i, parts = self.units[self.issued]
                for dst, src in parts:
                    dma("pool", dst, src, writes=[slot_tok[pg_i]])
                self.issued += 1

    ws = WStream()

    def wview(pg_i, kc, n):
        return pages[pg_i][:, 0:kc * n].rearrange("p (k n) -> p k n", k=kc)

    def wsrc(w_ap, r0, nrows, c0, ncols):
        return w_ap[r0:r0 + nrows, c0:c0 + ncols].rearrange("(k p) n -> p k n", p=128)

    unit_seq = []
    slot_rr = [0]

    def next_slot(slots):
        s = slots[slot_rr[0] % len(slots)]
        slot_rr[0] += 1
        return s

    mix_slots = [0, 1, 2]

    def sched_unit(parts_fn, slots):
        s = next_slot(slots)
        return ws.add((s, parts_fn(s)))

    Xv = [pages[3 + (j // 2)][:, (j % 2) * 4096:(j % 2 + 1) * 4096].bitcast(F32) for j in range(4)]
    X_tok = [cx.dma_tok(f"X{j}") for j in range(4)]
    hT = pages[5][:, :].rearrange("p (k t) -> p k t", k=16)
    hT_tok = cx.tok("hT")
    mT = pages[6][:, :].rearrange("p (k t) -> p k t", k=16)
    mT_tok = cx.tok("mT")
    uT = pages[7][:, 0:4096].rearrange("p (k t) -> p k t", k=8)
    uT_tok = cx.tok("uT")
    vT = pages[7][:, 4096:8192].rearrange("p (j c) -> p j c", j=JT)
    vT_tok = cx.tok("vT")
    pzT = pages[8][:, 0:4096].rearrange("p (k t) -> p k t", k=8)
    pz_tok = cx.tok("pzT")
    ypT = pages[8][:, 4096:8192].rearrange("p (k t) -> p k t", k=8)
    yp_tok = cx.tok("ypT")
    aT_tok = cx.tok("aT")
    xn_tok = [cx.dma_tok(f"xn{i}") for i in range(2)]
    scr_tok = [cx.tok(f"scr{i}") for i in range(4)]
    sm_tok = cx.tok("sm")
    rt_tok = cx.tok("route")
    xs_tok = cx.dma_tok("xs")
    x1s_tok = [cx.dma_tok(f"x1s{i}") for i in range(NT)]
    hh_tok = cx.tok("hhalo")
    hhalo = sb("hhalo", [128, 16, HALO], BF16)

    def scrv(i, n=1088):
        return scr[:, i * 1088:i * 1088 + n]

    WIN_ORDER = (4, 5, 0, 1, 2, 3)
    plan = []
    for b in range(NB):
        blk = {}
        blk["win"] = {cb: sched_unit(lambda s, cb=cb: [(wview(s, 16, 512), wsrc(w_in_d, 0, D, cb * 512, 512))], mix_slots)
                      for cb in WIN_ORDER}
        blk["mrg"] = []
        for cb in range(4):
            u_g1 = sched_unit(lambda s, cb=cb: [(wview(s, 16, 512), wsrc(w_mg_d, 0, D, cb * 512, 512))], mix_slots)
            u_g2 = sched_unit(lambda s, cb=cb: [(wview(s, 16, 512), wsrc(w_mg_d, 0, D, D + cb * 512, 512))], mix_slots)
            u_ab = sched_unit(lambda s, cb=cb: [
                (pages[s][:, 0:4096].rearrange("p (k n) -> p k n", k=8), wsrc(w_ba_d, 0, 1024, cb * 512, 512)),
                (pages[s][:, 4096:8192].rearrange("p (k n) -> p k n", k=8), wsrc(w_bb_d, 0, 1024, cb * 512, 512))],
                mix_slots)
            blk["mrg"].append((u_ab, u_g1, u_g2))
        blk["wout"] = [sched_unit(lambda s, cb=cb: [(wview(s, 16, 512), wsrc(w_out_d, 0, D, cb * 512, 512))], mix_slots)
                       for cb in range(4)]
        plan.append(blk)
    n_mix_units = len(ws.units)

    def wget(u):
        ws.ensure(u)
        s = ws.units[u][0]
        return s, slot_tok[s]

    def wdone(u, nslots=3):
        ws.ensure(u + nslots)

    def rms_rstd(e_src, src_ap, src_toks, junk_ap, junk_toks, col, npart=128, dst=None):
        sc = sm[0:npart, col:col + 1] if dst is None else dst
        op("act", lambda a: a.activation(out=junk_ap, in_=src_ap, func=AF.Square, accum_out=sc),
           reads=src_toks, writes=junk_toks + [sm_tok])
        op("act", lambda a: a.activation(out=sc, in_=sc, func=AF.Sqrt, scale=1.0 / D, bias=epsb[0:npart, :]),
           reads=[sm_tok] + CONST, writes=[sm_tok])
        op("dve", lambda v: v.reciprocal(out=sc, in_=sc), reads=[sm_tok], writes=[sm_tok])

    def transposes_bf(src_rows_ap, src_toks, dst_fn, dst_toks, gcol, evac_rr):
        for q in range(4):
            pb, pt = bank()
            pbb = pb[:, :].bitcast(BF16)
            for kk in range(4):
                k = q * 4 + kk
                op("pe", lambda pe, k=k, kk=kk, pbb=pbb: pe.transpose(out=pbb[:, kk * 128:(kk + 1) * 128],
                                                                       in_=src_rows_ap[:, k * 128:(k + 1) * 128],
                                                                       identity=ident_b[:, :]),
                   reads=src_toks + CONST, writes=[pt])
            for kk in range(4):
                k = q * 4 + kk
                if (evac_rr[0] % 2) == 0:
                    op("act", lambda a, k=k, kk=kk, pbb=pbb: a.activation(out=dst_fn(k), in_=pbb[:, kk * 128:(kk + 1) * 128],
                                                                          func=AF.Copy, scale=cvec[:, gcol + k:gcol + k + 1]),
                       reads=[pt] + CONST, writes=dst_toks)
                else:
                    op("dve", lambda v, k=k, kk=kk, pbb=pbb: v.tensor_scalar(out=dst_fn(k), in0=pbb[:, kk * 128:(kk + 1) * 128],
                                                                             scalar1=cvec[:, gcol + k:gcol + k + 1], scalar2=None,
                                                                             op0=ALU.mult),
                       reads=[pt] + CONST, writes=dst_toks)
                evac_rr[0] += 1

    evac_rr = [0]

    def emit_layernorm():
            for j in range(JT):
                vg = scrv(j, 1024)
                st6 = sm[:, 16:28]
                op("dve", lambda v, vg=vg: v.bn_stats(out=sm[:, 16:22], in_=vg[:, 0:512]), reads=[scr_tok[j]], writes=[sm_tok])
                op("dve", lambda v, vg=vg: v.bn_stats(out=sm[:, 22:28], in_=vg[:, 512:1024]), reads=[scr_tok[j], sm_tok], writes=[sm_tok])
                op("dve", lambda v: v.bn_aggr(out=sm[:, 28:30], in_=sm[:, 16:28]), reads=[sm_tok], writes=[sm_tok])
                op("act", lambda a: a.activation(out=sm[:, 29:30], in_=sm[:, 29:30], func=AF.Sqrt, scale=1.0, bias=epsb[:, :]),
                   reads=[sm_tok] + CONST, writes=[sm_tok])
                op("dve", lambda v: v.reciprocal(out=sm[:, 29:30], in_=sm[:, 29:30]), reads=[sm_tok], writes=[sm_tok])
                op("dve", lambda v, vg=vg: v.scalar_tensor_tensor(out=vg, in0=vg, scalar=sm[:, 28:29], in1=lnG[:, :],
                                                                  op0=ALU.subtract, op1=ALU.mult),
                   reads=[scr_tok[j], sm_tok] + CONST, writes=[scr_tok[j]])
                op("dve", lambda v, vg=vg, j=j: v.scalar_tensor_tensor(out=vT[:, j, :], in0=vg, scalar=sm[:, 29:30], in1=lnB[:, :],
                                                                       op0=ALU.mult, op1=ALU.add),
                   reads=[scr_tok[j], sm_tok] + CONST, writes=[vT_tok])


    pg6f = pages[6][:, :].bitcast(F32)

    def emit_pool_dve(b):
        W = HALO + TB
        for gi, w in enumerate((2, 4, 8, 16)):
            ca = aT[:, 2 * gi:2 * gi + 2, :]
            bufA = pg6f[:, 0:2 * W].rearrange("p (k t) -> p k t", k=2)
            bufB = pg6f[:, 2 * W:4 * W].rearrange("p (k t) -> p k t", k=2)
            cur, cur_t = ca, aT_tok
            sh = 1
            lvl = 0
            while sh < w:
                dst = bufA if lvl % 2 == 0 else bufB
                lo = 2 * sh - 1
                op("dve", lambda v, dst=dst, cur=cur, lo=lo, sh=sh: v.tensor_tensor(out=dst[:, :, lo:W], in0=cur[:, :, lo:W],
                                                                                     in1=cur[:, :, lo - sh:W - sh], op=ALU.add),
                   reads=[cur_t], writes=[mT_tok])
                cur, cur_t = dst, mT_tok
                sh *= 2
                lvl += 1
            if b == 0:
                for c2 in range(2):
                    op("dve", lambda v, cur=cur, gi=gi, c2=c2: v.tensor_tensor(out=cur[:, c2, HALO:2 * HALO], in0=cur[:, c2, HALO:2 * HALO],
                                                                              in1=corr[:, gi, :], op=ALU.mult),
                       reads=[cur_t] + CONST, writes=[cur_t])
            op("dve", lambda v, cur=cur, ca=ca, gi=gi, w=w: v.scalar_tensor_tensor(out=pzT[:, 2 * gi:2 * gi + 2, :], in0=cur[:, :, HALO:W],
                                                                                   scalar=1.0 / w, in1=ca[:, :, HALO:W],
                                                                                   op0=ALU.mult, op1=ALU.subtract),
               reads=[cur_t, aT_tok], writes=[pz_tok])

    def stepA_halo():
        dma("sp", Xv[0][0:HALO, :], xh_d[:, :], writes=[X_tok[0]])
        rms_rstd("act", Xv[0][0:HALO, :], [X_tok[0]], xn[0][0:HALO, :], [xn_tok[0]], 0, npart=HALO)
        op("act", lambda a: a.activation(out=xn[0][0:HALO, :], in_=Xv[0][0:HALO, :], func=AF.Copy, scale=sm[0:HALO, 0:1]),
           reads=[X_tok[0], sm_tok], writes=[xn_tok[0]])
        for q in range(4):
            pb, pt = bank()
            pbb = pb[:, :].bitcast(BF16)
            for kk in range(4):
                k = q * 4 + kk
                op("pe", lambda pe, k=k, kk=kk, pbb=pbb: pe.transpose(out=pbb[:, kk * HALO:(kk + 1) * HALO],
                                                                       in_=xn[0][0:HALO, k * 128:(k + 1) * 128],
                                                                       identity=ident_b[0:HALO, 0:HALO]),
                   reads=[xn_tok[0]] + CONST, writes=[pt])
            for kk in range(4):
                k = q * 4 + kk
                op("dve", lambda v, k=k, kk=kk, pbb=pbb: v.tensor_scalar(out=hhalo[:, k, :], in0=pbb[:, kk * HALO:(kk + 1) * HALO],
                                                                         scalar1=cvec[:, CV_GMIX + k:CV_GMIX + k + 1],
                                                                         scalar2=None, op0=ALU.mult),
                   reads=[pt] + CONST, writes=[hh_tok])

    def stepA_load(b, j):
        t0 = b * TB
        dma("sp", Xv[j], x_d[t0 + j * 128:t0 + (j + 1) * 128, :], writes=[X_tok[j]])

    def stepA_tile(b, j, xi):
        xb = xn[xi]
        xt = xn_tok[xi]
        rms_rstd("act", Xv[j], [X_tok[j]], xb[:, :], [xt], j)
        op("act", lambda a, j=j, xb=xb: a.activation(out=xb[:, :], in_=Xv[j], func=AF.Copy, scale=sm[:, j:j + 1]),
           reads=[X_tok[j], sm_tok], writes=[xt])
        transposes_bf(xb, [xt], lambda k, j=j: hT[:, k, j * 128:(j + 1) * 128], [hT_tok], CV_GMIX, evac_rr)

    pending_chain = []
    pending_scatter = []
    lg_tok = cx.tok("lg")

    def emit_chain(b, j):
        if True:
            ti = b * JT + j
            LG = sm[:, 32:68]
            GL = sm[:, 32:36]
            EL = sm[:, 36:68]
            GOH = sm[:, 68:72]
            PEN = sm[:, 72:76]
            EM = sm[:, 76:108]
            TOP = sm[:, 108:116]
            M0 = sm[:, 116:148]
            M1 = sm[:, 148:180]
            MM = sm[:, 180:212]
            RP = sm[:, 212:244]
            OK_ = sm[:, 244:276]
            JK = sm[:, 276:308]
            SC = sm[:, 308:324]
            GEX = sm[:, 324:328]
            R = [sm_tok]
            op("dve", lambda v, j=j: v.tensor_copy(out=LG, in_=LG4[:, j, :]), reads=[lg_tok, sm_tok], writes=R)
            op("dve", lambda v: v.tensor_reduce(out=SC[:, 0:1], in_=GL, axis=AX.X, op=ALU.max), reads=R, writes=R)
            op("dve", lambda v: v.tensor_scalar(out=GOH, in0=GL, scalar1=SC[:, 0:1], scalar2=None, op0=ALU.is_ge), reads=R, writes=R)
            op("dve", lambda v: v.tensor_scalar(out=SC[:, 1:2], in0=SC[:, 0:1], scalar1=-1.0, scalar2=None, op0=ALU.mult), reads=R, writes=R)
            op("act", lambda a: a.activation(out=GEX, in_=GL, func=AF.Exp, bias=SC[:, 1:2], scale=1.0, accum_out=SC[:, 2:3]),
               reads=R, writes=R)
            op("dve", lambda v: v.reciprocal(out=SC[:, 3:4], in_=SC[:, 2:3]), reads=R, writes=R)
            op("dve", lambda v: v.tensor_scalar(out=PEN, in0=GOH, scalar1=1e30, scalar2=-1e30, op0=ALU.mult, op1=ALU.add), reads=R, writes=R)
            for g4 in range(4):
                op("dve", lambda v, g4=g4: v.tensor_scalar(out=EM[:, g4 * 8:(g4 + 1) * 8], in0=EL[:, g4 * 8:(g4 + 1) * 8],
                                                           scalar1=PEN[:, g4:g4 + 1], scalar2=None, op0=ALU.add), reads=R, writes=R)
            op("dve", lambda v: v.max(out=TOP, in_=EM), reads=R, writes=R)
            op("dve", lambda v: v.tensor_scalar(out=M0, in0=EM, scalar1=TOP[:, 0:1], scalar2=None, op0=ALU.is_equal), reads=R, writes=R)
            op("dve", lambda v: v.tensor_scalar(out=M1, in0=EM, scalar1=TOP[:, 1:2], scalar2=None, op0=ALU.is_equal), reads=R, writes=R)
            op("dve", lambda v: v.tensor_scalar(out=SC[:, 4:5], in0=TOP[:, 0:1], scalar1=-1.0, scalar2=None, op0=ALU.mult), reads=R, writes=R)
            op("act", lambda a: a.activation(out=SC[:, 5:6], in_=TOP[:, 1:2], func=AF.Exp, bias=SC[:, 4:5], scale=1.0), reads=R, writes=R)
            op("dve", lambda v: v.tensor_scalar(out=SC[:, 6:7], in0=SC[:, 5:6], scalar1=1.0, scalar2=None, op0=ALU.add), reads=R, writes=R)
            op("dve", lambda v: v.reciprocal(out=SC[:, 6:7], in_=SC[:, 6:7]), reads=R, writes=R)
            op("dve", lambda v: v.tensor_tensor(out=MM, in0=M0, in1=M1, op=ALU.add), reads=R, writes=R)
            pr, prt = bank()
            op("pe", lambda pe, pr=pr: pe.matmul(pr[:, 0:NE], lhsT=ltri_f[:, :], rhs=MM, start=True, stop=False),
               reads=R + CONST + [rt_tok], writes=[prt])
            op("pe", lambda pe, pr=pr: pe.matmul(pr[:, 0:NE], lhsT=ones_f[:, :], rhs=msum[:, :], start=False, stop=True),
               reads=R + CONST + [rt_tok], writes=[prt])
            op("dve", lambda v, pr=pr: v.tensor_tensor(out=RP, in0=pr[:, 0:NE], in1=offs[:, :], op=ALU.add), reads=[prt] + R + CONST, writes=R)
            op("dve", lambda v, pr=pr: v.tensor_scalar(out=OK_, in0=pr[:, 0:NE], scalar1=float(CAP), scalar2=None, op0=ALU.is_le),
               reads=[prt] + R, writes=R)
            op("dve", lambda v: v.tensor_tensor(out=msum[:, :], in0=msum[:, :], in1=MM, op=ALU.add), reads=R + [rt_tok], writes=[rt_tok])
            for kk, MK in enumerate((M0, M1)):
                op("dve", lambda v, kk=kk, MK=MK: v.tensor_tensor(out=JK, in0=MK, in1=RP, op=ALU.mult), reads=R, writes=R)
                op("dve", lambda v, kk=kk: v.tensor_reduce(out=SC[:, 7 + kk:8 + kk], in_=JK, axis=AX.X, op=ALU.add), reads=R, writes=R)
                op("dve", lambda v, kk=kk, MK=MK: v.tensor_tensor(out=JK, in0=MK, in1=OK_, op=ALU.mult), reads=R, writes=R)
                op("dve", lambda v, kk=kk: v.tensor_reduce(out=SC[:, 9 + kk:10 + kk], in_=JK, axis=AX.X, op=ALU.add), reads=R, writes=R)
                op("dve", lambda v, kk=kk: v.tensor_scalar(out=SC[:, 7 + kk:8 + kk], in0=SC[:, 7 + kk:8 + kk], scalar1=-BIG,
                                                           scalar2=SC[:, 9 + kk:10 + kk], op0=ALU.add, op1=ALU.mult), reads=R, writes=R)
                op("dve", lambda v, kk=kk: v.tensor_scalar(out=SC[:, 7 + kk:8 + kk], in0=SC[:, 7 + kk:8 + kk], scalar1=BIG,
                                                           scalar2=None, op0=ALU.add), reads=R, writes=R)
                op("dve", lambda v, kk=kk, ti=ti: v.tensor_copy(out=idx_all[:, ti, kk:kk + 1], in_=SC[:, 7 + kk:8 + kk]),
                   reads=R, writes=[rt_tok])
            op("dve", lambda v, ti=ti: v.tensor_scalar(out=wt_all[:, ti, 0:1], in0=SC[:, 3:4], scalar1=SC[:, 6:7], scalar2=SC[:, 9:10],
                                                       op0=ALU.mult, op1=ALU.mult), reads=R, writes=[rt_tok])
            op("dve", lambda v, ti=ti: v.tensor_scalar(out=SC[:, 11:12], in0=SC[:, 3:4], scalar1=SC[:, 6:7], scalar2=SC[:, 5:6],
                                                       op0=ALU.mult, op1=ALU.mult), reads=R, writes=R)
            op("dve", lambda v, ti=ti: v.tensor_scalar(out=wt_all[:, ti, 1:2], in0=SC[:, 11:12], scalar1=SC[:, 10:11], scalar2=None,
                                                       op0=ALU.mult), reads=R, writes=[rt_tok])
            pending_scatter.append(ti)

    def emit_scatter_pair(tis):
        for ti in tis:
            dma("pool", xn[ti % 2][:, :], x1_d[ti * 128:(ti + 1) * 128, :], reads=[x1s_tok[ti]], writes=[xn_tok[ti % 2]])
        for ti in tis:
            xb = xn[ti % 2]
            op("act", lambda a, xb=xb, ti=ti: a.activation(out=xb[:, :], in_=xb[:, :], func=AF.Copy, scale=rs2_all[:, ti:ti + 1]),
               reads=[xn_tok[ti % 2], sm_tok], writes=[xn_tok[ti % 2]])
        for ti in tis:
            for kk in range(2):
                dma("pool", xs_d[:, :], xn[ti % 2][:, :], reads=[xn_tok[ti % 2], rt_tok], writes=[], dtok=xs_tok, indirect=True,
                    out_offset=bass.IndirectOffsetOnAxis(ap=idx_all[:, ti, kk:kk + 1], axis=0), in_offset=None,
                    bounds_check=bc_reg, oob_is_err=False)
        xs_tok.w = {xs_tok.dkey: xs_tok.dval}

    def emit_scatter_all():
        while pending_scatter:
            pair = [pending_scatter.pop(0)]
            if pending_scatter:
                pair.append(pending_scatter.pop(0))
            emit_scatter_pair(pair)


    stepA_halo()
    for j in range(JT):
        stepA_load(0, j)
    for j in range(JT):
        stepA_tile(0, j, j % 2)

    for b in range(NB):
        blk = plan[b]
        t0 = b * TB
        if b > 0:
            op("dve", lambda v: v.tensor_copy(out=aT[:, :, 0:HALO], in_=aT[:, :, TB:TB + HALO]), reads=[aT_tok], writes=[aT_tok])
        for cbi, cb in enumerate(WIN_ORDER):
            if cbi > 0:
                wdone(blk["win"][WIN_ORDER[cbi - 1]])
                if pending_chain:
                    emit_chain(*pending_chain.pop(0))
            if cbi == 2:
                emit_layernorm()
            if cbi == 4:
                emit_pool_dve(b)
            s, st = wget(blk["win"][cb])
            wv = wview(s, 16, 512)
            if cb < 4:
                for mm in range(4):
                    m = (cb % 2) * 4 + mm
                    pb, pt = bank()
                    for k in range(16):
                        op("pe", lambda pe, k=k, mm=mm, pb=pb, wv=wv: pe.matmul(pb[:, :], lhsT=wv[:, k, mm * 128:(mm + 1) * 128],
                                                                               rhs=hT[:, k, :], start=(k == 0), stop=(k == 15)),
                           reads=[st, hT_tok], writes=[pt])
                    if cb < 2:
                        op("dve", lambda v, m=m, pb=pb: v.tensor_copy(out=aT[:, m, HALO:HALO + TB], in_=pb[:, :]),
                           reads=[pt], writes=[aT_tok])
                        if b == 0:
                            pb2, pt2 = bank()
                            for k in range(16):
                                op("pe", lambda pe, k=k, mm=mm, pb2=pb2, wv=wv: pe.matmul(pb2[:, 0:HALO], lhsT=wv[:, k, mm * 128:(mm + 1) * 128],
                                                                                         rhs=hhalo[:, k, :], start=(k == 0), stop=(k == 15)),
                                   reads=[st, hh_tok], writes=[pt2])
                            op("dve", lambda v, m=m, pb2=pb2: v.tensor_copy(out=aT[:, m, 0:HALO], in_=pb2[:, 0:HALO]),
                               reads=[pt2], writes=[aT_tok])
                    else:
                        op("act", lambda a, m=m, pb=pb: a.activation(out=uT[:, m, :], in_=pb[:, :], func=AF.Gelu_apprx_tanh),
                           reads=[pt], writes=[uT_tok])
            else:
                half = cb - 4
                for j in range(JT):
                    pb, pt = bank()
                    for k in range(16):
                        op("pe", lambda pe, k=k, j=j, pb=pb, wv=wv: pe.matmul(pb[:, :], lhsT=hT[:, k, j * 128:(j + 1) * 128],
                                                                             rhs=wv[:, k, :], start=(k == 0), stop=(k == 15)),
                           reads=[st, hT_tok], writes=[pt])
                    op("act", lambda a, j=j, half=half, pb=pb: a.activation(out=scrv(j)[:, half * 512:(half + 1) * 512], in_=pb[:, :],
                                                                            func=AF.Gelu_apprx_tanh),
                       reads=[pt], writes=[scr_tok[j]])
        wdone(blk["win"][WIN_ORDER[-1]])
        while pending_chain:
            emit_chain(*pending_chain.pop(0))
        for gi in range(4):
            for mo in range(2):
                pb, pt = bank()
                for ki in range(2):
                    op("pe", lambda pe, gi=gi, mo=mo, ki=ki, pb=pb: pe.matmul(pb[:, :], lhsT=wpool[:, gi, ki, mo * 128:(mo + 1) * 128],
                                                                             rhs=pzT[:, 2 * gi + ki, :], start=(ki == 0), stop=(ki == 1)),
                       reads=[pz_tok] + CONST, writes=[pt])
                c = 2 * gi + mo
                op("act", lambda a, c=c, pb=pb: a.activation(out=ypT[:, c, :], in_=pb[:, :], func=AF.Copy,
                                                             scale=cvec[:, CV_PSC + c:CV_PSC + c + 1]),
                   reads=[pt] + CONST, writes=[yp_tok])
        for g8 in range(8):
            pb, pt = bank()
            for j in range(JT):
                op("pe", lambda pe, g8=g8, j=j, pb=pb: pe.matmul(pb[:, j * 128:(j + 1) * 128], lhsT=vT[:, j, g8 * 128:(g8 + 1) * 128],
                                                                 rhs=wsT[:, g8, :], start=True, stop=True),
                   reads=[vT_tok] + CONST, writes=[pt])
            tmp = scrv(1, 512)
            for j in range(JT):
                op("dve", lambda v, g8=g8, j=j, pb=pb, tmp=tmp: v.tensor_tensor(out=tmp[:, j * 128:(j + 1) * 128], in0=pb[:, j * 128:(j + 1) * 128],
                                                                               in1=bspB[:, g8, :], op=ALU.add),
                   reads=[pt] + CONST, writes=[scr_tok[1]])
            op("dve", lambda v, g8=g8, tmp=tmp: v.tensor_tensor(out=uT[:, g8, :], in0=uT[:, g8, :], in1=tmp, op=ALU.mult),
               reads=[scr_tok[1], uT_tok], writes=[uT_tok])

        emit_scatter_all()
        for cb in range(4):
            u_ab, u_g1, u_g2 = blk["mrg"][cb]
            for gi2, ug_ in enumerate((u_g1, u_g2)):
                s_g, t_g = wget(ug_)
                wgm = wview(s_g, 16, 512)
                for mm in range(4):
                    m = cb * 4 + mm
                    p1, p1t = bank()
                    for k in range(16):
                        op("pe", lambda pe, k=k, mm=mm, p1=p1, wgm=wgm: pe.matmul(p1[:, :], lhsT=wgm[:, k, mm * 128:(mm + 1) * 128], rhs=hT[:, k, :],
                                                                                 start=(k == 0), stop=(k == 15)),
                           reads=[t_g, hT_tok], writes=[p1t])
                    sgt = scrv(mm)[:, gi2 * 512:(gi2 + 1) * 512]
                    op("act", lambda a, m=m, p1=p1, sgt=sgt, gi2=gi2: a.activation(out=sgt, in_=p1[:, :], func=AF.Sigmoid,
                                                                                    bias=cvec[:, CV_BMG + 16 * gi2 + m:CV_BMG + 16 * gi2 + m + 1], scale=1.0),
                       reads=[p1t] + CONST, writes=[scr_tok[mm]])
                wdone(ug_)
            s_ab, t_ab = wget(u_ab)
            wa = pages[s_ab][:, 0:4096].rearrange("p (k n) -> p k n", k=8)
            wb = pages[s_ab][:, 4096:8192].rearrange("p (k n) -> p k n", k=8)
            for mm in range(4):
                m = cb * 4 + mm
                pa, pat = bank()
                for k in range(8):
                    op("pe", lambda pe, k=k, mm=mm, pa=pa, wa=wa: pe.matmul(pa[:, :], lhsT=wa[:, k, mm * 128:(mm + 1) * 128], rhs=ypT[:, k, :],
                                                                           start=(k == 0), stop=(k == 7)),
                       reads=[t_ab, yp_tok], writes=[pat])
                pbk, pbt = bank()
                for k in range(8):
                    op("pe", lambda pe, k=k, mm=mm, pbk=pbk, wb=wb: pe.matmul(pbk[:, :], lhsT=wb[:, k, mm * 128:(mm + 1) * 128], rhs=uT[:, k, :],
                                                                             start=(k == 0), stop=(k == 7)),
                       reads=[t_ab, uT_tok], writes=[pbt])
                s1 = scrv(mm)[:, 0:512]
                s2 = scrv(mm)[:, 512:1024]
                op("dve", lambda v, pa=pa, s1=s1: v.tensor_tensor(out=s1, in0=s1, in1=pa[:, :], op=ALU.mult),
                   reads=[pat, scr_tok[mm]], writes=[scr_tok[mm]])
                op("dve", lambda v, pbk=pbk, s2=s2: v.tensor_tensor(out=s2, in0=s2, in1=pbk[:, :], op=ALU.mult),
                   reads=[pbt, scr_tok[mm]], writes=[scr_tok[mm]])
                op("dve", lambda v, m=m, s1=s1, s2=s2: v.tensor_tensor(out=mT[:, m, :], in0=s1, in1=s2, op=ALU.add),
                   reads=[scr_tok[mm]], writes=[mT_tok])
            wdone(u_ab)

        for cb in range(4):
            s, st = wget(blk["wout"][cb])
            wv = wview(s, 16, 512)
            for j in range(JT):
                pb, pt = bank()
                for k in range(16):
                    op("pe", lambda pe, k=k, j=j, pb=pb, wv=wv: pe.matmul(pb[:, :], lhsT=mT[:, k, j * 128:(j + 1) * 128], rhs=wv[:, k, :],
                                                                         start=(k == 0), stop=(k == 15)),
                       reads=[st, mT_tok], writes=[pt])
                op("dve", lambda v, j=j, cb=cb, pb=pb: v.tensor_tensor(out=Xv[j][:, cb * 512:(cb + 1) * 512], in0=Xv[j][:, cb * 512:(cb + 1) * 512],
                                                                      in1=pb[:, :], op=ALU.add),
                   reads=[pt, X_tok[j]], writes=[X_tok[j]])
            wdone(blk["wout"][cb])
        for j in range(JT):
            ti = b * JT + j
            if stage == 1:
                dma("sp", y_d[ti * 128:(ti + 1) * 128, :], Xv[j], reads=[X_tok[j]], writes=[x1s_tok[ti]])
                if b + 1 < NB:
                    stepA_load(b + 1, j)
                    stepA_tile(b + 1, j, (j + 1) % 2)
                continue
            dma("sp", x1_d[ti * 128:(ti + 1) * 128, :], Xv[j], reads=[X_tok[j]], writes=[x1s_tok[ti]])
            junk = pages[8][:, 4096:4096 + D]
            rms_rstd("act", Xv[j], [X_tok[j]], junk, [yp_tok], 8, dst=rs2_all[:, ti:ti + 1])
            x1T = pages[8][:, 0:4096].bitcast(F32).rearrange("p (k t) -> p k t", k=16)
            for q in range(4):
                pb, pt = bank()
                for kk in range(4):
                    k = q * 4 + kk
                    op("pe", lambda pe, k=k, kk=kk, pb=pb, j=j: pe.transpose(out=pb[:, kk * 128:(kk + 1) * 128], in_=Xv[j][:, k * 128:(k + 1) * 128],
                                                                            identity=ident_f[:, :]),
                       reads=[X_tok[j]] + CONST, writes=[pt])
                x1q = x1T[:, q * 4:(q + 1) * 4, :]
                pbq = pb[:, :].rearrange("p (k t) -> p k t", k=4)
                if q % 2 == 0:
                    op("act", lambda a, pbq=pbq, x1q=x1q: a.activation(out=x1q, in_=pbq, func=AF.Copy), reads=[pt], writes=[pz_tok])
                else:
                    op("dve", lambda v, pbq=pbq, x1q=x1q: v.tensor_copy(out=x1q, in_=pbq), reads=[pt], writes=[pz_tok])
            pl, plt = bank()
            for k in range(16):
                op("pe", lambda pe, k=k, pl=pl: pe.matmul(pl[:, 0:36], lhsT=x1T[:, k, :], rhs=wr_sb[:, k, :], start=(k == 0), stop=(k == 15)),
                   reads=[pz_tok] + CONST, writes=[plt])
            op("dve", lambda v, pl=pl, j=j, ti=ti: v.scalar_tensor_tensor(out=LG4[:, j, :], in0=pl[:, 0:36], scalar=rs2_all[:, ti:ti + 1], in1=rbB[:, :],
                                                                        op0=ALU.mult, op1=ALU.add), reads=[plt, sm_tok] + CONST, writes=[lg_tok])
            if b + 1 < NB:
                stepA_load(b + 1, j)
                stepA_tile(b + 1, j, j % 2)
            pending_chain.append((b, j))
        if b + 1 == NB:
            while pending_chain:
                emit_chain(*pending_chain.pop(0))
            emit_scatter_all()


    if stage == 1:
        fin = {}
        for t in x1s_tok:
            Ctx._merge(fin, t.w)
        cx._wait("sp", fin)
        return nc

    barrier = cx.snapshot()
    moe_slots = [0, 1, 2, 5, 6, 7]
    for i in (5, 6, 7):
        slot_tok[i].w = dict(barrier)
    Ysb = Xv
    Y_tok = [cx.dma_tok(f"Ysb{j}", init=barrier) for j in range(4)]
    XeT = [pages[8][:, i * 4096:(i + 1) * 4096].rearrange("p (k r) -> p k r", k=16) for i in range(2)]
    XeT_tok = [cx.tok(f"XeT{i}", init=barrier) for i in range(2)]
    hidT = [aT[:, :, :].rearrange("p k t -> p (k t)")[:, i * 1024:(i + 1) * 1024].rearrange("p (m r) -> p m r", m=4) for i in range(2)]
    hid_tok = [cx.tok(f"hid{i}", init=barrier) for i in range(2)]
    ys_tok = cx.dma_tok("ys")
    slot_rr[0] = 0
    exp_units = []
    for e in range(NE):
        ug = sched_unit(lambda s, e=e: [(wview(s, 16, 512), wsrc(w_eg_d[e], 0, D, 0, 512))], moe_slots)
        uu = sched_unit(lambda s, e=e: [(wview(s, 16, 512), wsrc(w_eu_d[e], 0, D, 0, 512))], moe_slots)
        ud = sched_unit(lambda s, e=e: [(wview(s, 4, 2048), wsrc(w_ed_d[e], 0, 512, 0, D))], moe_slots)
        exp_units.append((ug, uu, ud))

    def wget_moe(u):
        ws.ensure(u)
        s = ws.units[u][0]
        return s, slot_tok[s]

    ws.ensure(exp_units[0][0] + 5)

    yrr = [0]

    def moe_rows_load(e):
        for r in range(2):
            dma("sp", xn[r][:, :], xs_d[e * CAP + r * 128:e * CAP + (r + 1) * 128, :], reads=[xs_tok], writes=[xn_tok[r]])

    def moe_transposes(e):
        xe = XeT[e % 2]
        xet = XeT_tok[e % 2]
        for r in range(2):
            transposes_bf(xn[r], [xn_tok[r]], lambda k, r=r, xe=xe: xe[:, k, r * 128:(r + 1) * 128], [xet], CV_GFFN, evac_rr)

    moe_rows_load(0)
    moe_transposes(0)
    for e in range(NE):
        ug, uu, ud = exp_units[e]
        xe = XeT[e % 2]
        xet = XeT_tok[e % 2]
        if e + 1 < NE:
            moe_rows_load(e + 1)
        sg_, tg_ = wget_moe(ug)
        su_, tu_ = wget_moe(uu)
        sd_, td_ = wget_moe(ud)
        wg = wview(sg_, 16, 512)
        wu = wview(su_, 16, 512)
        wd = wview(sd_, 4, 2048)
        hd = hidT[e % 2]
        hdt = hid_tok[e % 2]
        gu_banks = [bank() for _ in range(4)]
        for m in range(4):
            pb, pt = gu_banks[m]
            for k in range(16):
                op("pe", lambda pe, k=k, m=m, pb=pb: pe.matmul(pb[:, 0:CAP], lhsT=wg[:, k, m * 128:(m + 1) * 128], rhs=xe[:, k, :],
                                                               start=(k == 0), stop=(k == 15)), reads=[tg_, xet], writes=[pt])
        wdone(ug, 6)
        for m in range(4):
            pb, pt = gu_banks[m]
            for k in range(16):
                op("pe", lambda pe, k=k, m=m, pb=pb: pe.matmul(pb[:, CAP:2 * CAP], lhsT=wu[:, k, m * 128:(m + 1) * 128], rhs=xe[:, k, :],
                                                               start=(k == 0), stop=(k == 15)), reads=[tu_, xet], writes=[pt])
        wdone(uu, 6)
        for m in range(4):
            pb, pt = gu_banks[m]
            si = m % 2 + 2
            sg = scrv(si, CAP)
            op("act", lambda a, pb=pb, sg=sg: a.activation(out=sg, in_=pb[:, 0:CAP], func=AF.Silu), reads=[pt], writes=[scr_tok[si]])
            op("dve", lambda v, pb=pb, sg=sg, m=m: v.tensor_tensor(out=hd[:, m, :], in0=sg, in1=pb[:, CAP:2 * CAP], op=ALU.mult),
               reads=[pt, scr_tok[si]], writes=[hdt])
        if e + 1 < NE:
            moe_transposes(e + 1)
        for r in range(2):
            yi = yrr[0] % 4
            yrr[0] += 1
            for cb in range(4):
                pb, pt = bank()
                for m in range(4):
                    op("pe", lambda pe, m=m, r=r, cb=cb, pb=pb: pe.matmul(pb[:, :], lhsT=hd[:, m, r * 128:(r + 1) * 128],
                                                                         rhs=wd[:, m, cb * 512:(cb + 1) * 512], start=(m == 0), stop=(m == 3)),
                       reads=[td_, hdt], writes=[pt])
                if cb % 2 == 0:
                    op("act", lambda a, yi=yi, cb=cb, pb=pb: a.activation(out=Ysb[yi][:, cb * 512:(cb + 1) * 512], in_=pb[:, :], func=AF.Copy),
                       reads=[pt], writes=[Y_tok[yi]])
                else:
                    op("dve", lambda v, yi=yi, cb=cb, pb=pb: v.tensor_copy(out=Ysb[yi][:, cb * 512:(cb + 1) * 512], in_=pb[:, :]),
                       reads=[pt], writes=[Y_tok[yi]])
            dma("sp", ys_d[e * CAP + r * 128:e * CAP + (r + 1) * 128, :], Ysb[yi], reads=[Y_tok[yi]], writes=[], dtok=ys_tok)
        wdone(ud, 6)
    ys_tok.w = {ys_tok.dkey: ys_tok.dval}

    barrier2 = cx.snapshot()
    pg_pages = [0, 1, 2, 5]
    for ci, pgi in enumerate(pg_pages):
        dma("pool", wview(pgi, 16, 512), wsrc(w_pg_d, 0, D, ci * 512, 512), writes=[slot_tok[pgi]])
    wpg_tok = slot_tok[6]
    wpu = pages[6][:, 0:4096].rearrange("p (k n) -> p k n", k=2)
    dma("pool", wpu, w_pu_d.rearrange("(k p) n -> p k n", p=128), writes=[wpg_tok])
    gfin = pages[6][:, 4096:8192].bitcast(F32)
    bpg_b = scr[0:1, 3 * 1088:3 * 1088 + 1024].bitcast(BF16)
    scr_tok[3].w = dict(barrier2)
    bpg_tok = cx.dma_tok("bpg", init=barrier2)
    dma("pool", bpg_b, rows_d[0:1, R_BPG:R_BPG + D], writes=[bpg_tok])
    dma("sp", gfin, rows_d[0:1, R_GFIN:R_GFIN + D].partition_broadcast(128), writes=[wpg_tok])
    Gv = [pages[7 + (i // 2)][:, (i % 2) * 4096:(i % 2 + 1) * 4096].bitcast(F32) for i in range(4)]
    G_tok = [cx.dma_tok(f"G{i}", init=barrier2) for i in range(4)]
    for i in range(4):
        op("dve", lambda v, i=i: v.memset(Gv[i], 0.0), writes=[G_tok[i]])
    Xf_tok = [cx.dma_tok(f"Xf{j}", init=barrier2) for j in range(4)]
    h3T = [aT[:, :, :].rearrange("p k t -> p (k t)")[:, i * 2048:(i + 1) * 2048].rearrange("p (k t) -> p k t", k=16) for i in range(2)]
    h3_tok = [cx.tok(f"h3T{i}", init=barrier2) for i in range(2)]
    pbf = [sb(f"pbf{i}", [128, 256], BF16) for i in range(2)]
    pbf_tok = [cx.dma_tok(f"pbf{i}") for i in range(2)]
    pT = [sb(f"pT{i}", [128, 2, 128], BF16) for i in range(2)]
    pT_tok = [cx.tok(f"pT{i}") for i in range(2)]
    out_tok = cx.dma_tok("out")
    for st_ in scr_tok:
        pass
    def ple_A(ti):
        j = ti % 4
        Xt, Xk = Xv[j], Xf_tok[j]
        dma("sp", Xt, x1_d[ti * 128:(ti + 1) * 128, :], reads=[x1s_tok[ti]], writes=[Xk])
        for kk in range(2):
            gi = (ti % 2) * 2 + kk
            dma("pool", Gv[gi], ys_d[:, :], reads=[ys_tok, rt_tok], writes=[G_tok[gi]], indirect=True, out_offset=None,
                in_offset=bass.IndirectOffsetOnAxis(ap=idx_all[:, ti, kk:kk + 1], axis=0),
                bounds_check=bc_reg, oob_is_err=False)
        dma("pool", pbf[ti % 2][:, :], p_d[ti * 128:(ti + 1) * 128, :], writes=[pbf_tok[ti % 2]])
        for kk in range(2):
            gi = (ti % 2) * 2 + kk
            op("dve", lambda v, gi=gi, ti=ti, kk=kk, Xt=Xt: v.scalar_tensor_tensor(out=Xt, in0=Gv[gi], scalar=wt_all[:, ti, kk:kk + 1], in1=Xt,
                                                                                  op0=ALU.mult, op1=ALU.add),
               reads=[G_tok[gi], rt_tok, Xk], writes=[Xk])
        xb = xn[ti % 2]
        xt = xn_tok[ti % 2]
        rms_rstd("act", Xt, [Xk], xb[:, :], [xt], 9)
        op("act", lambda a, xb=xb, Xt=Xt: a.activation(out=xb[:, :], in_=Xt, func=AF.Copy, scale=sm[:, 9:10]),
           reads=[Xk, sm_tok], writes=[xt])

    def ple_C(ti):
        xb = xn[ti % 2]
        xt = xn_tok[ti % 2]
        h3 = h3T[ti % 2]
        h3t = h3_tok[ti % 2]
        transposes_bf(xb, [xt], lambda k, h3=h3: h3[:, k, :], [h3t], CV_GPLE, evac_rr)
        pb, pt = bank()
        pbb = pb[:, :].bitcast(BF16)
        for kk in range(2):
            op("pe", lambda pe, kk=kk, pbb=pbb, ti=ti: pe.transpose(out=pbb[:, kk * 128:(kk + 1) * 128], in_=pbf[ti % 2][:, kk * 128:(kk + 1) * 128],
                                                                   identity=ident_b[:, :]), reads=[pbf_tok[ti % 2]] + CONST, writes=[pt])
        op("dve", lambda v, pbb=pbb, ti=ti: v.tensor_copy(out=pT[ti % 2][:, :, :], in_=pbb[:, 0:256].rearrange("p (k t) -> p k t", k=2)),
           reads=[pt], writes=[pT_tok[ti % 2]])

    def ple_B(ti, cbs):
        j = ti % 4
        Xt, Xk = Xv[j], Xf_tok[j]
        xb = xn[ti % 2]
        xt = xn_tok[ti % 2]
        h3 = h3T[ti % 2]
        h3t = h3_tok[ti % 2]
        for cb in cbs:
            wg_ = wview(pg_pages[cb], 16, 512)
            pg_, pgt = bank()
            for k in range(16):
                op("pe", lambda pe, k=k, pg_=pg_, wg_=wg_, h3=h3: pe.matmul(pg_[:, :], lhsT=h3[:, k, :], rhs=wg_[:, k, :], start=(k == 0), stop=False),
                   reads=[slot_tok[pg_pages[cb]], h3t], writes=[pgt])
            op("pe", lambda pe, pg_=pg_, cb=cb: pe.matmul(pg_[:, :], lhsT=ones_b[0:1, :], rhs=bpg_b[0:1, cb * 512:(cb + 1) * 512], start=False, stop=True),
               reads=CONST + [bpg_tok], writes=[pgt])
            pu_, put = bank()
            for k in range(2):
                op("pe", lambda pe, k=k, pu_=pu_, cb=cb, ti=ti: pe.matmul(pu_[:, :], lhsT=pT[ti % 2][:, k, :], rhs=wpu[:, k, cb * 512:(cb + 1) * 512],
                                                                         start=(k == 0), stop=(k == 1)), reads=[wpg_tok, pT_tok[ti % 2]], writes=[put])
            si = cb % 2
            sg = scrv(si, 512)
            op("act", lambda a, pg_=pg_, sg=sg: a.activation(out=sg, in_=pg_[:, :], func=AF.Sigmoid), reads=[pgt], writes=[scr_tok[si]])
            op("dve", lambda v, pu_=pu_, sg=sg: v.tensor_tensor(out=sg, in0=sg, in1=pu_[:, :], op=ALU.mult), reads=[put, scr_tok[si]], writes=[scr_tok[si]])
            op("dve", lambda v, sg=sg, cb=cb, Xt=Xt: v.tensor_tensor(out=Xt[:, cb * 512:(cb + 1) * 512], in0=Xt[:, cb * 512:(cb + 1) * 512], in1=sg, op=ALU.add),
               reads=[scr_tok[si], Xk], writes=[Xk])

    def ple_B2(ti):
        j = ti % 4
        Xt, Xk = Xv[j], Xf_tok[j]
        xb = xn[ti % 2]
        xt = xn_tok[ti % 2]
        rms_rstd("act", Xt, [Xk], xb[:, :], [xt], 10)
        op("dve", lambda v, Xt=Xt: v.scalar_tensor_tensor(out=Xt, in0=Xt, scalar=sm[:, 10:11], in1=gfin, op0=ALU.mult, op1=ALU.mult),
           reads=[Xk, sm_tok, wpg_tok], writes=[Xk])
        dma("sp", y_d[ti * 128:(ti + 1) * 128, :], Xt, reads=[Xk], writes=[], dtok=out_tok)

    ple_A(0)
    ple_C(0)
    for ti in range(NT):
        if ti + 1 < NT:
            ple_A(ti + 1)
        ple_B(ti, (0, 1))
        if ti + 1 < NT:
            ple_C(ti + 1)
        ple_B(ti, (2, 3))
        ple_B2(ti)
    if DEBUG_X1:
        dbg_w = nc.dram_tensor("dbg_w", [128, NT * 2], F32, kind="ExternalOutput").ap()
        dbg_i = nc.dram_tensor("dbg_i", [128, NT * 2], I32, kind="ExternalOutput").ap()
        dma("sp", dbg_w[:, :], wt_all[:, :, :].rearrange("p a b -> p (a b)"), reads=[rt_tok], writes=[], dtok=out_tok)
        dma("sp", dbg_i[:, :], idx_all[:, :, :].rearrange("p a b -> p (a b)"), reads=[rt_tok], writes=[], dtok=out_tok)
    cx._wait("sp", {out_tok.dkey: out_tok.dval})
    return nc


_NC_CACHE = {}


def _prep_inputs(inp):
    f = lambda a: np.ascontiguousarray(np.asarray(a, dtype=np.float32))
    x = f(inp["x"]).reshape(4, 4096, D)
    p = f(inp["p"]).reshape(4, 4096, 256)

    def pk(v):
        v = f(v).reshape(-1)
        return v.reshape(-1, 128).T

    cvec = np.concatenate([pk(inp["g_mix"]), pk(inp["g_ffn"]), pk(inp["g_ple"]), pk(inp["pool_scale"]),
                           pk(inp["b_merge_gate"])], axis=1)
    assert cvec.shape == (128, CV_N)
    wrg = f(inp["w_router_group"]).reshape(D, 4)
    wre = f(inp["w_router_expert"]).reshape(4, D, 8).transpose(1, 0, 2).reshape(D, 32)
    wr = np.concatenate([wrg, wre], axis=1)
    wr = wr.reshape(16, 128, 36).transpose(1, 0, 2).reshape(128, 16 * 36)
    rb = np.concatenate([f(inp["b_router_group"]).reshape(-1), f(inp["b_router_expert"]).reshape(-1)])
    shared = {
        "cvec": np.ascontiguousarray(cvec),
        "wr": np.ascontiguousarray(wr),
        "w_in": f(inp["w_in"]).reshape(D, 3072),
        "w_pool": f(inp["w_pool"]).reshape(4, 256, 256),
        "w_ba": f(inp["w_branch_a"]).reshape(1024, D),
        "w_bb": f(inp["w_branch_b"]).reshape(1024, D),
        "w_sp": f(inp["w_spatial"]).reshape(8, 128, 128),
        "w_mg": f(inp["w_merge_gate"]).reshape(D, 2 * D),
        "w_out": f(inp["w_out"]).reshape(D, D),
        "w_eg": f(inp["w_exp_gate"]).reshape(NE, D, 512),
        "w_eu": f(inp["w_exp_up"]).reshape(NE, D, 512),
        "w_ed": f(inp["w_exp_down"]).reshape(NE, 512, D),
        "w_pg": f(inp["w_ple_gate"]).reshape(D, D),
        "w_pu": f(inp["w_ple_up"]).reshape(256, D),
    }
    maps = []
    for c in range(NCORES):
        bi, half = c // 2, c % 2
        t0 = half * T
        rows = np.zeros((1, R_N), np.float32)
        rows[0, R_LNG:R_LNG + 1024] = f(inp["sgu_ln_g"]).reshape(-1)
        rows[0, R_LNB:R_LNB + 1024] = f(inp["sgu_ln_b"]).reshape(-1)
        rows[0, R_BSP:R_BSP + 1024] = f(inp["b_spatial"]).reshape(-1)
        rows[0, R_RB:R_RB + 36] = rb
        rows[0, R_GFIN:R_GFIN + D] = f(inp["g_final"]).reshape(-1)
        rows[0, R_BPG:R_BPG + D] = f(inp["b_ple_gate"]).reshape(-1)
        rows[0, R_POS:R_POS + HALO] = t0 + 1 + np.arange(HALO)
        xh = np.zeros((HALO, D), np.float32)
        if half == 1:
            xh[:] = x[bi, t0 - HALO:t0]
        m = dict(shared)
        m["x"] = np.ascontiguousarray(x[bi, t0:t0 + T])
        m["xh"] = xh
        m["p"] = np.ascontiguousarray(p[bi, t0:t0 + T])
        m["rows"] = rows
        maps.append(m)
    return maps


def kernel(**inputs):
    stage = 2
    if stage not in _NC_CACHE:
        _NC_CACHE[stage] = build(stage)
    nc = _NC_CACHE[stage]
    maps = _prep_inputs(inputs)
    res = run_bass_kernel_spmd(nc, maps, core_ids=list(range(NCORES)))
    out = np.empty((4, 4096, D), np.float32)
    for c in range(NCORES):
        out[c // 2, (c % 2) * T:(c % 2 + 1) * T] = res.results[c]["y"]
    return out
```

```python
import numpy as np
import concourse.bass as bass
import concourse.mybir as mybir
from concourse.bass_utils import run_bass_kernel_spmd

F32 = mybir.dt.float32
BF16 = mybir.dt.bfloat16
I32 = mybir.dt.int32
AF = mybir.ActivationFunctionType
ALU = mybir.AluOpType
AX = mybir.AxisListType

NCORES = 8
T = 2048
D = 2048
NT = T // 128
TB = 512
NB = T // TB
JT = TB // 128
HALO = 16
CAP = 256
NE = 32
EPS = 1e-6
BIG = float(1 << 20)
EPOCH = 6000
SAME_ENGINE_SYNC = True
DEBUG_X1 = False

CV_GMIX, CV_GFFN, CV_GPLE, CV_PSC, CV_BMG, CV_N = 0, 16, 32, 48, 56, 88
R_LNG, R_LNB, R_BSP, R_RB, R_GFIN, R_BPG, R_POS, R_N = 0, 1024, 2048, 3072, 3108, 5156, 7204, 7220


class Tok:
    __slots__ = ("w", "r", "name", "dkey", "dval")

    def __init__(self, name, init=None):
        self.name = name
        self.w = dict(init) if init else {}
        self.r = dict(init) if init else {}


class Ctx:
    def __init__(self):
        self.nc = bass.Bass("TRN2", target_bir_lowering=False)
        nc = self.nc
        self.eng = dict(pe=nc.tensor, act=nc.scalar, dve=nc.vector, pool=nc.gpsimd, sp=nc.sync)
        self.semh = {}
        self.cur = {}
        self.cnt = {}
        self.known = {e: {} for e in self.eng}
        self.nsem = 0
        for e in self.eng:
            self._new_epoch(e, 0)
        self.dma_sems = []

    def _sem(self, name):
        self.nsem += 1
        return self.nc.semaphore(name).__enter__()

    def _new_epoch(self, e, ep):
        key = (e, ep)
        self.semh[key] = self._sem(f"s_{e}_{ep}")
        self.cur[e] = key
        self.cnt[e] = 0

    def tok(self, name, init=None):
        return Tok(name, init)

    def dma_tok(self, name, init=None):
        t = Tok(name, init)
        key = ("dma", name)
        self.semh[key] = self._sem(f"d_{name}")
        t.dkey = key
        t.dval = 0
        return t

    def snapshot(self):
        d = {}
        for e in self.eng:
            if self.cnt[e] > 0:
                d[self.cur[e]] = self.cnt[e]
            elif self.cur[e][1] > 0:
                d[(e, self.cur[e][1] - 1)] = EPOCH
        for t in self.dma_sems:
            if t.dval > 0:
                d[t.dkey] = t.dval
        return d

    def _wait(self, e, deps):
        eng = self.eng[e]
        kn = self.known[e]
        for key, val in deps.items():
            if kn.get(key, 0) >= val:
                continue
            if key[0] == e and not SAME_ENGINE_SYNC:
                continue
            if key[0] == e and e in ("pe",):
                continue
            eng.wait_ge(self.semh[key], val)
            kn[key] = val

    @staticmethod
    def _merge(d, src):
        for k, v in src.items():
            if d.get(k, 0) < v:
                d[k] = v

    def op(self, e, fn, reads=(), writes=(), wd=()):
        deps = {}
        for t in reads:
            self._merge(deps, t.w)
        for t in writes:
            self._merge(deps, t.w)
            self._merge(deps, t.r)
        for t in wd:
            self._merge(deps, t.r)
        self._wait(e, deps)
        ins = fn(self.eng[e])
        if self.cnt[e] >= EPOCH:
            self._new_epoch(e, self.cur[e][1] + 1)
        self.cnt[e] += 1
        key, val = self.cur[e], self.cnt[e]
        ins.then_inc(self.semh[key], 1)
        for t in reads:
            t.r[key] = val
        for t in writes:
            t.w = {key: val}
            t.r = {}
        for t in wd:
            t.w[key] = val
        return ins

    def dma(self, e, out, in_, reads=(), writes=(), dtok=None, **kw):
        deps = {}
        for t in reads:
            self._merge(deps, t.w)
        for t in writes:
            self._merge(deps, t.w)
            self._merge(deps, t.r)
        self._wait(e, deps)
        if dtok is None:
            dtok = writes[0]
        if dtok not in self.dma_sems:
            self.dma_sems.append(dtok)
        eng = self.eng[e]
        if "indirect" in kw:
            kw = dict(kw)
            kw.pop("indirect")
            ins = eng.indirect_dma_start(out=out, in_=in_, **kw)
        else:
            ins = eng.dma_start(out=out, in_=in_, **kw)
        dtok.dval += 16
        ins.then_inc(self.semh[dtok.dkey], 16)
        key, val = dtok.dkey, dtok.dval
        for t in reads:
            t.r[key] = val
        for t in writes:
            if t is dtok:
                t.w = {key: val}
                t.r = {}
            else:
                t.w = {key: val}
                t.r = {}
        return ins


def build(stage=2):
    cx = Ctx()
    nc = cx.nc
    op, dma = cx.op, cx.dma

    def din(name, shape, dt=F32):
        return nc.dram_tensor(name, list(shape), dt, kind="ExternalInput").ap()

    x_d = din("x", [T, D])
    xh_d = din("xh", [HALO, D])
    p_d = din("p", [T, 256])
    cvec_d = din("cvec", [128, CV_N])
    wr_d = din("wr", [128, 16 * 36])
    rows_d = din("rows", [1, R_N])
    w_in_d = din("w_in", [D, 3072])
    w_pool_d = din("w_pool", [4, 256, 256])
    w_ba_d = din("w_ba", [1024, D])
    w_bb_d = din("w_bb", [1024, D])
    w_sp_d = din("w_sp", [8, 128, 128])
    w_mg_d = din("w_mg", [D, 2 * D])
    w_out_d = din("w_out", [D, D])
    w_eg_d = din("w_eg", [NE, D, 512])
    w_eu_d = din("w_eu", [NE, D, 512])
    w_ed_d = din("w_ed", [NE, 512, D])
    w_pg_d = din("w_pg", [D, D])
    w_pu_d = din("w_pu", [256, D])
    y_d = nc.dram_tensor("y", [T, D], F32, kind="ExternalOutput").ap()
    xs_d = nc.dram_tensor("xs_scr", [NE * CAP, D], BF16).ap()
    ys_d = nc.dram_tensor("ys_scr", [NE * CAP, D], F32).ap()
    x1_d = nc.dram_tensor("x1_scr", [T, D], F32, kind=("ExternalOutput" if DEBUG_X1 else "Internal")).ap()

    def sb(name, shape, dt):
        return nc.alloc_sbuf_tensor("sb_" + name, list(shape), dt)

    bc_reg = nc.gpsimd.to_reg(NE * CAP - 1)
    NPAGE = 9
    pages = [sb(f"pg{i}", [128, 8192], BF16) for i in range(NPAGE)]
    aT = sb("aT", [128, 8, HALO + TB], BF16)
    xn = [sb(f"xn{i}", [128, D], BF16) for i in range(2)]
    scr = sb("scr", [128, 4 * 1088], F32)
    ident_b = sb("ident_b", [128, 128], BF16)
    ident_f = sb("ident_f", [128, 128], F32)
    ltri_f = sb("ltri_f", [128, 128], F32)
    ones_f = sb("ones_f", [128, 128], F32)
    ones_b = sb("ones_b", [1, 128], BF16)
    cvec = sb("cvec", [128, CV_N], F32)
    wr_sb = sb("wr_sb", [128, 16, 36], F32)
    wsT = sb("wsT", [128, 8, 128], BF16)
    bspB = sb("bspB", [128, 8, 128], F32)
    wpool = sb("wpool", [128, 4, 2, 256], BF16)
    lnG = sb("lnG", [128, 1024], F32)
    lnB = sb("lnB", [128, 1024], F32)
    rbB = sb("rbB", [128, 36], F32)
    corr = sb("corr", [128, 4, HALO], F32)
    offs = sb("offs", [128, NE], F32)
    offs_i = sb("offs_i", [128, NE], I32)
    msum = sb("msum", [128, NE], F32)
    wt_all = sb("wt_all", [128, NT, 2], F32)
    idx_all = sb("idx_all", [128, NT, 2], I32)
    sm = sb("sm", [128, 336], F32)
    posb = sb("posb", [128, HALO], F32)
    epsb = sb("epsb", [128, 1], F32)

    psb = [nc.alloc_psum_tensor(f"ps{i}", [128, 512], F32) for i in range(8)]
    ps_tok = [cx.tok(f"ps{i}") for i in range(8)]
    ps_i = [0]

    def bank():
        i = ps_i[0] % 8
        ps_i[0] += 1
        return psb[i], ps_tok[i]

    t_const = cx.dma_tok("const")
    t_cst2 = cx.tok("cst2")

    dma("sp", cvec[:, :], cvec_d[:, :], writes=[t_const])
    dma("sp", wr_sb[:, :, :], wr_d[:, :].rearrange("p (k n) -> p k n", k=16), writes=[t_const])
    dma("sp", lnG[:, :], rows_d[0:1, R_LNG:R_LNG + 1024].partition_broadcast(128), writes=[t_const])
    dma("sp", lnB[:, :], rows_d[0:1, R_LNB:R_LNB + 1024].partition_broadcast(128), writes=[t_const])
    dma("sp", bspB[:, :, :], rows_d[0:1, R_BSP:R_BSP + 1024].partition_broadcast(128), writes=[t_const])
    dma("sp", rbB[:, :], rows_d[0:1, R_RB:R_RB + 36].partition_broadcast(128), writes=[t_const])
    dma("sp", posb[:, :], rows_d[0:1, R_POS:R_POS + HALO].partition_broadcast(128), writes=[t_const])
    dma("pool", wpool[:, :, :, :], w_pool_d.rearrange("g (k p) n -> p g k n", p=128), writes=[t_const])

    op("pool", lambda g: g.memset(ones_f[:, :], 1.0), writes=[t_cst2])
    op("pool", lambda g: g.memset(ident_f[:, :], 1.0), writes=[t_cst2])
    op("pool", lambda g: g.affine_select(out=ident_f[:, :], in_=ident_f[:, :], pattern=[[1, 128]],
                                         compare_op=ALU.is_equal, fill=0.0, base=0, channel_multiplier=-1),
       writes=[t_cst2])
    op("pool", lambda g: g.memset(ltri_f[:, :], 1.0), writes=[t_cst2])
    op("pool", lambda g: g.affine_select(out=ltri_f[:, :], in_=ltri_f[:, :], pattern=[[1, 128]],
                                         compare_op=ALU.is_ge, fill=0.0, base=0, channel_multiplier=-1),
       writes=[t_cst2])
    op("pool", lambda g: g.iota(offs_i[:, :], pattern=[[CAP, NE]], base=-1, channel_multiplier=0), writes=[t_cst2])
    op("dve", lambda v: v.tensor_copy(out=ident_b[:, :], in_=ident_f[:, :]), reads=[t_cst2], writes=[t_cst2])
    op("dve", lambda v: v.tensor_copy(out=offs[:, :], in_=offs_i[:, :]), reads=[t_cst2], writes=[t_cst2])
    op("dve", lambda v: v.memset(ones_b[:, :], 1.0), writes=[t_cst2])
    op("dve", lambda v: v.memset(msum[:, :], 0.0), writes=[t_cst2])
    op("dve", lambda v: v.memset(epsb[:, :], EPS), writes=[t_cst2])
    for gi, w in enumerate((2, 4, 8, 16)):
        op("dve", lambda v, gi=gi, w=w: v.tensor_scalar(out=corr[:, gi, :], in0=posb[:, :], scalar1=float(w),
                                                        scalar2=None, op0=ALU.min),
           reads=[t_const], writes=[t_cst2])
        op("dve", lambda v, gi=gi: v.reciprocal(out=corr[:, gi, :], in_=corr[:, gi, :]), reads=[t_cst2], writes=[t_cst2])
        op("dve", lambda v, gi=gi, w=w: v.tensor_scalar(out=corr[:, gi, :], in0=corr[:, gi, :], scalar1=float(w),
                                                        scalar2=None, op0=ALU.mult),
           reads=[t_cst2], writes=[t_cst2])
    t_wsl = cx.dma_tok("wsl")
    for g8 in range(8):
        stg = scr[:, 0:128]
        dma("sp", stg, w_sp_d[g8], writes=[t_wsl])
        pb, pt = bank()
        op("pe", lambda pe, pb=pb: pe.transpose(out=pb[:, 0:128], in_=scr[:, 0:128], identity=ident_f[:, :]),
           reads=[t_wsl, t_cst2], writes=[pt])
        op("dve", lambda v, pb=pb, g8=g8: v.tensor_copy(out=wsT[:, g8, :], in_=pb[:, 0:128]), reads=[pt], writes=[t_cst2])
        t_wsl.r.update(pt.w)
    op("dve", lambda v: v.memset(wsT[64:128, :, 0:64], 0.0), reads=[t_cst2], writes=[t_cst2])
    for k in range(16):
        op("dve", lambda v, k=k: v.tensor_scalar(out=wr_sb[:, k, :], in0=wr_sb[:, k, :], scalar1=cvec[:, CV_GFFN + k:CV_GFFN + k + 1],
                                                 scalar2=None, op0=ALU.mult), reads=[t_const, t_cst2], writes=[t_cst2])
    CONST = [t_const, t_cst2]

    NSLOT_MIX = 3
    slot_pages = [0, 1, 2]
    slot_tok = {i: cx.dma_tok(f"wslot{i}") for i in range(NPAGE)}

    class WStream:
        def __init__(self):
            self.units = []
            self.issued = 0
            self.slots = list(slot_pages)

        def add(self, parts):
            self.units.append(parts)
            return len(self.units) - 1

        def slot_of(self, i):
            return self.units[i][0]

        def ensure(self, upto):
            while self.issued <= min(upto, len(self.units) - 1):
                pg_i, parts = self.units[self.issued]
                for dst, src in parts:
                    dma("pool", dst, src, writes=[slot_tok[pg_i]])
                self.issued += 1

    ws = WStream()

    def wview(pg_i, kc, n):
        return pages[pg_i][:, 0:kc * n].rearrange("p (k n) -> p k n", k=kc)

    def wsrc(w_ap, r0, nrows, c0, ncols):
        return w_ap[r0:r0 + nrows, c0:c0 + ncols].rearrange("(k p) n -> p k n", p=128)

    unit_seq = []
    slot_rr = [0]

    def next_slot(slots):
        s = slots[slot_rr[0] % len(slots)]
        slot_rr[0] += 1
        return s

    mix_slots = [0, 1, 2]

    def sched_unit(parts_fn, slots):
        s = next_slot(slots)
        return ws.add((s, parts_fn(s)))

    Xv = [pages[3 + (j // 2)][:, (j % 2) * 4096:(j % 2 + 1) * 4096].bitcast(F32) for j in range(4)]
    X_tok = [cx.dma_tok(f"X{j}") for j in range(4)]
    hT = pages[5][:, :].rearrange("p (k t) -> p k t", k=16)
    hT_tok = cx.tok("hT")
    mT = pages[6][:, :].rearrange("p (k t) -> p k t", k=16)
    mT_tok = cx.tok("mT")
    uT = pages[7][:, 0:4096].rearrange("p (k t) -> p k t", k=8)
    uT_tok = cx.tok("uT")
    vT = pages[7][:, 4096:8192].rearrange("p (j c) -> p j c", j=JT)
    vT_tok = cx.tok("vT")
    pzT = pages[8][:, 0:4096].rearrange("p (k t) -> p k t", k=8)
    pz_tok = cx.tok("pzT")
    ypT = pages[8][:, 4096:8192].rearrange("p (k t) -> p k t", k=8)
    yp_tok = cx.tok("ypT")
    aT_tok = cx.tok("aT")
    xn_tok = [cx.dma_tok(f"xn{i}") for i in range(2)]
    scr_tok = [cx.tok(f"scr{i}") for i in range(4)]
    sm_tok = cx.tok("sm")
    rt_tok = cx.tok("route")
    xs_tok = cx.dma_tok("xs")
    x1s_tok = [cx.dma_tok(f"x1s{i}") for i in range(NT)]
    hh_tok = cx.tok("hhalo")
    hhalo = sb("hhalo", [128, 16, HALO], BF16)

    def scrv(i, n=1088):
        return scr[:, i * 1088:i * 1088 + n]

    WIN_ORDER = (4, 5, 0, 1, 2, 3)
    plan = []
    for b in range(NB):
        blk = {}
        blk["win"] = {cb: sched_unit(lambda s, cb=cb: [(wview(s, 16, 512), wsrc(w_in_d, 0, D, cb * 512, 512))], mix_slots)
                      for cb in WIN_ORDER}
        blk["mrg"] = []
        for cb in range(4):
            u_g1 = sched_unit(lambda s, cb=cb: [(wview(s, 16, 512), wsrc(w_mg_d, 0, D, cb * 512, 512))], mix_slots)
            u_g2 = sched_unit(lambda s, cb=cb: [(wview(s, 16, 512), wsrc(w_mg_d, 0, D, D + cb * 512, 512))], mix_slots)
            u_ab = sched_unit(lambda s, cb=cb: [
                (pages[s][:, 0:4096].rearrange("p (k n) -> p k n", k=8), wsrc(w_ba_d, 0, 1024, cb * 512, 512)),
                (pages[s][:, 4096:8192].rearrange("p (k n) -> p k n", k=8), wsrc(w_bb_d, 0, 1024, cb * 512, 512))],
                mix_slots)
            blk["mrg"].append((u_ab, u_g1, u_g2))
        blk["wout"] = [sched_unit(lambda s, cb=cb: [(wview(s, 16, 512), wsrc(w_out_d, 0, D, cb * 512, 512))], mix_slots)
                       for cb in range(4)]
        plan.append(blk)
    n_mix_units = len(ws.units)

    def wget(u):
        ws.ensure(u)
        s = ws.units[u][0]
        return s, slot_tok[s]

    def wdone(u, nslots=3):
        ws.ensure(u + nslots)

    def rms_rstd(e_src, src_ap, src_toks, junk_ap, junk_toks, col, npart=128):
        sc = sm[0:npart, col:col + 1]
        op("act", lambda a: a.activation(out=junk_ap, in_=src_ap, func=AF.Square, accum_out=sc),
           reads=src_toks, writes=junk_toks + [sm_tok])
        op("act", lambda a: a.activation(out=sc, in_=sc, func=AF.Sqrt, scale=1.0 / D, bias=epsb[0:npart, :]),
           reads=[sm_tok] + CONST, writes=[sm_tok])
        op("dve", lambda v: v.reciprocal(out=sc, in_=sc), reads=[sm_tok], writes=[sm_tok])

    def transposes_bf(src_rows_ap, src_toks, dst_fn, dst_toks, gcol, evac_rr):
        for q in range(4):
            pb, pt = bank()
            pbb = pb[:, :].bitcast(BF16)
            for kk in range(4):
                k = q * 4 + kk
                op("pe", lambda pe, k=k, kk=kk, pbb=pbb: pe.transpose(out=pbb[:, kk * 128:(kk + 1) * 128],
                                                                       in_=src_rows_ap[:, k * 128:(k + 1) * 128],
                                                                       identity=ident_b[:, :]),
                   reads=src_toks + CONST, writes=[pt])
            evac_rr[0] += 1
            use_act = (evac_rr[0] % 2) == 0
            for kk in range(4):
                k = q * 4 + kk
                if use_act:
                    op("act", lambda a, k=k, kk=kk, pbb=pbb: a.activation(out=dst_fn(k), in_=pbb[:, kk * 128:(kk + 1) * 128],
                                                                          func=AF.Copy, scale=cvec[:, gcol + k:gcol + k + 1]),
                       reads=[pt] + CONST, wd=dst_toks)
                else:
                    op("dve", lambda v, k=k, kk=kk, pbb=pbb: v.tensor_scalar(out=dst_fn(k), in0=pbb[:, kk * 128:(kk + 1) * 128],
                                                                             scalar1=cvec[:, gcol + k:gcol + k + 1], scalar2=None,
                                                                             op0=ALU.mult),
                       reads=[pt] + CONST, wd=dst_toks)

    evac_rr = [0]

    def emit_layernorm():
            for j in range(JT):
                vg = scrv(j, 1024)
                st6 = sm[:, 16:28]
                op("dve", lambda v, vg=vg: v.bn_stats(out=sm[:, 16:22], in_=vg[:, 0:512]), reads=[scr_tok[j]], writes=[sm_tok])
                op("dve", lambda v, vg=vg: v.bn_stats(out=sm[:, 22:28], in_=vg[:, 512:1024]), reads=[scr_tok[j], sm_tok], writes=[sm_tok])
                op("dve", lambda v: v.bn_aggr(out=sm[:, 28:30], in_=sm[:, 16:28]), reads=[sm_tok], writes=[sm_tok])
                op("act", lambda a: a.activation(out=sm[:, 29:30], in_=sm[:, 29:30], func=AF.Sqrt, scale=1.0, bias=epsb[:, :]),
                   reads=[sm_tok] + CONST, writes=[sm_tok])
                op("dve", lambda v: v.reciprocal(out=sm[:, 29:30], in_=sm[:, 29:30]), reads=[sm_tok], writes=[sm_tok])
                op("dve", lambda v, vg=vg: v.scalar_tensor_tensor(out=vg, in0=vg, scalar=sm[:, 28:29], in1=lnG[:, :],
                                                                  op0=ALU.subtract, op1=ALU.mult),
                   reads=[scr_tok[j], sm_tok] + CONST, writes=[scr_tok[j]])
                op("dve", lambda v, vg=vg, j=j: v.scalar_tensor_tensor(out=vT[:, j, :], in0=vg, scalar=sm[:, 29:30], in1=lnB[:, :],
                                                                       op0=ALU.mult, op1=ALU.add),
                   reads=[scr_tok[j], sm_tok] + CONST, writes=[vT_tok])


    pg6f = pages[6][:, :].bitcast(F32)

    def emit_pool_dve(b):
        W = HALO + TB
        for gi, w in enumerate((2, 4, 8, 16)):
            ca = aT[:, 2 * gi:2 * gi + 2, :]
            bufA = pg6f[:, 0:2 * W].rearrange("p (k t) -> p k t", k=2)
            bufB = pg6f[:, 2 * W:4 * W].rearrange("p (k t) -> p k t", k=2)
            cur, cur_t = ca, aT_tok
            sh = 1
            lvl = 0
            while sh < w:
                dst = bufA if lvl % 2 == 0 else bufB
                lo = 2 * sh - 1
                op("dve", lambda v, dst=dst, cur=cur, lo=lo, sh=sh: v.tensor_tensor(out=dst[:, :, lo:W], in0=cur[:, :, lo:W],
                                                                                     in1=cur[:, :, lo - sh:W - sh], op=ALU.add),
                   reads=[cur_t], writes=[mT_tok])
                cur, cur_t = dst, mT_tok
                sh *= 2
                lvl += 1
            if b == 0:
                for c2 in range(2):
                    op("dve", lambda v, cur=cur, gi=gi, c2=c2: v.tensor_tensor(out=cur[:, c2, HALO:2 * HALO], in0=cur[:, c2, HALO:2 * HALO],
                                                                              in1=corr[:, gi, :], op=ALU.mult),
                       reads=[cur_t] + CONST, writes=[cur_t])
            op("dve", lambda v, cur=cur, ca=ca, gi=gi, w=w: v.scalar_tensor_tensor(out=pzT[:, 2 * gi:2 * gi + 2, :], in0=cur[:, :, HALO:W],
                                                                                   scalar=1.0 / w, in1=ca[:, :, HALO:W],
                                                                                   op0=ALU.mult, op1=ALU.subtract),
               reads=[cur_t, aT_tok], writes=[pz_tok])

    def stepA_halo():
        dma("sp", Xv[0][0:HALO, :], xh_d[:, :], writes=[X_tok[0]])
        rms_rstd("act", Xv[0][0:HALO, :], [X_tok[0]], xn[0][0:HALO, :], [xn_tok[0]], 0, npart=HALO)
        op("act", lambda a: a.activation(out=xn[0][0:HALO, :], in_=Xv[0][0:HALO, :], func=AF.Copy, scale=sm[0:HALO, 0:1]),
           reads=[X_tok[0], sm_tok], writes=[xn_tok[0]])
        for q in range(4):
            pb, pt = bank()
            pbb = pb[:, :].bitcast(BF16)
            for kk in range(4):
                k = q * 4 + kk
                op("pe", lambda pe, k=k, kk=kk, pbb=pbb: pe.transpose(out=pbb[:, kk * HALO:(kk + 1) * HALO],
                                                                       in_=xn[0][0:HALO, k * 128:(k + 1) * 128],
                                                                       identity=ident_b[0:HALO, 0:HALO]),
                   reads=[xn_tok[0]] + CONST, writes=[pt])
            for kk in range(4):
                k = q * 4 + kk
                op("dve", lambda v, k=k, kk=kk, pbb=pbb: v.tensor_scalar(out=hhalo[:, k, :], in0=pbb[:, kk * HALO:(kk + 1) * HALO],
                                                                         scalar1=cvec[:, CV_GMIX + k:CV_GMIX + k + 1],
                                                                         scalar2=None, op0=ALU.mult),
                   reads=[pt] + CONST, writes=[hh_tok])

    def stepA_load(b, j):
        t0 = b * TB
        dma("sp", Xv[j], x_d[t0 + j * 128:t0 + (j + 1) * 128, :], writes=[X_tok[j]])

    def stepA_tile(b, j, xi):
        xb = xn[xi]
        xt = xn_tok[xi]
        rms_rstd("act", Xv[j], [X_tok[j]], xb[:, :], [xt], j)
        op("act", lambda a, j=j, xb=xb: a.activation(out=xb[:, :], in_=Xv[j], func=AF.Copy, scale=sm[:, j:j + 1]),
           reads=[X_tok[j], sm_tok], writes=[xt])
        transposes_bf(xb, [xt], lambda k, j=j: hT[:, k, j * 128:(j + 1) * 128], [hT_tok], CV_GMIX, evac_rr)

    stepA_halo()
    for j in range(JT):
        stepA_load(0, j)
    for j in range(JT):
        stepA_tile(0, j, j % 2)

    for b in range(NB):
        blk = plan[b]
        t0 = b * TB
        if b > 0:
            op("dve", lambda v: v.tensor_copy(out=aT[:, :, 0:HALO], in_=aT[:, :, TB:TB + HALO]), reads=[aT_tok], writes=[aT_tok])
        for cbi, cb in enumerate(WIN_ORDER):
            if cbi > 0:
                wdone(blk["win"][WIN_ORDER[cbi - 1]])
            if cbi == 2:
                emit_layernorm()
            if cbi == 4:
                emit_pool_dve(b)
            s, st = wget(blk["win"][cb])
            wv = wview(s, 16, 512)
            if cb < 4:
                for mm in range(4):
                    m = (cb % 2) * 4 + mm
                    pb, pt = bank()
                    for k in range(16):
                        op("pe", lambda pe, k=k, mm=mm, pb=pb, wv=wv: pe.matmul(pb[:, :], lhsT=wv[:, k, mm * 128:(mm + 1) * 128],
                                                                               rhs=hT[:, k, :], start=(k == 0), stop=(k == 15)),
                           reads=[st, hT_tok], writes=[pt])
                    if cb < 2:
                        op("dve", lambda v, m=m, pb=pb: v.tensor_copy(out=aT[:, m, HALO:HALO + TB], in_=pb[:, :]),
                           reads=[pt], writes=[aT_tok])
                        if b == 0:
                            pb2, pt2 = bank()
                            for k in range(16):
                                op("pe", lambda pe, k=k, mm=mm, pb2=pb2, wv=wv: pe.matmul(pb2[:, 0:HALO], lhsT=wv[:, k, mm * 128:(mm + 1) * 128],
                                                                                         rhs=hhalo[:, k, :], start=(k == 0), stop=(k == 15)),
                                   reads=[st, hh_tok], writes=[pt2])
                            op("dve", lambda v, m=m, pb2=pb2: v.tensor_copy(out=aT[:, m, 0:HALO], in_=pb2[:, 0:HALO]),
                               reads=[pt2], writes=[aT_tok])
                    else:
                        op("act", lambda a, m=m, pb=pb: a.activation(out=uT[:, m, :], in_=pb[:, :], func=AF.Gelu_apprx_tanh),
                           reads=[pt], writes=[uT_tok])
            else:
                half = cb - 4
                for j in range(JT):
                    pb, pt = bank()
                    for k in range(16):
                        op("pe", lambda pe, k=k, j=j, pb=pb, wv=wv: pe.matmul(pb[:, :], lhsT=hT[:, k, j * 128:(j + 1) * 128],
                                                                             rhs=wv[:, k, :], start=(k == 0), stop=(k == 15)),
                           reads=[st, hT_tok], writes=[pt])
                    op("act", lambda a, j=j, half=half, pb=pb: a.activation(out=scrv(j)[:, half * 512:(half + 1) * 512], in_=pb[:, :],
                                                                            func=AF.Gelu_apprx_tanh),
                       reads=[pt], writes=[scr_tok[j]])
        wdone(blk["win"][WIN_ORDER[-1]])
        for gi in range(4):
            for mo in range(2):
                pb, pt = bank()
                for ki in range(2):
                    op("pe", lambda pe, gi=gi, mo=mo, ki=ki, pb=pb: pe.matmul(pb[:, :], lhsT=wpool[:, gi, ki, mo * 128:(mo + 1) * 128],
                                                                             rhs=pzT[:, 2 * gi + ki, :], start=(ki == 0), stop=(ki == 1)),
                       reads=[pz_tok] + CONST, writes=[pt])
                c = 2 * gi + mo
                op("act", lambda a, c=c, pb=pb: a.activation(out=ypT[:, c, :], in_=pb[:, :], func=AF.Copy,
                                                             scale=cvec[:, CV_PSC + c:CV_PSC + c + 1]),
                   reads=[pt] + CONST, writes=[yp_tok])
        for g8 in range(8):
            pb, pt = bank()
            for j in range(JT):
                op("pe", lambda pe, g8=g8, j=j, pb=pb: pe.matmul(pb[:, j * 128:(j + 1) * 128], lhsT=vT[:, j, g8 * 128:(g8 + 1) * 128],
                                                                 rhs=wsT[:, g8, :], start=True, stop=True),
                   reads=[vT_tok] + CONST, writes=[pt])
            tmp = scrv(1, 512)
            for j in range(JT):
                op("dve", lambda v, g8=g8, j=j, pb=pb, tmp=tmp: v.tensor_tensor(out=tmp[:, j * 128:(j + 1) * 128], in0=pb[:, j * 128:(j + 1) * 128],
                                                                               in1=bspB[:, g8, :], op=ALU.add),
                   reads=[pt] + CONST, writes=[scr_tok[1]])
            op("dve", lambda v, g8=g8, tmp=tmp: v.tensor_tensor(out=uT[:, g8, :], in0=uT[:, g8, :], in1=tmp, op=ALU.mult),
               reads=[scr_tok[1], uT_tok], writes=[uT_tok])

        for cb in range(4):
            u_ab, u_g1, u_g2 = blk["mrg"][cb]
            for gi2, ug_ in enumerate((u_g1, u_g2)):
                s_g, t_g = wget(ug_)
                wgm = wview(s_g, 16, 512)
                for mm in range(4):
                    m = cb * 4 + mm
                    p1, p1t = bank()
                    for k in range(16):
                        op("pe", lambda pe, k=k, mm=mm, p1=p1, wgm=wgm: pe.matmul(p1[:, :], lhsT=wgm[:, k, mm * 128:(mm + 1) * 128], rhs=hT[:, k, :],
                                                                                 start=(k == 0), stop=(k == 15)),
                           reads=[t_g, hT_tok], writes=[p1t])
                    sgt = scrv(mm)[:, gi2 * 512:(gi2 + 1) * 512]
                    op("act", lambda a, m=m, p1=p1, sgt=sgt, gi2=gi2: a.activation(out=sgt, in_=p1[:, :], func=AF.Sigmoid,
                                                                                    bias=cvec[:, CV_BMG + 16 * gi2 + m:CV_BMG + 16 * gi2 + m + 1], scale=1.0),
                       reads=[p1t] + CONST, writes=[scr_tok[mm]])
                wdone(ug_)
            s_ab, t_ab = wget(u_ab)
            wa = pages[s_ab][:, 0:4096].rearrange("p (k n) -> p k n", k=8)
            wb = pages[s_ab][:, 4096:8192].rearrange("p (k n) -> p k n", k=8)
            for mm in range(4):
                m = cb * 4 + mm
                pa, pat = bank()
                for k in range(8):
                    op("pe", lambda pe, k=k, mm=mm, pa=pa, wa=wa: pe.matmul(pa[:, :], lhsT=wa[:, k, mm * 128:(mm + 1) * 128], rhs=ypT[:, k, :],
                                                                           start=(k == 0), stop=(k == 7)),
                       reads=[t_ab, yp_tok], writes=[pat])
                pbk, pbt = bank()
                for k in range(8):
                    op("pe", lambda pe, k=k, mm=mm, pbk=pbk, wb=wb: pe.matmul(pbk[:, :], lhsT=wb[:, k, mm * 128:(mm + 1) * 128], rhs=uT[:, k, :],
                                                                             start=(k == 0), stop=(k == 7)),
                       reads=[t_ab, uT_tok], writes=[pbt])
                s1 = scrv(mm)[:, 0:512]
                s2 = scrv(mm)[:, 512:1024]
                op("dve", lambda v, pa=pa, s1=s1: v.tensor_tensor(out=s1, in0=s1, in1=pa[:, :], op=ALU.mult),
                   reads=[pat, scr_tok[mm]], writes=[scr_tok[mm]])
                op("dve", lambda v, pbk=pbk, s2=s2: v.tensor_tensor(out=s2, in0=s2, in1=pbk[:, :], op=ALU.mult),
                   reads=[pbt, scr_tok[mm]], writes=[scr_tok[mm]])
                op("dve", lambda v, m=m, s1=s1, s2=s2: v.tensor_tensor(out=mT[:, m, :], in0=s1, in1=s2, op=ALU.add),
                   reads=[scr_tok[mm]], writes=[mT_tok])
            wdone(u_ab)

        for cb in range(4):
            s, st = wget(blk["wout"][cb])
            wv = wview(s, 16, 512)
            for j in range(JT):
                pb, pt = bank()
                for k in range(16):
                    op("pe", lambda pe, k=k, j=j, pb=pb, wv=wv: pe.matmul(pb[:, :], lhsT=mT[:, k, j * 128:(j + 1) * 128], rhs=wv[:, k, :],
                                                                         start=(k == 0), stop=(k == 15)),
                       reads=[st, mT_tok], writes=[pt])
                op("dve", lambda v, j=j, cb=cb, pb=pb: v.tensor_tensor(out=Xv[j][:, cb * 512:(cb + 1) * 512], in0=Xv[j][:, cb * 512:(cb + 1) * 512],
                                                                      in1=pb[:, :], op=ALU.add),
                   reads=[pt, X_tok[j]], writes=[X_tok[j]])
            wdone(blk["wout"][cb])
        for j in range(JT):
            ti = b * JT + j
            if stage == 1:
                dma("sp", y_d[ti * 128:(ti + 1) * 128, :], Xv[j], reads=[X_tok[j]], writes=[x1s_tok[ti]])
                if b + 1 < NB:
                    stepA_load(b + 1, j)
                    stepA_tile(b + 1, j, (j + 1) % 2)
                continue
            dma("sp", x1_d[ti * 128:(ti + 1) * 128, :], Xv[j], reads=[X_tok[j]], writes=[x1s_tok[ti]])
            xb = xn[j % 2]
            xt = xn_tok[j % 2]
            rms_rstd("act", Xv[j], [X_tok[j]], xb[:, :], [xt], 8)
            op("act", lambda a, j=j, xb=xb: a.activation(out=xb[:, :], in_=Xv[j], func=AF.Copy, scale=sm[:, 8:9]),
               reads=[X_tok[j], sm_tok], writes=[xt])
            x1T = pages[8][:, 0:4096].bitcast(F32).rearrange("p (k t) -> p k t", k=16)
            for q in range(4):
                pb, pt = bank()
                for kk in range(4):
                    k = q * 4 + kk
                    op("pe", lambda pe, k=k, kk=kk, pb=pb, j=j: pe.transpose(out=pb[:, kk * 128:(kk + 1) * 128], in_=Xv[j][:, k * 128:(k + 1) * 128],
                                                                            identity=ident_f[:, :]),
                       reads=[X_tok[j]] + CONST, writes=[pt])
                x1q = x1T[:, q * 4:(q + 1) * 4, :]
                pbq = pb[:, :].rearrange("p (k t) -> p k t", k=4)
                if q % 2 == 0:
                    op("act", lambda a, pbq=pbq, x1q=x1q: a.activation(out=x1q, in_=pbq, func=AF.Copy), reads=[pt], wd=[pz_tok])
                else:
                    op("dve", lambda v, pbq=pbq, x1q=x1q: v.tensor_copy(out=x1q, in_=pbq), reads=[pt], wd=[pz_tok])
            pl, plt = bank()
            for k in range(16):
                op("pe", lambda pe, k=k, pl=pl: pe.matmul(pl[:, 0:36], lhsT=x1T[:, k, :], rhs=wr_sb[:, k, :], start=(k == 0), stop=(k == 15)),
                   reads=[pz_tok] + CONST, writes=[plt])
            if b + 1 < NB:
                stepA_load(b + 1, j)
                stepA_tile(b + 1, j, (j + 1) % 2)
            LG = sm[:, 32:68]
            GL = sm[:, 32:36]
            EL = sm[:, 36:68]
            GOH = sm[:, 68:72]
            PEN = sm[:, 72:76]
            EM = sm[:, 76:108]
            TOP = sm[:, 108:116]
            M0 = sm[:, 116:148]
            M1 = sm[:, 148:180]
            MM = sm[:, 180:212]
            RP = sm[:, 212:244]
            OK_ = sm[:, 244:276]
            JK = sm[:, 276:308]
            SC = sm[:, 308:324]
            GEX = sm[:, 324:328]
            R = [sm_tok]
            op("dve", lambda v, pl=pl: v.scalar_tensor_tensor(out=LG, in0=pl[:, 0:36], scalar=sm[:, 8:9], in1=rbB[:, :],
                                                              op0=ALU.mult, op1=ALU.add), reads=[plt, sm_tok] + CONST, writes=R)
            op("dve", lambda v: v.tensor_reduce(out=SC[:, 0:1], in_=GL, axis=AX.X, op=ALU.max), reads=R, writes=R)
            op("dve", lambda v: v.tensor_scalar(out=GOH, in0=GL, scalar1=SC[:, 0:1], scalar2=None, op0=ALU.is_ge), reads=R, writes=R)
            op("dve", lambda v: v.tensor_scalar(out=SC[:, 1:2], in0=SC[:, 0:1], scalar1=-1.0, scalar2=None, op0=ALU.mult), reads=R, writes=R)
            op("act", lambda a: a.activation(out=GEX, in_=GL, func=AF.Exp, bias=SC[:, 1:2], scale=1.0, accum_out=SC[:, 2:3]),
               reads=R, writes=R)
            op("dve", lambda v: v.reciprocal(out=SC[:, 3:4], in_=SC[:, 2:3]), reads=R, writes=R)
            op("dve", lambda v: v.tensor_scalar(out=PEN, in0=GOH, scalar1=1e30, scalar2=-1e30, op0=ALU.mult, op1=ALU.add), reads=R, writes=R)
            for g4 in range(4):
                op("dve", lambda v, g4=g4: v.tensor_scalar(out=EM[:, g4 * 8:(g4 + 1) * 8], in0=EL[:, g4 * 8:(g4 + 1) * 8],
                                                           scalar1=PEN[:, g4:g4 + 1], scalar2=None, op0=ALU.add), reads=R, writes=R)
            op("dve", lambda v: v.max(out=TOP, in_=EM), reads=R, writes=R)
            op("dve", lambda v: v.tensor_scalar(out=M0, in0=EM, scalar1=TOP[:, 0:1], scalar2=None, op0=ALU.is_equal), reads=R, writes=R)
            op("dve", lambda v: v.tensor_scalar(out=M1, in0=EM, scalar1=TOP[:, 1:2], scalar2=None, op0=ALU.is_equal), reads=R, writes=R)
            op("dve", lambda v: v.tensor_scalar(out=SC[:, 4:5], in0=TOP[:, 0:1], scalar1=-1.0, scalar2=None, op0=ALU.mult), reads=R, writes=R)
            op("act", lambda a: a.activation(out=SC[:, 5:6], in_=TOP[:, 1:2], func=AF.Exp, bias=SC[:, 4:5], scale=1.0), reads=R, writes=R)
            op("dve", lambda v: v.tensor_scalar(out=SC[:, 6:7], in0=SC[:, 5:6], scalar1=1.0, scalar2=None, op0=ALU.add), reads=R, writes=R)
            op("dve", lambda v: v.reciprocal(out=SC[:, 6:7], in_=SC[:, 6:7]), reads=R, writes=R)
            op("dve", lambda v: v.tensor_tensor(out=MM, in0=M0, in1=M1, op=ALU.add), reads=R, writes=R)
            pr, prt = bank()
            op("pe", lambda pe, pr=pr: pe.matmul(pr[:, 0:NE], lhsT=ltri_f[:, :], rhs=MM, start=True, stop=False),
               reads=R + CONST + [rt_tok], writes=[prt])
            op("pe", lambda pe, pr=pr: pe.matmul(pr[:, 0:NE], lhsT=ones_f[:, :], rhs=msum[:, :], start=False, stop=True),
               reads=R + CONST + [rt_tok], writes=[prt])
            op("dve", lambda v, pr=pr: v.tensor_tensor(out=RP, in0=pr[:, 0:NE], in1=offs[:, :], op=ALU.add), reads=[prt] + R + CONST, writes=R)
            op("dve", lambda v, pr=pr: v.tensor_scalar(out=OK_, in0=pr[:, 0:NE], scalar1=float(CAP), scalar2=None, op0=ALU.is_le),
               reads=[prt] + R, writes=R)
            op("dve", lambda v: v.tensor_tensor(out=msum[:, :], in0=msum[:, :], in1=MM, op=ALU.add), reads=R + [rt_tok], writes=[rt_tok])
            for kk, MK in enumerate((M0, M1)):
                op("dve", lambda v, kk=kk, MK=MK: v.tensor_tensor(out=JK, in0=MK, in1=RP, op=ALU.mult), reads=R, writes=R)
                op("dve", lambda v, kk=kk: v.tensor_reduce(out=SC[:, 7 + kk:8 + kk], in_=JK, axis=AX.X, op=ALU.add), reads=R, writes=R)
                op("dve", lambda v, kk=kk, MK=MK: v.tensor_tensor(out=JK, in0=MK, in1=OK_, op=ALU.mult), reads=R, writes=R)
                op("dve", lambda v, kk=kk: v.tensor_reduce(out=SC[:, 9 + kk:10 + kk], in_=JK, axis=AX.X, op=ALU.add), reads=R, writes=R)
                op("dve", lambda v, kk=kk: v.tensor_scalar(out=SC[:, 7 + kk:8 + kk], in0=SC[:, 7 + kk:8 + kk], scalar1=-BIG,
                                                           scalar2=SC[:, 9 + kk:10 + kk], op0=ALU.add, op1=ALU.mult), reads=R, writes=R)
                op("dve", lambda v, kk=kk: v.tensor_scalar(out=SC[:, 7 + kk:8 + kk], in0=SC[:, 7 + kk:8 + kk], scalar1=BIG,
                                                           scalar2=None, op0=ALU.add), reads=R, writes=R)
                op("dve", lambda v, kk=kk, ti=ti: v.tensor_copy(out=idx_all[:, ti, kk:kk + 1], in_=SC[:, 7 + kk:8 + kk]),
                   reads=R, writes=[rt_tok])
            op("dve", lambda v, ti=ti: v.tensor_scalar(out=wt_all[:, ti, 0:1], in0=SC[:, 3:4], scalar1=SC[:, 6:7], scalar2=SC[:, 9:10],
                                                       op0=ALU.mult, op1=ALU.mult), reads=R, writes=[rt_tok])
            op("dve", lambda v, ti=ti: v.tensor_scalar(out=SC[:, 11:12], in0=SC[:, 3:4], scalar1=SC[:, 6:7], scalar2=SC[:, 5:6],
                                                       op0=ALU.mult, op1=ALU.mult), reads=R, writes=R)
            op("dve", lambda v, ti=ti: v.tensor_scalar(out=wt_all[:, ti, 1:2], in0=SC[:, 11:12], scalar1=SC[:, 10:11], scalar2=None,
                                                       op0=ALU.mult), reads=R, writes=[rt_tok])
            for kk in range(2):
                dma("pool", xs_d[:, :], xb[:, :], reads=[xt, rt_tok], writes=[], dtok=xs_tok, indirect=True,
                    out_offset=bass.IndirectOffsetOnAxis(ap=idx_all[:, ti, kk:kk + 1], axis=0), in_offset=None,
                    bounds_check=bc_reg, oob_is_err=False)
            xs_tok.w = {xs_tok.dkey: xs_tok.dval}

    if stage == 1:
        fin = {}
        for t in x1s_tok:
            Ctx._merge(fin, t.w)
        cx._wait("sp", fin)
        return nc

    barrier = cx.snapshot()
    moe_slots = [0, 1, 2, 5, 6, 7]
    for i in (5, 6, 7):
        slot_tok[i].w = dict(barrier)
    Ysb = Xv
    Y_tok = [cx.dma_tok(f"Ysb{j}", init=barrier) for j in range(4)]
    XeT = [pages[8][:, i * 4096:(i + 1) * 4096].rearrange("p (k r) -> p k r", k=16) for i in range(2)]
    XeT_tok = [cx.tok(f"XeT{i}", init=barrier) for i in range(2)]
    hidT = [aT[:, :, :].rearrange("p k t -> p (k t)")[:, i * 1024:(i + 1) * 1024].rearrange("p (m r) -> p m r", m=4) for i in range(2)]
    hid_tok = [cx.tok(f"hid{i}", init=barrier) for i in range(2)]
    ys_tok = cx.dma_tok("ys")
    slot_rr[0] = 0
    exp_units = []
    for e in range(NE):
        ug = sched_unit(lambda s, e=e: [(wview(s, 16, 512), wsrc(w_eg_d[e], 0, D, 0, 512))], moe_slots)
        uu = sched_unit(lambda s, e=e: [(wview(s, 16, 512), wsrc(w_eu_d[e], 0, D, 0, 512))], moe_slots)
        ud = sched_unit(lambda s, e=e: [(wview(s, 4, 2048), wsrc(w_ed_d[e], 0, 512, 0, D))], moe_slots)
        exp_units.append((ug, uu, ud))

    def wget_moe(u):
        ws.ensure(u)
        s = ws.units[u][0]
        return s, slot_tok[s]

    ws.ensure(exp_units[0][0] + 5)

    yrr = [0]

    def moe_rows_load(e):
        for r in range(2):
            dma("sp", xn[r][:, :], xs_d[e * CAP + r * 128:e * CAP + (r + 1) * 128, :], reads=[xs_tok], writes=[xn_tok[r]])

    def moe_transposes(e):
        xe = XeT[e % 2]
        xet = XeT_tok[e % 2]
        for r in range(2):
            transposes_bf(xn[r], [xn_tok[r]], lambda k, r=r, xe=xe: xe[:, k, r * 128:(r + 1) * 128], [xet], CV_GFFN, evac_rr)

    moe_rows_load(0)
    moe_transposes(0)
    for e in range(NE):
        ug, uu, ud = exp_units[e]
        xe = XeT[e % 2]
        xet = XeT_tok[e % 2]
        if e + 1 < NE:
            moe_rows_load(e + 1)
        sg_, tg_ = wget_moe(ug)
        su_, tu_ = wget_moe(uu)
        sd_, td_ = wget_moe(ud)
        wg = wview(sg_, 16, 512)
        wu = wview(su_, 16, 512)
        wd = wview(sd_, 4, 2048)
        hd = hidT[e % 2]
        hdt = hid_tok[e % 2]
        gu_banks = [bank() for _ in range(4)]
        for m in range(4):
            pb, pt = gu_banks[m]
            for k in range(16):
                op("pe", lambda pe, k=k, m=m, pb=pb: pe.matmul(pb[:, 0:CAP], lhsT=wg[:, k, m * 128:(m + 1) * 128], rhs=xe[:, k, :],
                                                               start=(k == 0), stop=(k == 15)), reads=[tg_, xet], writes=[pt])
        wdone(ug, 6)
        for m in range(4):
            pb, pt = gu_banks[m]
            for k in range(16):
                op("pe", lambda pe, k=k, m=m, pb=pb: pe.matmul(pb[:, CAP:2 * CAP], lhsT=wu[:, k, m * 128:(m + 1) * 128], rhs=xe[:, k, :],
                                                               start=(k == 0), stop=(k == 15)), reads=[tu_, xet], writes=[pt])
        wdone(uu, 6)
        for m in range(4):
            pb, pt = gu_banks[m]
            si = m % 2 + 2
            sg = scrv(si, CAP)
            op("act", lambda a, pb=pb, sg=sg: a.activation(out=sg, in_=pb[:, 0:CAP], func=AF.Silu), reads=[pt], writes=[scr_tok[si]])
            op("dve", lambda v, pb=pb, sg=sg, m=m: v.tensor_tensor(out=hd[:, m, :], in0=sg, in1=pb[:, CAP:2 * CAP], op=ALU.mult),
               reads=[pt, scr_tok[si]], writes=[hdt])
        if e + 1 < NE:
            moe_transposes(e + 1)
        for r in range(2):
            yi = yrr[0] % 4
            yrr[0] += 1
            for cb in range(4):
                pb, pt = bank()
                for m in range(4):
                    op("pe", lambda pe, m=m, r=r, cb=cb, pb=pb: pe.matmul(pb[:, :], lhsT=hd[:, m, r * 128:(r + 1) * 128],
                                                                         rhs=wd[:, m, cb * 512:(cb + 1) * 512], start=(m == 0), stop=(m == 3)),
                       reads=[td_, hdt], writes=[pt])
                if cb % 2 == 0:
                    op("act", lambda a, yi=yi, cb=cb, pb=pb: a.activation(out=Ysb[yi][:, cb * 512:(cb + 1) * 512], in_=pb[:, :], func=AF.Copy),
                       reads=[pt], wd=[Y_tok[yi]])
                else:
                    op("dve", lambda v, yi=yi, cb=cb, pb=pb: v.tensor_copy(out=Ysb[yi][:, cb * 512:(cb + 1) * 512], in_=pb[:, :]),
                       reads=[pt], wd=[Y_tok[yi]])
            dma("sp", ys_d[e * CAP + r * 128:e * CAP + (r + 1) * 128, :], Ysb[yi], reads=[Y_tok[yi]], writes=[], dtok=ys_tok)
        wdone(ud, 6)
    ys_tok.w = {ys_tok.dkey: ys_tok.dval}

    barrier2 = cx.snapshot()
    pg_pages = [0, 1, 2, 5]
    for ci, pgi in enumerate(pg_pages):
        dma("pool", wview(pgi, 16, 512), wsrc(w_pg_d, 0, D, ci * 512, 512), writes=[slot_tok[pgi]])
    wpg_tok = slot_tok[6]
    wpu = pages[6][:, 0:4096].rearrange("p (k n) -> p k n", k=2)
    dma("pool", wpu, w_pu_d.rearrange("(k p) n -> p k n", p=128), writes=[wpg_tok])
    gfin = pages[6][:, 4096:8192].bitcast(F32)
    bpg_b = scr[0:1, 3 * 1088:3 * 1088 + 1024].bitcast(BF16)
    scr_tok[3].w = dict(barrier2)
    bpg_tok = cx.dma_tok("bpg", init=barrier2)
    dma("pool", bpg_b, rows_d[0:1, R_BPG:R_BPG + D], writes=[bpg_tok])
    dma("sp", gfin, rows_d[0:1, R_GFIN:R_GFIN + D].partition_broadcast(128), writes=[wpg_tok])
    Gv = [pages[7 + (i // 2)][:, (i % 2) * 4096:(i % 2 + 1) * 4096].bitcast(F32) for i in range(4)]
    G_tok = [cx.dma_tok(f"G{i}", init=barrier2) for i in range(4)]
    for i in range(4):
        op("dve", lambda v, i=i: v.memset(Gv[i], 0.0), writes=[G_tok[i]])
    Xf_tok = [cx.dma_tok(f"Xf{j}", init=barrier2) for j in range(4)]
    h3T = [aT[:, :, :].rearrange("p k t -> p (k t)")[:, i * 2048:(i + 1) * 2048].rearrange("p (k t) -> p k t", k=16) for i in range(2)]
    h3_tok = [cx.tok(f"h3T{i}", init=barrier2) for i in range(2)]
    pbf = [sb(f"pbf{i}", [128, 256], BF16) for i in range(2)]
    pbf_tok = [cx.dma_tok(f"pbf{i}") for i in range(2)]
    pT = [sb(f"pT{i}", [128, 2, 128], BF16) for i in range(2)]
    pT_tok = [cx.tok(f"pT{i}") for i in range(2)]
    out_tok = cx.dma_tok("out")
    for st_ in scr_tok:
        pass
    def ple_A(ti):
        j = ti % 4
        Xt, Xk = Xv[j], Xf_tok[j]
        dma("sp", Xt, x1_d[ti * 128:(ti + 1) * 128, :], reads=[x1s_tok[ti]], writes=[Xk])
        for kk in range(2):
            gi = (ti % 2) * 2 + kk
            dma("pool", Gv[gi], ys_d[:, :], reads=[ys_tok, rt_tok], writes=[G_tok[gi]], indirect=True, out_offset=None,
                in_offset=bass.IndirectOffsetOnAxis(ap=idx_all[:, ti, kk:kk + 1], axis=0),
                bounds_check=bc_reg, oob_is_err=False)
        dma("pool", pbf[ti % 2][:, :], p_d[ti * 128:(ti + 1) * 128, :], writes=[pbf_tok[ti % 2]])
        for kk in range(2):
            gi = (ti % 2) * 2 + kk
            op("dve", lambda v, gi=gi, ti=ti, kk=kk, Xt=Xt: v.scalar_tensor_tensor(out=Xt, in0=Gv[gi], scalar=wt_all[:, ti, kk:kk + 1], in1=Xt,
                                                                                  op0=ALU.mult, op1=ALU.add),
               reads=[G_tok[gi], rt_tok, Xk], writes=[Xk])
        xb = xn[ti % 2]
        xt = xn_tok[ti % 2]
        rms_rstd("act", Xt, [Xk], xb[:, :], [xt], 9)
        op("act", lambda a, xb=xb, Xt=Xt: a.activation(out=xb[:, :], in_=Xt, func=AF.Copy, scale=sm[:, 9:10]),
           reads=[Xk, sm_tok], writes=[xt])

    def ple_C(ti):
        xb = xn[ti % 2]
        xt = xn_tok[ti % 2]
        h3 = h3T[ti % 2]
        h3t = h3_tok[ti % 2]
        transposes_bf(xb, [xt], lambda k, h3=h3: h3[:, k, :], [h3t], CV_GPLE, evac_rr)
        pb, pt = bank()
        pbb = pb[:, :].bitcast(BF16)
        for kk in range(2):
            op("pe", lambda pe, kk=kk, pbb=pbb, ti=ti: pe.transpose(out=pbb[:, kk * 128:(kk + 1) * 128], in_=pbf[ti % 2][:, kk * 128:(kk + 1) * 128],
                                                                   identity=ident_b[:, :]), reads=[pbf_tok[ti % 2]] + CONST, writes=[pt])
        op("dve", lambda v, pbb=pbb, ti=ti: v.tensor_copy(out=pT[ti % 2][:, :, :], in_=pbb[:, 0:256].rearrange("p (k t) -> p k t", k=2)),
           reads=[pt], writes=[pT_tok[ti % 2]])

    def ple_B(ti):
        j = ti % 4
        Xt, Xk = Xv[j], Xf_tok[j]
        xb = xn[ti % 2]
        xt = xn_tok[ti % 2]
        h3 = h3T[ti % 2]
        h3t = h3_tok[ti % 2]
        for cb in range(4):
            wg_ = wview(pg_pages[cb], 16, 512)
            pg_, pgt = bank()
            for k in range(16):
                op("pe", lambda pe, k=k, pg_=pg_, wg_=wg_, h3=h3: pe.matmul(pg_[:, :], lhsT=h3[:, k, :], rhs=wg_[:, k, :], start=(k == 0), stop=False),
                   reads=[slot_tok[pg_pages[cb]], h3t], writes=[pgt])
            op("pe", lambda pe, pg_=pg_, cb=cb: pe.matmul(pg_[:, :], lhsT=ones_b[0:1, :], rhs=bpg_b[0:1, cb * 512:(cb + 1) * 512], start=False, stop=True),
               reads=CONST + [bpg_tok], writes=[pgt])
            pu_, put = bank()
            for k in range(2):
                op("pe", lambda pe, k=k, pu_=pu_, cb=cb, ti=ti: pe.matmul(pu_[:, :], lhsT=pT[ti % 2][:, k, :], rhs=wpu[:, k, cb * 512:(cb + 1) * 512],
                                                                         start=(k == 0), stop=(k == 1)), reads=[wpg_tok, pT_tok[ti % 2]], writes=[put])
            si = cb % 2
            sg = scrv(si, 512)
            op("act", lambda a, pg_=pg_, sg=sg: a.activation(out=sg, in_=pg_[:, :], func=AF.Sigmoid), reads=[pgt], writes=[scr_tok[si]])
            op("dve", lambda v, pu_=pu_, sg=sg: v.tensor_tensor(out=sg, in0=sg, in1=pu_[:, :], op=ALU.mult), reads=[put, scr_tok[si]], writes=[scr_tok[si]])
            op("dve", lambda v, sg=sg, cb=cb, Xt=Xt: v.tensor_tensor(out=Xt[:, cb * 512:(cb + 1) * 512], in0=Xt[:, cb * 512:(cb + 1) * 512], in1=sg, op=ALU.add),
               reads=[scr_tok[si], Xk], writes=[Xk])

    def ple_B2(ti):
        j = ti % 4
        Xt, Xk = Xv[j], Xf_tok[j]
        xb = xn[ti % 2]
        xt = xn_tok[ti % 2]
        rms_rstd("act", Xt, [Xk], xb[:, :], [xt], 10)
        op("dve", lambda v, Xt=Xt: v.scalar_tensor_tensor(out=Xt, in0=Xt, scalar=sm[:, 10:11], in1=gfin, op0=ALU.mult, op1=ALU.mult),
           reads=[Xk, sm_tok, wpg_tok], writes=[Xk])
        dma("sp", y_d[ti * 128:(ti + 1) * 128, :], Xt, reads=[Xk], writes=[], dtok=out_tok)

    ple_A(0)
    ple_C(0)
    for ti in range(NT):
        if ti + 1 < NT:
            ple_A(ti + 1)
        ple_B(ti)
        if ti + 1 < NT:
            ple_C(ti + 1)
        ple_B2(ti)
    if DEBUG_X1:
        dbg_w = nc.dram_tensor("dbg_w", [128, NT * 2], F32, kind="ExternalOutput").ap()
        dbg_i = nc.dram_tensor("dbg_i", [128, NT * 2], I32, kind="ExternalOutput").ap()
        dma("sp", dbg_w[:, :], wt_all[:, :, :].rearrange("p a b -> p (a b)"), reads=[rt_tok], writes=[], dtok=out_tok)
        dma("sp", dbg_i[:, :], idx_all[:, :, :].rearrange("p a b -> p (a b)"), reads=[rt_tok], writes=[], dtok=out_tok)
    cx._wait("sp", {out_tok.dkey: out_tok.dval})
    return nc


_NC_CACHE = {}


def _prep_inputs(inp):
    f = lambda a: np.ascontiguousarray(np.asarray(a, dtype=np.float32))
    x = f(inp["x"]).reshape(4, 4096, D)
    p = f(inp["p"]).reshape(4, 4096, 256)

    def pk(v):
        v = f(v).reshape(-1)
        return v.reshape(-1, 128).T

    cvec = np.concatenate([pk(inp["g_mix"]), pk(inp["g_ffn"]), pk(inp["g_ple"]), pk(inp["pool_scale"]),
                           pk(inp["b_merge_gate"])], axis=1)
    assert cvec.shape == (128, CV_N)
    wrg = f(inp["w_router_group"]).reshape(D, 4)
    wre = f(inp["w_router_expert"]).reshape(4, D, 8).transpose(1, 0, 2).reshape(D, 32)
    wr = np.concatenate([wrg, wre], axis=1)
    wr = wr.reshape(16, 128, 36).transpose(1, 0, 2).reshape(128, 16 * 36)
    rb = np.concatenate([f(inp["b_router_group"]).reshape(-1), f(inp["b_router_expert"]).reshape(-1)])
    shared = {
        "cvec": np.ascontiguousarray(cvec),
        "wr": np.ascontiguousarray(wr),
        "w_in": f(inp["w_in"]).reshape(D, 3072),
        "w_pool": f(inp["w_pool"]).reshape(4, 256, 256),
        "w_ba": f(inp["w_branch_a"]).reshape(1024, D),
        "w_bb": f(inp["w_branch_b"]).reshape(1024, D),
        "w_sp": f(inp["w_spatial"]).reshape(8, 128, 128),
        "w_mg": f(inp["w_merge_gate"]).reshape(D, 2 * D),
        "w_out": f(inp["w_out"]).reshape(D, D),
        "w_eg": f(inp["w_exp_gate"]).reshape(NE, D, 512),
        "w_eu": f(inp["w_exp_up"]).reshape(NE, D, 512),
        "w_ed": f(inp["w_exp_down"]).reshape(NE, 512, D),
        "w_pg": f(inp["w_ple_gate"]).reshape(D, D),
        "w_pu": f(inp["w_ple_up"]).reshape(256, D),
    }
    maps = []
    for c in range(NCORES):
        bi, half = c // 2, c % 2
        t0 = half * T
        rows = np.zeros((1, R_N), np.float32)
        rows[0, R_LNG:R_LNG + 1024] = f(inp["sgu_ln_g"]).reshape(-1)
        rows[0, R_LNB:R_LNB + 1024] = f(inp["sgu_ln_b"]).reshape(-1)
        rows[0, R_BSP:R_BSP + 1024] = f(inp["b_spatial"]).reshape(-1)
        rows[0, R_RB:R_RB + 36] = rb
        rows[0, R_GFIN:R_GFIN + D] = f(inp["g_final"]).reshape(-1)
        rows[0, R_BPG:R_BPG + D] = f(inp["b_ple_gate"]).reshape(-1)
        rows[0, R_POS:R_POS + HALO] = t0 + 1 + np.arange(HALO)
        xh = np.zeros((HALO, D), np.float32)
        if half == 1:
            xh[:] = x[bi, t0 - HALO:t0]
        m = dict(shared)
        m["x"] = np.ascontiguousarray(x[bi, t0:t0 + T])
        m["xh"] = xh
        m["p"] = np.ascontiguousarray(p[bi, t0:t0 + T])
        m["rows"] = rows
        maps.append(m)
    return maps


def kernel(**inputs):
    stage = 2
    if stage not in _NC_CACHE:
        _NC_CACHE[stage] = build(stage)
    nc = _NC_CACHE[stage]
    maps = _prep_inputs(inputs)
    res = run_bass_kernel_spmd(nc, maps, core_ids=list(range(NCORES)))
    out = np.empty((4, 4096, D), np.float32)
    for c in range(NCORES):
        out[c // 2, (c % 2) * T:(c % 2 + 1) * T] = res.results[c]["y"]
    return out
```

```python
import numpy as np
import concourse.bass as bass
import concourse.mybir as mybir
from concourse.bass_utils import run_bass_kernel_spmd

F32 = mybir.dt.float32
BF16 = mybir.dt.bfloat16
I32 = mybir.dt.int32
AF = mybir.ActivationFunctionType
ALU = mybir.AluOpType
AX = mybir.AxisListType

NCORES = 8
T = 2048
D = 2048
NT = T // 128
TB = 512
NB = T // TB
JT = TB // 128
HALO = 16
CAP = 256
NE = 32
EPS = 1e-6
BIG = float(1 << 20)
EPOCH = 6000
SAME_ENGINE_SYNC = True
DEBUG_X1 = False

CV_GMIX, CV_GFFN, CV_GPLE, CV_PSC, CV_BMG, CV_N = 0, 16, 32, 48, 56, 88
R_LNG, R_LNB, R_BSP, R_RB, R_GFIN, R_BPG, R_POS, R_N = 0, 1024, 2048, 3072, 3108, 5156, 7204, 7220


class Tok:
    __slots__ = ("w", "r", "name", "dkey", "dval")

    def __init__(self, name, init=None):
        self.name = name
        self.w = dict(init) if init else {}
        self.r = dict(init) if init else {}


class Ctx:
    def __init__(self):
        self.nc = bass.Bass("TRN2", target_bir_lowering=False)
        nc = self.nc
        self.eng = dict(pe=nc.tensor, act=nc.scalar, dve=nc.vector, pool=nc.gpsimd, sp=nc.sync)
        self.semh = {}
        self.cur = {}
        self.cnt = {}
        self.known = {e: {} for e in self.eng}
        self.nsem = 0
        for e in self.eng:
            self._new_epoch(e, 0)
        self.dma_sems = []

    def _sem(self, name):
        self.nsem += 1
        return self.nc.semaphore(name).__enter__()

    def _new_epoch(self, e, ep):
        key = (e, ep)
        self.semh[key] = self._sem(f"s_{e}_{ep}")
        self.cur[e] = key
        self.cnt[e] = 0

    def tok(self, name, init=None):
        return Tok(name, init)

    def dma_tok(self, name, init=None):
        t = Tok(name, init)
        key = ("dma", name)
        self.semh[key] = self._sem(f"d_{name}")
        t.dkey = key
        t.dval = 0
        return t

    def snapshot(self):
        d = {}
        for e in self.eng:
            if self.cnt[e] > 0:
                d[self.cur[e]] = self.cnt[e]
            elif self.cur[e][1] > 0:
                d[(e, self.cur[e][1] - 1)] = EPOCH
        for t in self.dma_sems:
            if t.dval > 0:
                d[t.dkey] = t.dval
        return d

    def _wait(self, e, deps):
        eng = self.eng[e]
        kn = self.known[e]
        for key, val in deps.items():
            if kn.get(key, 0) >= val:
                continue
            if key[0] == e and not SAME_ENGINE_SYNC:
                continue
            if key[0] == e and e in ("pe",):
                continue
            eng.wait_ge(self.semh[key], val)
            kn[key] = val

    @staticmethod
    def _merge(d, src):
        for k, v in src.items():
            if d.get(k, 0) < v:
                d[k] = v

    def op(self, e, fn, reads=(), writes=(), wd=()):
        deps = {}
        for t in reads:
            self._merge(deps, t.w)
        for t in writes:
            self._merge(deps, t.w)
            self._merge(deps, t.r)
        for t in wd:
            self._merge(deps, t.r)
        self._wait(e, deps)
        ins = fn(self.eng[e])
        if self.cnt[e] >= EPOCH:
            self._new_epoch(e, self.cur[e][1] + 1)
        self.cnt[e] += 1
        key, val = self.cur[e], self.cnt[e]
        ins.then_inc(self.semh[key], 1)
        for t in reads:
            t.r[key] = val
        for t in writes:
            t.w = {key: val}
            t.r = {}
        for t in wd:
            t.w[key] = val
        return ins

    def dma(self, e, out, in_, reads=(), writes=(), dtok=None, **kw):
        deps = {}
        for t in reads:
            self._merge(deps, t.w)
        for t in writes:
            self._merge(deps, t.w)
            self._merge(deps, t.r)
        self._wait(e, deps)
        if dtok is None:
            dtok = writes[0]
        if dtok not in self.dma_sems:
            self.dma_sems.append(dtok)
        eng = self.eng[e]
        if "indirect" in kw:
            kw = dict(kw)
            kw.pop("indirect")
            ins = eng.indirect_dma_start(out=out, in_=in_, **kw)
        else:
            ins = eng.dma_start(out=out, in_=in_, **kw)
        dtok.dval += 16
        ins.then_inc(self.semh[dtok.dkey], 16)
        key, val = dtok.dkey, dtok.dval
        for t in reads:
            t.r[key] = val
        for t in writes:
            if t is dtok:
                t.w = {key: val}
                t.r = {}
            else:
                t.w = {key: val}
                t.r = {}
        return ins


def build(stage=2):
    cx = Ctx()
    nc = cx.nc
    op, dma = cx.op, cx.dma

    def din(name, shape, dt=F32):
        return nc.dram_tensor(name, list(shape), dt, kind="ExternalInput").ap()

    x_d = din("x", [T, D])
    xh_d = din("xh", [HALO, D])
    p_d = din("p", [T, 256])
    cvec_d = din("cvec", [128, CV_N])
    wr_d = din("wr", [128, 16 * 36])
    rows_d = din("rows", [1, R_N])
    w_in_d = din("w_in", [D, 3072])
    w_pool_d = din("w_pool", [4, 256, 256])
    w_ba_d = din("w_ba", [1024, D])
    w_bb_d = din("w_bb", [1024, D])
    w_sp_d = din("w_sp", [8, 128, 128])
    w_mg_d = din("w_mg", [D, 2 * D])
    w_out_d = din("w_out", [D, D])
    w_eg_d = din("w_eg", [NE, D, 512])
    w_eu_d = din("w_eu", [NE, D, 512])
    w_ed_d = din("w_ed", [NE, 512, D])
    w_pg_d = din("w_pg", [D, D])
    w_pu_d = din("w_pu", [256, D])
    y_d = nc.dram_tensor("y", [T, D], F32, kind="ExternalOutput").ap()
    xs_d = nc.dram_tensor("xs_scr", [NE * CAP, D], BF16).ap()
    ys_d = nc.dram_tensor("ys_scr", [NE * CAP, D], F32).ap()
    x1_d = nc.dram_tensor("x1_scr", [T, D], F32, kind=("ExternalOutput" if DEBUG_X1 else "Internal")).ap()

    def sb(name, shape, dt):
        return nc.alloc_sbuf_tensor("sb_" + name, list(shape), dt)

    bc_reg = nc.gpsimd.to_reg(NE * CAP - 1)
    NPAGE = 9
    pages = [sb(f"pg{i}", [128, 8192], BF16) for i in range(NPAGE)]
    aT = sb("aT", [128, 8, HALO + TB], BF16)
    xn = [sb(f"xn{i}", [128, D], BF16) for i in range(2)]
    scr = sb("scr", [128, 4 * 1088], F32)
    ident_b = sb("ident_b", [128, 128], BF16)
    ident_f = sb("ident_f", [128, 128], F32)
    ltri_f = sb("ltri_f", [128, 128], F32)
    ones_f = sb("ones_f", [128, 128], F32)
    ones_b = sb("ones_b", [1, 128], BF16)
    cvec = sb("cvec", [128, CV_N], F32)
    wr_sb = sb("wr_sb", [128, 16, 36], F32)
    wsT = sb("wsT", [128, 8, 128], BF16)
    bspB = sb("bspB", [128, 8, 128], F32)
    wpool = sb("wpool", [128, 4, 2, 256], BF16)
    lnG = sb("lnG", [128, 1024], F32)
    lnB = sb("lnB", [128, 1024], F32)
    rbB = sb("rbB", [128, 36], F32)
    corr = sb("corr", [128, 4, HALO], F32)
    offs = sb("offs", [128, NE], F32)
    offs_i = sb("offs_i", [128, NE], I32)
    msum = sb("msum", [128, NE], F32)
    wt_all = sb("wt_all", [128, NT, 2], F32)
    idx_all = sb("idx_all", [128, NT, 2], I32)
    sm = sb("sm", [128, 336], F32)
    posb = sb("posb", [128, HALO], F32)
    epsb = sb("epsb", [128, 1], F32)

    psb = [nc.alloc_psum_tensor(f"ps{i}", [128, 512], F32) for i in range(8)]
    ps_tok = [cx.tok(f"ps{i}") for i in range(8)]
    ps_i = [0]

    def bank():
        i = ps_i[0] % 8
        ps_i[0] += 1
        return psb[i], ps_tok[i]

    t_const = cx.dma_tok("const")
    t_const_p = cx.dma_tok("const_p")
    t_cst2 = cx.tok("cst2")

    dma("sp", cvec[:, :], cvec_d[:, :], writes=[t_const])
    dma("sp", wr_sb[:, :, :], wr_d[:, :].rearrange("p (k n) -> p k n", k=16), writes=[t_const])
    dma("sp", lnG[:, :], rows_d[0:1, R_LNG:R_LNG + 1024].partition_broadcast(128), writes=[t_const])
    dma("sp", lnB[:, :], rows_d[0:1, R_LNB:R_LNB + 1024].partition_broadcast(128), writes=[t_const])
    dma("sp", bspB[:, :, :], rows_d[0:1, R_BSP:R_BSP + 1024].partition_broadcast(128), writes=[t_const])
    dma("sp", rbB[:, :], rows_d[0:1, R_RB:R_RB + 36].partition_broadcast(128), writes=[t_const])
    dma("sp", posb[:, :], rows_d[0:1, R_POS:R_POS + HALO].partition_broadcast(128), writes=[t_const])
    dma("pool", wpool[:, :, :, :], w_pool_d.rearrange("g (k p) n -> p g k n", p=128), writes=[t_const_p])

    op("pool", lambda g: g.memset(ones_f[:, :], 1.0), writes=[t_cst2])
    op("pool", lambda g: g.memset(ident_f[:, :], 1.0), writes=[t_cst2])
    op("pool", lambda g: g.affine_select(out=ident_f[:, :], in_=ident_f[:, :], pattern=[[1, 128]],
                                         compare_op=ALU.is_equal, fill=0.0, base=0, channel_multiplier=-1),
       writes=[t_cst2])
    op("pool", lambda g: g.memset(ltri_f[:, :], 1.0), writes=[t_cst2])
    op("pool", lambda g: g.affine_select(out=ltri_f[:, :], in_=ltri_f[:, :], pattern=[[1, 128]],
                                         compare_op=ALU.is_ge, fill=0.0, base=0, channel_multiplier=-1),
       writes=[t_cst2])
    op("pool", lambda g: g.iota(offs_i[:, :], pattern=[[CAP, NE]], base=-1, channel_multiplier=0), writes=[t_cst2])
    op("dve", lambda v: v.tensor_copy(out=ident_b[:, :], in_=ident_f[:, :]), reads=[t_cst2], writes=[t_cst2])
    op("dve", lambda v: v.tensor_copy(out=offs[:, :], in_=offs_i[:, :]), reads=[t_cst2], writes=[t_cst2])
    op("dve", lambda v: v.memset(ones_b[:, :], 1.0), writes=[t_cst2])
    op("dve", lambda v: v.memset(msum[:, :], 0.0), writes=[t_cst2])
    op("dve", lambda v: v.memset(epsb[:, :], EPS), writes=[t_cst2])
    for gi, w in enumerate((2, 4, 8, 16)):
        op("dve", lambda v, gi=gi, w=w: v.tensor_scalar(out=corr[:, gi, :], in0=posb[:, :], scalar1=float(w),
                                                        scalar2=None, op0=ALU.min),
           reads=[t_const], writes=[t_cst2])
        op("dve", lambda v, gi=gi: v.reciprocal(out=corr[:, gi, :], in_=corr[:, gi, :]), reads=[t_cst2], writes=[t_cst2])
        op("dve", lambda v, gi=gi, w=w: v.tensor_scalar(out=corr[:, gi, :], in0=corr[:, gi, :], scalar1=float(w),
                                                        scalar2=None, op0=ALU.mult),
           reads=[t_cst2], writes=[t_cst2])
    t_wsl = cx.dma_tok("wsl")
    for g8 in range(8):
        stg = scr[:, 0:128]
        dma("sp", stg, w_sp_d[g8], writes=[t_wsl])
        pb, pt = bank()
        op("pe", lambda pe, pb=pb: pe.transpose(out=pb[:, 0:128], in_=scr[:, 0:128], identity=ident_f[:, :]),
           reads=[t_wsl, t_cst2], writes=[pt])
        op("dve", lambda v, pb=pb, g8=g8: v.tensor_copy(out=wsT[:, g8, :], in_=pb[:, 0:128]), reads=[pt], writes=[t_cst2])
        t_wsl.r.update(pt.w)
    op("dve", lambda v: v.memset(wsT[64:128, :, 0:64], 0.0), reads=[t_cst2], writes=[t_cst2])
    for k in range(16):
        op("dve", lambda v, k=k: v.tensor_scalar(out=wr_sb[:, k, :], in0=wr_sb[:, k, :], scalar1=cvec[:, CV_GFFN + k:CV_GFFN + k + 1],
                                                 scalar2=None, op0=ALU.mult), reads=[t_const, t_cst2], writes=[t_cst2])
    CONST = [t_const, t_const_p, t_cst2]

    NSLOT_MIX = 3
    slot_pages = [0, 1, 2]
    slot_tok = {i: cx.dma_tok(f"wslot{i}") for i in range(NPAGE)}

    class WStream:
        def __init__(self):
            self.units = []
            self.issued = 0
            self.slots = list(slot_pages)

        def add(self, parts):
            self.units.append(parts)
            return len(self.units) - 1

        def slot_of(self, i):
            return self.units[i][0]

        def ensure(self, upto):
            while self.issued <= min(upto, len(self.units) - 1):
                pg_i, parts = self.units[self.issued]
                for dst, src in parts:
                    dma("pool", dst, src, writes=[slot_tok[pg_i]])
                self.issued += 1

    ws = WStream()

    def wview(pg_i, kc, n):
        return pages[pg_i][:, 0:kc * n].rearrange("p (k n) -> p k n", k=kc)

    def wsrc(w_ap, r0, nrows, c0, ncols):
        return w_ap[r0:r0 + nrows, c0:c0 + ncols].rearrange("(k p) n -> p k n", p=128)

    unit_seq = []
    slot_rr = [0]

    def next_slot(slots):
        s = slots[slot_rr[0] % len(slots)]
        slot_rr[0] += 1
        return s

    mix_slots = [0, 1, 2]

    def sched_unit(parts_fn, slots):
        s = next_slot(slots)
        return ws.add((s, parts_fn(s)))

    Xv = [pages[3 + (j // 2)][:, (j % 2) * 4096:(j % 2 + 1) * 4096].bitcast(F32) for j in range(4)]
    X_tok = [cx.dma_tok(f"X{j}") for j in range(4)]
    hT = pages[5][:, :].rearrange("p (k t) -> p k t", k=16)
    hT_tok = cx.tok("hT")
    mT = pages[6][:, :].rearrange("p (k t) -> p k t", k=16)
    mT_tok = cx.tok("mT")
    uT = pages[7][:, 0:4096].rearrange("p (k t) -> p k t", k=8)
    uT_tok = cx.tok("uT")
    vT = pages[7][:, 4096:8192].rearrange("p (j c) -> p j c", j=JT)
    vT_tok = cx.tok("vT")
    pzT = pages[8][:, 0:4096].rearrange("p (k t) -> p k t", k=8)
    pz_tok = cx.tok("pzT")
    ypT = pages[8][:, 4096:8192].rearrange("p (k t) -> p k t", k=8)
    yp_tok = cx.tok("ypT")
    aT_tok = cx.tok("aT")
    xn_tok = [cx.dma_tok(f"xn{i}") for i in range(2)]
    scr_tok = [cx.tok(f"scr{i}") for i in range(4)]
    sm_tok = cx.tok("sm")
    rt_tok = cx.tok("route")
    xs_tok = cx.dma_tok("xs")
    x1s_tok = [cx.dma_tok(f"x1s{i}") for i in range(NT)]
    hh_tok = cx.tok("hhalo")
    hhalo = sb("hhalo", [128, 16, HALO], BF16)

    def scrv(i, n=1088):
        return scr[:, i * 1088:i * 1088 + n]

    WIN_ORDER = (4, 5, 0, 1, 2, 3)
    plan = []
    for b in range(NB):
        blk = {}
        blk["win"] = {cb: sched_unit(lambda s, cb=cb: [(wview(s, 16, 512), wsrc(w_in_d, 0, D, cb * 512, 512))], mix_slots)
                      for cb in WIN_ORDER}
        blk["mrg"] = []
        for cb in range(4):
            u_g1 = sched_unit(lambda s, cb=cb: [(wview(s, 16, 512), wsrc(w_mg_d, 0, D, cb * 512, 512))], mix_slots)
            u_g2 = sched_unit(lambda s, cb=cb: [(wview(s, 16, 512), wsrc(w_mg_d, 0, D, D + cb * 512, 512))], mix_slots)
            u_ab = sched_unit(lambda s, cb=cb: [
                (pages[s][:, 0:4096].rearrange("p (k n) -> p k n", k=8), wsrc(w_ba_d, 0, 1024, cb * 512, 512)),
                (pages[s][:, 4096:8192].rearrange("p (k n) -> p k n", k=8), wsrc(w_bb_d, 0, 1024, cb * 512, 512))],
                mix_slots)
            blk["mrg"].append((u_ab, u_g1, u_g2))
        blk["wout"] = [sched_unit(lambda s, cb=cb: [(wview(s, 16, 512), wsrc(w_out_d, 0, D, cb * 512, 512))], mix_slots)
                       for cb in range(4)]
        plan.append(blk)
    n_mix_units = len(ws.units)

    def wget(u):
        ws.ensure(u)
        s = ws.units[u][0]
        return s, slot_tok[s]

    def wdone(u, nslots=3):
        ws.ensure(u + nslots)

    def rms_rstd(e_src, src_ap, src_toks, junk_ap, junk_toks, col, npart=128):
        sc = sm[0:npart, col:col + 1]
        op("act", lambda a: a.activation(out=junk_ap, in_=src_ap, func=AF.Square, accum_out=sc),
           reads=src_toks, writes=junk_toks + [sm_tok])
        op("act", lambda a: a.activation(out=sc, in_=sc, func=AF.Sqrt, scale=1.0 / D, bias=epsb[0:npart, :]),
           reads=[sm_tok] + CONST, writes=[sm_tok])
        op("dve", lambda v: v.reciprocal(out=sc, in_=sc), reads=[sm_tok], writes=[sm_tok])

    def transposes_bf(src_rows_ap, src_toks, dst_fn, dst_toks, gcol, evac_rr):
        for q in range(4):
            pb, pt = bank()
            pbb = pb[:, :].bitcast(BF16)
            for kk in range(4):
                k = q * 4 + kk
                op("pe", lambda pe, k=k, kk=kk, pbb=pbb: pe.transpose(out=pbb[:, kk * 128:(kk + 1) * 128],
                                                                       in_=src_rows_ap[:, k * 128:(k + 1) * 128],
                                                                       identity=ident_b[:, :]),
                   reads=src_toks + CONST, writes=[pt])
            evac_rr[0] += 1
            use_act = (evac_rr[0] % 2) == 0
            for kk in range(4):
                k = q * 4 + kk
                if use_act:
                    op("act", lambda a, k=k, kk=kk, pbb=pbb: a.activation(out=dst_fn(k), in_=pbb[:, kk * 128:(kk + 1) * 128],
                                                                          func=AF.Copy, scale=cvec[:, gcol + k:gcol + k + 1]),
                       reads=[pt] + CONST, wd=dst_toks)
                else:
                    op("dve", lambda v, k=k, kk=kk, pbb=pbb: v.tensor_scalar(out=dst_fn(k), in0=pbb[:, kk * 128:(kk + 1) * 128],
                                                                             scalar1=cvec[:, gcol + k:gcol + k + 1], scalar2=None,
                                                                             op0=ALU.mult),
                       reads=[pt] + CONST, wd=dst_toks)

    evac_rr = [0]

    def emit_layernorm():
            for j in range(JT):
                vg = scrv(j, 1024)
                st6 = sm[:, 16:28]
                op("dve", lambda v, vg=vg: v.bn_stats(out=sm[:, 16:22], in_=vg[:, 0:512]), reads=[scr_tok[j]], writes=[sm_tok])
                op("dve", lambda v, vg=vg: v.bn_stats(out=sm[:, 22:28], in_=vg[:, 512:1024]), reads=[scr_tok[j], sm_tok], writes=[sm_tok])
                op("dve", lambda v: v.bn_aggr(out=sm[:, 28:30], in_=sm[:, 16:28]), reads=[sm_tok], writes=[sm_tok])
                op("act", lambda a: a.activation(out=sm[:, 29:30], in_=sm[:, 29:30], func=AF.Sqrt, scale=1.0, bias=epsb[:, :]),
                   reads=[sm_tok] + CONST, writes=[sm_tok])
                op("dve", lambda v: v.reciprocal(out=sm[:, 29:30], in_=sm[:, 29:30]), reads=[sm_tok], writes=[sm_tok])
                op("dve", lambda v, vg=vg: v.scalar_tensor_tensor(out=vg, in0=vg, scalar=sm[:, 28:29], in1=lnG[:, :],
                                                                  op0=ALU.subtract, op1=ALU.mult),
                   reads=[scr_tok[j], sm_tok] + CONST, writes=[scr_tok[j]])
                op("dve", lambda v, vg=vg, j=j: v.scalar_tensor_tensor(out=vT[:, j, :], in0=vg, scalar=sm[:, 29:30], in1=lnB[:, :],
                                                                       op0=ALU.mult, op1=ALU.add),
                   reads=[scr_tok[j], sm_tok] + CONST, writes=[vT_tok])


    pg6f = pages[6][:, :].bitcast(F32)

    def emit_pool_dve(b):
        W = HALO + TB
        for gi, w in enumerate((2, 4, 8, 16)):
            ca = aT[:, 2 * gi:2 * gi + 2, :]
            bufA = pg6f[:, 0:2 * W].rearrange("p (k t) -> p k t", k=2)
            bufB = pg6f[:, 2 * W:4 * W].rearrange("p (k t) -> p k t", k=2)
            cur, cur_t = ca, aT_tok
            sh = 1
            lvl = 0
            while sh < w:
                dst = bufA if lvl % 2 == 0 else bufB
                lo = 2 * sh - 1
                op("dve", lambda v, dst=dst, cur=cur, lo=lo, sh=sh: v.tensor_tensor(out=dst[:, :, lo:W], in0=cur[:, :, lo:W],
                                                                                     in1=cur[:, :, lo - sh:W - sh], op=ALU.add),
                   reads=[cur_t], writes=[mT_tok])
                cur, cur_t = dst, mT_tok
                sh *= 2
                lvl += 1
            if b == 0:
                for c2 in range(2):
                    op("dve", lambda v, cur=cur, gi=gi, c2=c2: v.tensor_tensor(out=cur[:, c2, HALO:2 * HALO], in0=cur[:, c2, HALO:2 * HALO],
                                                                              in1=corr[:, gi, :], op=ALU.mult),
                       reads=[cur_t] + CONST, writes=[cur_t])
            op("dve", lambda v, cur=cur, ca=ca, gi=gi, w=w: v.scalar_tensor_tensor(out=pzT[:, 2 * gi:2 * gi + 2, :], in0=cur[:, :, HALO:W],
                                                                                   scalar=1.0 / w, in1=ca[:, :, HALO:W],
                                                                                   op0=ALU.mult, op1=ALU.subtract),
               reads=[cur_t, aT_tok], writes=[pz_tok])

    def stepA_halo():
        dma("sp", Xv[0][0:HALO, :], xh_d[:, :], writes=[X_tok[0]])
        rms_rstd("act", Xv[0][0:HALO, :], [X_tok[0]], xn[0][0:HALO, :], [xn_tok[0]], 0, npart=HALO)
        op("act", lambda a: a.activation(out=xn[0][0:HALO, :], in_=Xv[0][0:HALO, :], func=AF.Copy, scale=sm[0:HALO, 0:1]),
           reads=[X_tok[0], sm_tok], writes=[xn_tok[0]])
        for q in range(4):
            pb, pt = bank()
            pbb = pb[:, :].bitcast(BF16)
            for kk in range(4):
                k = q * 4 + kk
                op("pe", lambda pe, k=k, kk=kk, pbb=pbb: pe.transpose(out=pbb[:, kk * HALO:(kk + 1) * HALO],
                                                                       in_=xn[0][0:HALO, k * 128:(k + 1) * 128],
                                                                       identity=ident_b[0:HALO, 0:HALO]),
                   reads=[xn_tok[0]] + CONST, writes=[pt])
            for kk in range(4):
                k = q * 4 + kk
                op("dve", lambda v, k=k, kk=kk, pbb=pbb: v.tensor_scalar(out=hhalo[:, k, :], in0=pbb[:, kk * HALO:(kk + 1) * HALO],
                                                                         scalar1=cvec[:, CV_GMIX + k:CV_GMIX + k + 1],
                                                                         scalar2=None, op0=ALU.mult),
                   reads=[pt] + CONST, writes=[hh_tok])

    def stepA_load(b, j):
        t0 = b * TB
        dma("sp", Xv[j], x_d[t0 + j * 128:t0 + (j + 1) * 128, :], writes=[X_tok[j]])

    def stepA_tile(b, j, xi):
        xb = xn[xi]
        xt = xn_tok[xi]
        rms_rstd("act", Xv[j], [X_tok[j]], xb[:, :], [xt], j)
        op("act", lambda a, j=j, xb=xb: a.activation(out=xb[:, :], in_=Xv[j], func=AF.Copy, scale=sm[:, j:j + 1]),
           reads=[X_tok[j], sm_tok], writes=[xt])
        transposes_bf(xb, [xt], lambda k, j=j: hT[:, k, j * 128:(j + 1) * 128], [hT_tok], CV_GMIX, evac_rr)

    stepA_halo()
    for j in range(JT):
        stepA_load(0, j)
    for j in range(JT):
        stepA_tile(0, j, j % 2)

    for b in range(NB):
        blk = plan[b]
        t0 = b * TB
        if b > 0:
            op("dve", lambda v: v.tensor_copy(out=aT[:, :, 0:HALO], in_=aT[:, :, TB:TB + HALO]), reads=[aT_tok], writes=[aT_tok])
        for cbi, cb in enumerate(WIN_ORDER):
            if cbi > 0:
                wdone(blk["win"][WIN_ORDER[cbi - 1]])
            if cbi == 2:
                emit_layernorm()
            if cbi == 4:
                emit_pool_dve(b)
            s, st = wget(blk["win"][cb])
            wv = wview(s, 16, 512)
            if cb < 4:
                for mm in range(4):
                    m = (cb % 2) * 4 + mm
                    pb, pt = bank()
                    for k in range(16):
                        op("pe", lambda pe, k=k, mm=mm, pb=pb, wv=wv: pe.matmul(pb[:, :], lhsT=wv[:, k, mm * 128:(mm + 1) * 128],
                                                                               rhs=hT[:, k, :], start=(k == 0), stop=(k == 15)),
                           reads=[st, hT_tok], writes=[pt])
                    if cb < 2:
                        op("dve", lambda v, m=m, pb=pb: v.tensor_copy(out=aT[:, m, HALO:HALO + TB], in_=pb[:, :]),
                           reads=[pt], writes=[aT_tok])
                        if b == 0:
                            pb2, pt2 = bank()
                            for k in range(16):
                                op("pe", lambda pe, k=k, mm=mm, pb2=pb2, wv=wv: pe.matmul(pb2[:, 0:HALO], lhsT=wv[:, k, mm * 128:(mm + 1) * 128],
                                                                                         rhs=hhalo[:, k, :], start=(k == 0), stop=(k == 15)),
                                   reads=[st, hh_tok], writes=[pt2])
                            op("dve", lambda v, m=m, pb2=pb2: v.tensor_copy(out=aT[:, m, 0:HALO], in_=pb2[:, 0:HALO]),
                               reads=[pt2], writes=[aT_tok])
                    else:
                        op("act", lambda a, m=m, pb=pb: a.activation(out=uT[:, m, :], in_=pb[:, :], func=AF.Gelu_apprx_tanh),
                           reads=[pt], writes=[uT_tok])
            else:
                half = cb - 4
                for j in range(JT):
                    pb, pt = bank()
                    for k in range(16):
                        op("pe", lambda pe, k=k, j=j, pb=pb, wv=wv: pe.matmul(pb[:, :], lhsT=hT[:, k, j * 128:(j + 1) * 128],
                                                                             rhs=wv[:, k, :], start=(k == 0), stop=(k == 15)),
                           reads=[st, hT_tok], writes=[pt])
                    op("act", lambda a, j=j, half=half, pb=pb: a.activation(out=scrv(j)[:, half * 512:(half + 1) * 512], in_=pb[:, :],
                                                                            func=AF.Gelu_apprx_tanh),
                       reads=[pt], writes=[scr_tok[j]])
        wdone(blk["win"][WIN_ORDER[-1]])
        for gi in range(4):
            for mo in range(2):
                pb, pt = bank()
                for ki in range(2):
                    op("pe", lambda pe, gi=gi, mo=mo, ki=ki, pb=pb: pe.matmul(pb[:, :], lhsT=wpool[:, gi, ki, mo * 128:(mo + 1) * 128],
                                                                             rhs=pzT[:, 2 * gi + ki, :], start=(ki == 0), stop=(ki == 1)),
                       reads=[pz_tok] + CONST, writes=[pt])
                c = 2 * gi + mo
                op("act", lambda a, c=c, pb=pb: a.activation(out=ypT[:, c, :], in_=pb[:, :], func=AF.Copy,
                                                             scale=cvec[:, CV_PSC + c:CV_PSC + c + 1]),
                   reads=[pt] + CONST, writes=[yp_tok])
        for g8 in range(8):
            pb, pt = bank()
            for j in range(JT):
                op("pe", lambda pe, g8=g8, j=j, pb=pb: pe.matmul(pb[:, j * 128:(j + 1) * 128], lhsT=vT[:, j, g8 * 128:(g8 + 1) * 128],
                                                                 rhs=wsT[:, g8, :], start=True, stop=True),
                   reads=[vT_tok] + CONST, writes=[pt])
            tmp = scrv(1, 512)
            for j in range(JT):
                op("dve", lambda v, g8=g8, j=j, pb=pb, tmp=tmp: v.tensor_tensor(out=tmp[:, j * 128:(j + 1) * 128], in0=pb[:, j * 128:(j + 1) * 128],
                                                                               in1=bspB[:, g8, :], op=ALU.add),
                   reads=[pt] + CONST, writes=[scr_tok[1]])
            op("dve", lambda v, g8=g8, tmp=tmp: v.tensor_tensor(out=uT[:, g8, :], in0=uT[:, g8, :], in1=tmp, op=ALU.mult),
               reads=[scr_tok[1], uT_tok], writes=[uT_tok])

        for cb in range(4):
            u_ab, u_g1, u_g2 = blk["mrg"][cb]
            for gi2, ug_ in enumerate((u_g1, u_g2)):
                s_g, t_g = wget(ug_)
                wgm = wview(s_g, 16, 512)
                for mm in range(4):
                    m = cb * 4 + mm
                    p1, p1t = bank()
                    for k in range(16):
                        op("pe", lambda pe, k=k, mm=mm, p1=p1, wgm=wgm: pe.matmul(p1[:, :], lhsT=wgm[:, k, mm * 128:(mm + 1) * 128], rhs=hT[:, k, :],
                                                                                 start=(k == 0), stop=(k == 15)),
                           reads=[t_g, hT_tok], writes=[p1t])
                    sgt = scrv(mm)[:, gi2 * 512:(gi2 + 1) * 512]
                    op("act", lambda a, m=m, p1=p1, sgt=sgt, gi2=gi2: a.activation(out=sgt, in_=p1[:, :], func=AF.Sigmoid,
                                                                                    bias=cvec[:, CV_BMG + 16 * gi2 + m:CV_BMG + 16 * gi2 + m + 1], scale=1.0),
                       reads=[p1t] + CONST, writes=[scr_tok[mm]])
                wdone(ug_)
            s_ab, t_ab = wget(u_ab)
            wa = pages[s_ab][:, 0:4096].rearrange("p (k n) -> p k n", k=8)
            wb = pages[s_ab][:, 4096:8192].rearrange("p (k n) -> p k n", k=8)
            for mm in range(4):
                m = cb * 4 + mm
                pa, pat = bank()
                for k in range(8):
                    op("pe", lambda pe, k=k, mm=mm, pa=pa, wa=wa: pe.matmul(pa[:, :], lhsT=wa[:, k, mm * 128:(mm + 1) * 128], rhs=ypT[:, k, :],
                                                                           start=(k == 0), stop=(k == 7)),
                       reads=[t_ab, yp_tok], writes=[pat])
                pbk, pbt = bank()
                for k in range(8):
                    op("pe", lambda pe, k=k, mm=mm, pbk=pbk, wb=wb: pe.matmul(pbk[:, :], lhsT=wb[:, k, mm * 128:(mm + 1) * 128], rhs=uT[:, k, :],
                                                                             start=(k == 0), stop=(k == 7)),
                       reads=[t_ab, uT_tok], writes=[pbt])
                s1 = scrv(mm)[:, 0:512]
                s2 = scrv(mm)[:, 512:1024]
                op("dve", lambda v, pa=pa, s1=s1: v.tensor_tensor(out=s1, in0=s1, in1=pa[:, :], op=ALU.mult),
                   reads=[pat, scr_tok[mm]], writes=[scr_tok[mm]])
                op("dve", lambda v, pbk=pbk, s2=s2: v.tensor_tensor(out=s2, in0=s2, in1=pbk[:, :], op=ALU.mult),
                   reads=[pbt, scr_tok[mm]], writes=[scr_tok[mm]])
                op("dve", lambda v, m=m, s1=s1, s2=s2: v.tensor_tensor(out=mT[:, m, :], in0=s1, in1=s2, op=ALU.add),
                   reads=[scr_tok[mm]], writes=[mT_tok])
            wdone(u_ab)

        for cb in range(4):
            s, st = wget(blk["wout"][cb])
            wv = wview(s, 16, 512)
            for j in range(JT):
                pb, pt = bank()
                for k in range(16):
                    op("pe", lambda pe, k=k, j=j, pb=pb, wv=wv: pe.matmul(pb[:, :], lhsT=mT[:, k, j * 128:(j + 1) * 128], rhs=wv[:, k, :],
                                                                         start=(k == 0), stop=(k == 15)),
                       reads=[st, mT_tok], writes=[pt])
                op("dve", lambda v, j=j, cb=cb, pb=pb: v.tensor_tensor(out=Xv[j][:, cb * 512:(cb + 1) * 512], in0=Xv[j][:, cb * 512:(cb + 1) * 512],
                                                                      in1=pb[:, :], op=ALU.add),
                   reads=[pt, X_tok[j]], writes=[X_tok[j]])
            wdone(blk["wout"][cb])
        for j in range(JT):
            ti = b * JT + j
            if stage == 1:
                dma("sp", y_d[ti * 128:(ti + 1) * 128, :], Xv[j], reads=[X_tok[j]], writes=[x1s_tok[ti]])
                if b + 1 < NB:
                    stepA_load(b + 1, j)
                    stepA_tile(b + 1, j, (j + 1) % 2)
                continue
            dma("sp", x1_d[ti * 128:(ti + 1) * 128, :], Xv[j], reads=[X_tok[j]], writes=[x1s_tok[ti]])
            xb = xn[j % 2]
            xt = xn_tok[j % 2]
            rms_rstd("act", Xv[j], [X_tok[j]], xb[:, :], [xt], 8)
            op("act", lambda a, j=j, xb=xb: a.activation(out=xb[:, :], in_=Xv[j], func=AF.Copy, scale=sm[:, 8:9]),
               reads=[X_tok[j], sm_tok], writes=[xt])
            x1T = pages[8][:, 0:4096].bitcast(F32).rearrange("p (k t) -> p k t", k=16)
            for q in range(4):
                pb, pt = bank()
                for kk in range(4):
                    k = q * 4 + kk
                    op("pe", lambda pe, k=k, kk=kk, pb=pb, j=j: pe.transpose(out=pb[:, kk * 128:(kk + 1) * 128], in_=Xv[j][:, k * 128:(k + 1) * 128],
                                                                            identity=ident_f[:, :]),
                       reads=[X_tok[j]] + CONST, writes=[pt])
                x1q = x1T[:, q * 4:(q + 1) * 4, :]
                pbq = pb[:, :].rearrange("p (k t) -> p k t", k=4)
                if q % 2 == 0:
                    op("act", lambda a, pbq=pbq, x1q=x1q: a.activation(out=x1q, in_=pbq, func=AF.Copy), reads=[pt], wd=[pz_tok])
                else:
                    op("dve", lambda v, pbq=pbq, x1q=x1q: v.tensor_copy(out=x1q, in_=pbq), reads=[pt], wd=[pz_tok])
            pl, plt = bank()
            for k in range(16):
                op("pe", lambda pe, k=k, pl=pl: pe.matmul(pl[:, 0:36], lhsT=x1T[:, k, :], rhs=wr_sb[:, k, :], start=(k == 0), stop=(k == 15)),
                   reads=[pz_tok] + CONST, writes=[plt])
            if b + 1 < NB:
                stepA_load(b + 1, j)
                stepA_tile(b + 1, j, (j + 1) % 2)
            LG = sm[:, 32:68]
            GL = sm[:, 32:36]
            EL = sm[:, 36:68]
            GOH = sm[:, 68:72]
            PEN = sm[:, 72:76]
            EM = sm[:, 76:108]
            TOP = sm[:, 108:116]
            M0 = sm[:, 116:148]
            M1 = sm[:, 148:180]
            MM = sm[:, 180:212]
            RP = sm[:, 212:244]
            OK_ = sm[:, 244:276]
            JK = sm[:, 276:308]
            SC = sm[:, 308:324]
            GEX = sm[:, 324:328]
            R = [sm_tok]
            op("dve", lambda v, pl=pl: v.scalar_tensor_tensor(out=LG, in0=pl[:, 0:36], scalar=sm[:, 8:9], in1=rbB[:, :],
                                                              op0=ALU.mult, op1=ALU.add), reads=[plt, sm_tok] + CONST, writes=R)
            op("dve", lambda v: v.tensor_reduce(out=SC[:, 0:1], in_=GL, axis=AX.X, op=ALU.max), reads=R, writes=R)
            op("dve", lambda v: v.tensor_scalar(out=GOH, in0=GL, scalar1=SC[:, 0:1], scalar2=None, op0=ALU.is_ge), reads=R, writes=R)
            op("dve", lambda v: v.tensor_scalar(out=SC[:, 1:2], in0=SC[:, 0:1], scalar1=-1.0, scalar2=None, op0=ALU.mult), reads=R, writes=R)
            op("act", lambda a: a.activation(out=GEX, in_=GL, func=AF.Exp, bias=SC[:, 1:2], scale=1.0, accum_out=SC[:, 2:3]),
               reads=R, writes=R)
            op("dve", lambda v: v.reciprocal(out=SC[:, 3:4], in_=SC[:, 2:3]), reads=R, writes=R)
            op("dve", lambda v: v.tensor_scalar(out=PEN, in0=GOH, scalar1=1e30, scalar2=-1e30, op0=ALU.mult, op1=ALU.add), reads=R, writes=R)
            for g4 in range(4):
                op("dve", lambda v, g4=g4: v.tensor_scalar(out=EM[:, g4 * 8:(g4 + 1) * 8], in0=EL[:, g4 * 8:(g4 + 1) * 8],
                                                           scalar1=PEN[:, g4:g4 + 1], scalar2=None, op0=ALU.add), reads=R, writes=R)
            op("dve", lambda v: v.max(out=TOP, in_=EM), reads=R, writes=R)
            op("dve", lambda v: v.tensor_scalar(out=M0, in0=EM, scalar1=TOP[:, 0:1], scalar2=None, op0=ALU.is_equal), reads=R, writes=R)
            op("dve", lambda v: v.tensor_scalar(out=M1, in0=EM, scalar1=TOP[:, 1:2], scalar2=None, op0=ALU.is_equal), reads=R, writes=R)
            op("dve", lambda v: v.tensor_scalar(out=SC[:, 4:5], in0=TOP[:, 0:1], scalar1=-1.0, scalar2=None, op0=ALU.mult), reads=R, writes=R)
            op("act", lambda a: a.activation(out=SC[:, 5:6], in_=TOP[:, 1:2], func=AF.Exp, bias=SC[:, 4:5], scale=1.0), reads=R, writes=R)
            op("dve", lambda v: v.tensor_scalar(out=SC[:, 6:7], in0=SC[:, 5:6], scalar1=1.0, scalar2=None, op0=ALU.add), reads=R, writes=R)
            op("dve", lambda v: v.reciprocal(out=SC[:, 6:7], in_=SC[:, 6:7]), reads=R, writes=R)
            op("dve", lambda v: v.tensor_tensor(out=MM, in0=M0, in1=M1, op=ALU.add), reads=R, writes=R)
            pr, prt = bank()
            op("pe", lambda pe, pr=pr: pe.matmul(pr[:, 0:NE], lhsT=ltri_f[:, :], rhs=MM, start=True, stop=False),
               reads=R + CONST + [rt_tok], writes=[prt])
            op("pe", lambda pe, pr=pr: pe.matmul(pr[:, 0:NE], lhsT=ones_f[:, :], rhs=msum[:, :], start=False, stop=True),
               reads=R + CONST + [rt_tok], writes=[prt])
            op("dve", lambda v, pr=pr: v.tensor_tensor(out=RP, in0=pr[:, 0:NE], in1=offs[:, :], op=ALU.add), reads=[prt] + R + CONST, writes=R)
            op("dve", lambda v, pr=pr: v.tensor_scalar(out=OK_, in0=pr[:, 0:NE], scalar1=float(CAP), scalar2=None, op0=ALU.is_le),
               reads=[prt] + R, writes=R)
            op("dve", lambda v: v.tensor_tensor(out=msum[:, :], in0=msum[:, :], in1=MM, op=ALU.add), reads=R + [rt_tok], writes=[rt_tok])
            for kk, MK in enumerate((M0, M1)):
                op("dve", lambda v, kk=kk, MK=MK: v.tensor_tensor(out=JK, in0=MK, in1=RP, op=ALU.mult), reads=R, writes=R)
                op("dve", lambda v, kk=kk: v.tensor_reduce(out=SC[:, 7 + kk:8 + kk], in_=JK, axis=AX.X, op=ALU.add), reads=R, writes=R)
                op("dve", lambda v, kk=kk, MK=MK: v.tensor_tensor(out=JK, in0=MK, in1=OK_, op=ALU.mult), reads=R, writes=R)
                op("dve", lambda v, kk=kk: v.tensor_reduce(out=SC[:, 9 + kk:10 + kk], in_=JK, axis=AX.X, op=ALU.add), reads=R, writes=R)
                op("dve", lambda v, kk=kk: v.tensor_scalar(out=SC[:, 7 + kk:8 + kk], in0=SC[:, 7 + kk:8 + kk], scalar1=-BIG,
                                                           scalar2=SC[:, 9 + kk:10 + kk], op0=ALU.add, op1=ALU.mult), reads=R, writes=R)
                op("dve", lambda v, kk=kk: v.tensor_scalar(out=SC[:, 7 + kk:8 + kk], in0=SC[:, 7 + kk:8 + kk], scalar1=BIG,
                                                           scalar2=None, op0=ALU.add), reads=R, writes=R)
                op("dve", lambda v, kk=kk, ti=ti: v.tensor_copy(out=idx_all[:, ti, kk:kk + 1], in_=SC[:, 7 + kk:8 + kk]),
                   reads=R, writes=[rt_tok])
            op("dve", lambda v, ti=ti: v.tensor_scalar(out=wt_all[:, ti, 0:1], in0=SC[:, 3:4], scalar1=SC[:, 6:7], scalar2=SC[:, 9:10],
                                                       op0=ALU.mult, op1=ALU.mult), reads=R, writes=[rt_tok])
            op("dve", lambda v, ti=ti: v.tensor_scalar(out=SC[:, 11:12], in0=SC[:, 3:4], scalar1=SC[:, 6:7], scalar2=SC[:, 5:6],
                                                       op0=ALU.mult, op1=ALU.mult), reads=R, writes=R)
            op("dve", lambda v, ti=ti: v.tensor_scalar(out=wt_all[:, ti, 1:2], in0=SC[:, 11:12], scalar1=SC[:, 10:11], scalar2=None,
                                                       op0=ALU.mult), reads=R, writes=[rt_tok])
            for kk in range(2):
                dma("pool", xs_d[:, :], xb[:, :], reads=[xt, rt_tok], writes=[], dtok=xs_tok, indirect=True,
                    out_offset=bass.IndirectOffsetOnAxis(ap=idx_all[:, ti, kk:kk + 1], axis=0), in_offset=None,
                    bounds_check=bc_reg, oob_is_err=False)
            xs_tok.w = {xs_tok.dkey: xs_tok.dval}

    if stage == 1:
        fin = {}
        for t in x1s_tok:
            Ctx._merge(fin, t.w)
        cx._wait("sp", fin)
        return nc

    barrier = cx.snapshot()
    moe_slots = [0, 1, 2, 5, 6, 7]
    for i in (5, 6, 7):
        slot_tok[i].w = dict(barrier)
    Ysb = Xv
    Y_tok = [cx.dma_tok(f"Ysb{j}", init=barrier) for j in range(4)]
    XeT = [pages[8][:, i * 4096:(i + 1) * 4096].rearrange("p (k r) -> p k r", k=16) for i in range(2)]
    XeT_tok = [cx.tok(f"XeT{i}", init=barrier) for i in range(2)]
    hidT = [aT[:, :, :].rearrange("p k t -> p (k t)")[:, i * 1024:(i + 1) * 1024].rearrange("p (m r) -> p m r", m=4) for i in range(2)]
    hid_tok = [cx.tok(f"hid{i}", init=barrier) for i in range(2)]
    ys_tok = cx.dma_tok("ys")
    slot_rr[0] = 0
    exp_units = []
    for e in range(NE):
        ug = sched_unit(lambda s, e=e: [(wview(s, 16, 512), wsrc(w_eg_d[e], 0, D, 0, 512))], moe_slots)
        uu = sched_unit(lambda s, e=e: [(wview(s, 16, 512), wsrc(w_eu_d[e], 0, D, 0, 512))], moe_slots)
        ud = sched_unit(lambda s, e=e: [(wview(s, 4, 2048), wsrc(w_ed_d[e], 0, 512, 0, D))], moe_slots)
        exp_units.append((ug, uu, ud))

    def wget_moe(u):
        ws.ensure(u)
        s = ws.units[u][0]
        return s, slot_tok[s]

    ws.ensure(exp_units[0][0] + 5)

    yrr = [0]

    def moe_rows_load(e):
        for r in range(2):
            dma("sp", xn[r][:, :], xs_d[e * CAP + r * 128:e * CAP + (r + 1) * 128, :], reads=[xs_tok], writes=[xn_tok[r]])

    def moe_transposes(e):
        xe = XeT[e % 2]
        xet = XeT_tok[e % 2]
        for r in range(2):
            transposes_bf(xn[r], [xn_tok[r]], lambda k, r=r, xe=xe: xe[:, k, r * 128:(r + 1) * 128], [xet], CV_GFFN, evac_rr)

    moe_rows_load(0)
    moe_transposes(0)
    for e in range(NE):
        ug, uu, ud = exp_units[e]
        xe = XeT[e % 2]
        xet = XeT_tok[e % 2]
        if e + 1 < NE:
            moe_rows_load(e + 1)
        sg_, tg_ = wget_moe(ug)
        su_, tu_ = wget_moe(uu)
        sd_, td_ = wget_moe(ud)
        wg = wview(sg_, 16, 512)
        wu = wview(su_, 16, 512)
        wd = wview(sd_, 4, 2048)
        hd = hidT[e % 2]
        hdt = hid_tok[e % 2]
        gu_banks = [bank() for _ in range(4)]
        for m in range(4):
            pb, pt = gu_banks[m]
            for k in range(16):
                op("pe", lambda pe, k=k, m=m, pb=pb: pe.matmul(pb[:, 0:CAP], lhsT=wg[:, k, m * 128:(m + 1) * 128], rhs=xe[:, k, :],
                                                               start=(k == 0), stop=(k == 15)), reads=[tg_, xet], writes=[pt])
        wdone(ug, 6)
        for m in range(4):
            pb, pt = gu_banks[m]
            for k in range(16):
                op("pe", lambda pe, k=k, m=m, pb=pb: pe.matmul(pb[:, CAP:2 * CAP], lhsT=wu[:, k, m * 128:(m + 1) * 128], rhs=xe[:, k, :],
                                                               start=(k == 0), stop=(k == 15)), reads=[tu_, xet], writes=[pt])
        wdone(uu, 6)
        for m in range(4):
            pb, pt = gu_banks[m]
            si = m % 2 + 2
            sg = scrv(si, CAP)
            op("act", lambda a, pb=pb, sg=sg: a.activation(out=sg, in_=pb[:, 0:CAP], func=AF.Silu), reads=[pt], writes=[scr_tok[si]])
            op("dve", lambda v, pb=pb, sg=sg, m=m: v.tensor_tensor(out=hd[:, m, :], in0=sg, in1=pb[:, CAP:2 * CAP], op=ALU.mult),
               reads=[pt, scr_tok[si]], writes=[hdt])
        if e + 1 < NE:
            moe_transposes(e + 1)
        for r in range(2):
            yi = yrr[0] % 4
            yrr[0] += 1
            for cb in range(4):
                pb, pt = bank()
                for m in range(4):
                    op("pe", lambda pe, m=m, r=r, cb=cb, pb=pb: pe.matmul(pb[:, :], lhsT=hd[:, m, r * 128:(r + 1) * 128],
                                                                         rhs=wd[:, m, cb * 512:(cb + 1) * 512], start=(m == 0), stop=(m == 3)),
                       reads=[td_, hdt], writes=[pt])
                if cb % 2 == 0:
                    op("act", lambda a, yi=yi, cb=cb, pb=pb: a.activation(out=Ysb[yi][:, cb * 512:(cb + 1) * 512], in_=pb[:, :], func=AF.Copy),
                       reads=[pt], wd=[Y_tok[yi]])
                else:
                    op("dve", lambda v, yi=yi, cb=cb, pb=pb: v.tensor_copy(out=Ysb[yi][:, cb * 512:(cb + 1) * 512], in_=pb[:, :]),
                       reads=[pt], wd=[Y_tok[yi]])
            dma("sp", ys_d[e * CAP + r * 128:e * CAP + (r + 1) * 128, :], Ysb[yi], reads=[Y_tok[yi]], writes=[], dtok=ys_tok)
        wdone(ud, 6)
    ys_tok.w = {ys_tok.dkey: ys_tok.dval}

    barrier2 = cx.snapshot()
    pg_pages = [0, 1, 2, 5]
    for ci, pgi in enumerate(pg_pages):
        dma("pool", wview(pgi, 16, 512), wsrc(w_pg_d, 0, D, ci * 512, 512), writes=[slot_tok[pgi]])
    wpg_tok = slot_tok[6]
    wpu = pages[6][:, 0:4096].rearrange("p (k n) -> p k n", k=2)
    dma("pool", wpu, w_pu_d.rearrange("(k p) n -> p k n", p=128), writes=[wpg_tok])
    gfin = pages[6][:, 4096:8192].bitcast(F32)
    bpg_b = scr[0:1, 3 * 1088:3 * 1088 + 1024].bitcast(BF16)
    scr_tok[3].w = dict(barrier2)
    bpg_tok = cx.dma_tok("bpg", init=barrier2)
    dma("pool", bpg_b, rows_d[0:1, R_BPG:R_BPG + D], writes=[bpg_tok])
    gfin_tok = cx.dma_tok("gfin")
    gfin_tok.w = dict(slot_tok[6].w)
    gfin_tok.r = dict(slot_tok[6].r)
    dma("sp", gfin, rows_d[0:1, R_GFIN:R_GFIN + D].partition_broadcast(128), writes=[gfin_tok])
    Gv = [pages[7 + (i // 2)][:, (i % 2) * 4096:(i % 2 + 1) * 4096].bitcast(F32) for i in range(4)]
    G_tok = [cx.dma_tok(f"G{i}", init=barrier2) for i in range(4)]
    for i in range(4):
        op("dve", lambda v, i=i: v.memset(Gv[i], 0.0), writes=[G_tok[i]])
    Xf_tok = [cx.dma_tok(f"Xf{j}", init=barrier2) for j in range(4)]
    h3T = [aT[:, :, :].rearrange("p k t -> p (k t)")[:, i * 2048:(i + 1) * 2048].rearrange("p (k t) -> p k t", k=16) for i in range(2)]
    h3_tok = [cx.tok(f"h3T{i}", init=barrier2) for i in range(2)]
    pbf = [sb(f"pbf{i}", [128, 256], BF16) for i in range(2)]
    pbf_tok = [cx.dma_tok(f"pbf{i}") for i in range(2)]
    pT = [sb(f"pT{i}", [128, 2, 128], BF16) for i in range(2)]
    pT_tok = [cx.tok(f"pT{i}") for i in range(2)]
    out_tok = cx.dma_tok("out")
    for st_ in scr_tok:
        pass
    def ple_A(ti):
        j = ti % 4
        Xt, Xk = Xv[j], Xf_tok[j]
        dma("sp", Xt, x1_d[ti * 128:(ti + 1) * 128, :], reads=[x1s_tok[ti]], writes=[Xk])
        for kk in range(2):
            gi = (ti % 2) * 2 + kk
            dma("pool", Gv[gi], ys_d[:, :], reads=[ys_tok, rt_tok], writes=[G_tok[gi]], indirect=True, out_offset=None,
                in_offset=bass.IndirectOffsetOnAxis(ap=idx_all[:, ti, kk:kk + 1], axis=0),
                bounds_check=bc_reg, oob_is_err=False)
        dma("pool", pbf[ti % 2][:, :], p_d[ti * 128:(ti + 1) * 128, :], writes=[pbf_tok[ti % 2]])
        for kk in range(2):
            gi = (ti % 2) * 2 + kk
            op("dve", lambda v, gi=gi, ti=ti, kk=kk, Xt=Xt: v.scalar_tensor_tensor(out=Xt, in0=Gv[gi], scalar=wt_all[:, ti, kk:kk + 1], in1=Xt,
                                                                                  op0=ALU.mult, op1=ALU.add),
               reads=[G_tok[gi], rt_tok, Xk], writes=[Xk])
        xb = xn[ti % 2]
        xt = xn_tok[ti % 2]
        rms_rstd("act", Xt, [Xk], xb[:, :], [xt], 9)
        op("act", lambda a, xb=xb, Xt=Xt: a.activation(out=xb[:, :], in_=Xt, func=AF.Copy, scale=sm[:, 9:10]),
           reads=[Xk, sm_tok], writes=[xt])

    def ple_C(ti):
        xb = xn[ti % 2]
        xt = xn_tok[ti % 2]
        h3 = h3T[ti % 2]
        h3t = h3_tok[ti % 2]
        transposes_bf(xb, [xt], lambda k, h3=h3: h3[:, k, :], [h3t], CV_GPLE, evac_rr)
        pb, pt = bank()
        pbb = pb[:, :].bitcast(BF16)
        for kk in range(2):
            op("pe", lambda pe, kk=kk, pbb=pbb, ti=ti: pe.transpose(out=pbb[:, kk * 128:(kk + 1) * 128], in_=pbf[ti % 2][:, kk * 128:(kk + 1) * 128],
                                                                   identity=ident_b[:, :]), reads=[pbf_tok[ti % 2]] + CONST, writes=[pt])
        op("dve", lambda v, pbb=pbb, ti=ti: v.tensor_copy(out=pT[ti % 2][:, :, :], in_=pbb[:, 0:256].rearrange("p (k t) -> p k t", k=2)),
           reads=[pt], writes=[pT_tok[ti % 2]])

    def ple_B(ti):
        j = ti % 4
        Xt, Xk = Xv[j], Xf_tok[j]
        xb = xn[ti % 2]
        xt = xn_tok[ti % 2]
        h3 = h3T[ti % 2]
        h3t = h3_tok[ti % 2]
        for cb in range(4):
            wg_ = wview(pg_pages[cb], 16, 512)
            pg_, pgt = bank()
            for k in range(16):
                op("pe", lambda pe, k=k, pg_=pg_, wg_=wg_, h3=h3: pe.matmul(pg_[:, :], lhsT=h3[:, k, :], rhs=wg_[:, k, :], start=(k == 0), stop=False),
                   reads=[slot_tok[pg_pages[cb]], h3t], writes=[pgt])
            op("pe", lambda pe, pg_=pg_, cb=cb: pe.matmul(pg_[:, :], lhsT=ones_b[0:1, :], rhs=bpg_b[0:1, cb * 512:(cb + 1) * 512], start=False, stop=True),
               reads=CONST + [bpg_tok], writes=[pgt])
            pu_, put = bank()
            for k in range(2):
                op("pe", lambda pe, k=k, pu_=pu_, cb=cb, ti=ti: pe.matmul(pu_[:, :], lhsT=pT[ti % 2][:, k, :], rhs=wpu[:, k, cb * 512:(cb + 1) * 512],
                                                                         start=(k == 0), stop=(k == 1)), reads=[wpg_tok, pT_tok[ti % 2]], writes=[put])
            si = cb % 2
            sg = scrv(si, 512)
            op("act", lambda a, pg_=pg_, sg=sg: a.activation(out=sg, in_=pg_[:, :], func=AF.Sigmoid), reads=[pgt], writes=[scr_tok[si]])
            op("dve", lambda v, pu_=pu_, sg=sg: v.tensor_tensor(out=sg, in0=sg, in1=pu_[:, :], op=ALU.mult), reads=[put, scr_tok[si]], writes=[scr_tok[si]])
            op("dve", lambda v, sg=sg, cb=cb, Xt=Xt: v.tensor_tensor(out=Xt[:, cb * 512:(cb + 1) * 512], in0=Xt[:, cb * 512:(cb + 1) * 512], in1=sg, op=ALU.add),
               reads=[scr_tok[si], Xk], writes=[Xk])

    def ple_B2(ti):
        j = ti % 4
        Xt, Xk = Xv[j], Xf_tok[j]
        xb = xn[ti % 2]
        xt = xn_tok[ti % 2]
        rms_rstd("act", Xt, [Xk], xb[:, :], [xt], 10)
        op("dve", lambda v, Xt=Xt: v.scalar_tensor_tensor(out=Xt, in0=Xt, scalar=sm[:, 10:11], in1=gfin, op0=ALU.mult, op1=ALU.mult),
           reads=[Xk, sm_tok, gfin_tok], writes=[Xk])
        dma("sp", y_d[ti * 128:(ti + 1) * 128, :], Xt, reads=[Xk], writes=[], dtok=out_tok)

    ple_A(0)
    ple_C(0)
    for ti in range(NT):
        if ti + 1 < NT:
            ple_A(ti + 1)
        ple_B(ti)
        if ti + 1 < NT:
            ple_C(ti + 1)
        ple_B2(ti)
    if DEBUG_X1:
        dbg_w = nc.dram_tensor("dbg_w", [128, NT * 2], F32, kind="ExternalOutput").ap()
        dbg_i = nc.dram_tensor("dbg_i", [128, NT * 2], I32, kind="ExternalOutput").ap()
        dma("sp", dbg_w[:, :], wt_all[:, :, :].rearrange("p a b -> p (a b)"), reads=[rt_tok], writes=[], dtok=out_tok)
        dma("sp", dbg_i[:, :], idx_all[:, :, :].rearrange("p a b -> p (a b)"), reads=[rt_tok], writes=[], dtok=out_tok)
    cx._wait("sp", {out_tok.dkey: out_tok.dval})
    return nc


_NC_CACHE = {}


def _prep_inputs(inp):
    f = lambda a: np.ascontiguousarray(np.asarray(a, dtype=np.float32))
    x = f(inp["x"]).reshape(4, 4096, D)
    p = f(inp["p"]).reshape(4, 4096, 256)

    def pk(v):
        v = f(v).reshape(-1)
        return v.reshape(-1, 128).T

    cvec = np.concatenate([pk(inp["g_mix"]), pk(inp["g_ffn"]), pk(inp["g_ple"]), pk(inp["pool_scale"]),
                           pk(inp["b_merge_gate"])], axis=1)
    assert cvec.shape == (128, CV_N)
    wrg = f(inp["w_router_group"]).reshape(D, 4)
    wre = f(inp["w_router_expert"]).reshape(4, D, 8).transpose(1, 0, 2).reshape(D, 32)
    wr = np.concatenate([wrg, wre], axis=1)
    wr = wr.reshape(16, 128, 36).transpose(1, 0, 2).reshape(128, 16 * 36)
    rb = np.concatenate([f(inp["b_router_group"]).reshape(-1), f(inp["b_router_expert"]).reshape(-1)])
    shared = {
        "cvec": np.ascontiguousarray(cvec),
        "wr": np.ascontiguousarray(wr),
        "w_in": f(inp["w_in"]).reshape(D, 3072),
        "w_pool": f(inp["w_pool"]).reshape(4, 256, 256),
        "w_ba": f(inp["w_branch_a"]).reshape(1024, D),
        "w_bb": f(inp["w_branch_b"]).reshape(1024, D),
        "w_sp": f(inp["w_spatial"]).reshape(8, 128, 128),
        "w_mg": f(inp["w_merge_gate"]).reshape(D, 2 * D),
        "w_out": f(inp["w_out"]).reshape(D, D),
        "w_eg": f(inp["w_exp_gate"]).reshape(NE, D, 512),
        "w_eu": f(inp["w_exp_up"]).reshape(NE, D, 512),
        "w_ed": f(inp["w_exp_down"]).reshape(NE, 512, D),
        "w_pg": f(inp["w_ple_gate"]).reshape(D, D),
        "w_pu": f(inp["w_ple_up"]).reshape(256, D),
    }
    maps = []
    for c in range(NCORES):
        bi, half = c // 2, c % 2
        t0 = half * T
        rows = np.zeros((1, R_N), np.float32)
        rows[0, R_LNG:R_LNG + 1024] = f(inp["sgu_ln_g"]).reshape(-1)
        rows[0, R_LNB:R_LNB + 1024] = f(inp["sgu_ln_b"]).reshape(-1)
        rows[0, R_BSP:R_BSP + 1024] = f(inp["b_spatial"]).reshape(-1)
        rows[0, R_RB:R_RB + 36] = rb
        rows[0, R_GFIN:R_GFIN + D] = f(inp["g_final"]).reshape(-1)
        rows[0, R_BPG:R_BPG + D] = f(inp["b_ple_gate"]).reshape(-1)
        rows[0, R_POS:R_POS + HALO] = t0 + 1 + np.arange(HALO)
        xh = np.zeros((HALO, D), np.float32)
        if half == 1:
            xh[:] = x[bi, t0 - HALO:t0]
        m = dict(shared)
        m["x"] = np.ascontiguousarray(x[bi, t0:t0 + T])
        m["xh"] = xh
        m["p"] = np.ascontiguousarray(p[bi, t0:t0 + T])
        m["rows"] = rows
        maps.append(m)
    return maps


def kernel(**inputs):
    stage = 2
    if stage not in _NC_CACHE:
        _NC_CACHE[stage] = build(stage)
    nc = _NC_CACHE[stage]
    maps = _prep_inputs(inputs)
    res = run_bass_kernel_spmd(nc, maps, core_ids=list(range(NCORES)))
    out = np.empty((4, 4096, D), np.float32)
    for c in range(NCORES):
        out[c // 2, (c % 2) * T:(c % 2 + 1) * T] = res.results[c]["y"]
    return out
```
